# Optimizing a Trainium2 kernel written in Bass

```python
import math
import jax
import jax.numpy as jnp
from jax import lax
import numpy as np

D_MODEL = 1024
BATCH = 8
SEQ = 4096
DEPTH = 4

N_MIXERS = 4
HEAD_DIM = 64
ROPE_THETA = 500000.0
ROT_DIM = HEAD_DIM // 4
Q_BLOCK = 128

A_HEADS = 16
A_KV_HEADS = 4
A_WINDOW = 128
B_HEADS = 16
B_Q_RANK = 384
B_KV_RANK = 256
B_NOPE = 64
B_ROPE = 32
B_V = 64
C_HEADS = 16
C_KV_HEADS = 4
C_CMP_LEN = 32
C_CMP_STRIDE = 16
C_CMP_HIDDEN = 128
C_SEL_LEN = 64
C_N_SEL = 16
C_WINDOW = 512
D_HEADS = 8
D_SUB = 64
M_GROUPS = 4
M_PER_GROUP = 8
M_EXPERTS = M_GROUPS * M_PER_GROUP
M_TOPK = 2
M_HIDDEN = 512

LN_EPS = 1e-5
RMS_EPS = 1e-6

kernel_name = 'hybrid_interleaved_deepnorm_hmoe'


def layer_norm(x, g, b):
    xf = x.astype(jnp.float32)
    mu = jnp.mean(xf, -1, keepdims=True)
    var = jnp.mean(jnp.square(xf - mu), -1, keepdims=True)
    return ((xf - mu) * lax.rsqrt(var + LN_EPS) * g + b).astype(x.dtype)


def rms_norm(x, g):
    xf = x.astype(jnp.float32)
    return (xf * lax.rsqrt(jnp.mean(jnp.square(xf), -1, keepdims=True) + RMS_EPS) * g).astype(x.dtype)


def rope(x, rot_dim):
    s = x.shape[1]
    half = rot_dim // 2
    inv_freq = 1.0 / (ROPE_THETA ** (jnp.arange(half, dtype=jnp.float32) * (2.0 / rot_dim)))
    ang = jnp.arange(s, dtype=jnp.float32)[:, None] * inv_freq[None, :]
    cos = jnp.cos(ang)[None, :, None, :]
    sin = jnp.sin(ang)[None, :, None, :]
    xr = x[..., :rot_dim].astype(jnp.float32)
    x1, x2 = xr[..., :half], xr[..., half:]
    rot = jnp.concatenate([x1 * cos - x2 * sin, x2 * cos + x1 * sin], -1).astype(x.dtype)
    return jnp.concatenate([rot, x[..., rot_dim:]], -1)


def masked_softmax(s, mask):
    s = jnp.where(mask, s, -jnp.inf)
    m = jnp.max(s, -1, keepdims=True)
    m = jnp.where(jnp.isfinite(m), m, 0.0)
    e = jnp.where(mask, jnp.exp(s - m), 0.0)
    return e / jnp.maximum(jnp.sum(e, -1, keepdims=True), 1e-30)


def causal_attention(q, k, v, scale):
    b, s, h, dq = q.shape
    nb = s // Q_BLOCK
    qb = q.reshape(b, nb, Q_BLOCK, h, dq).transpose(1, 0, 2, 3, 4)
    kpos = jnp.arange(s)

    def block(args):
        qi, blk = args
        sc = jnp.einsum('bqhd,bkhd->bhqk', qi, k).astype(jnp.float32) * scale
        qpos = blk * Q_BLOCK + jnp.arange(Q_BLOCK)
        sc = jnp.where(kpos[None, :] <= qpos[:, None], sc, -jnp.inf)
        p = jax.nn.softmax(sc, axis=-1)
        return jnp.einsum('bhqk,bkhd->bqhd', p.astype(v.dtype), v)

    o = lax.map(block, (qb, jnp.arange(nb)))
    return o.transpose(1, 0, 2, 3, 4).reshape(b, s, h, v.shape[-1])


def swa_sink_gqa(x, w_in, sinks, w_o):
    b, s, _ = x.shape
    g = A_HEADS // A_KV_HEADS
    blk = A_WINDOW
    nb = s // blk
    qd, kd = A_HEADS * HEAD_DIM, A_KV_HEADS * HEAD_DIM
    qkv = x @ w_in
    q = rope(qkv[..., :qd].reshape(b, s, A_HEADS, HEAD_DIM), ROT_DIM)
    k = rope(qkv[..., qd:qd + kd].reshape(b, s, A_KV_HEADS, HEAD_DIM), ROT_DIM)
    v = qkv[..., qd + kd:].reshape(b, s, A_KV_HEADS, HEAD_DIM)
    qb = q.reshape(b, nb, blk, A_KV_HEADS, g, HEAD_DIM)

    def band(t):
        tb = t.reshape(b, nb, blk, A_KV_HEADS, HEAD_DIM)
        prev = jnp.pad(tb, ((0, 0), (1, 0), (0, 0), (0, 0), (0, 0)))[:, :-1]
        return jnp.concatenate([prev, tb], axis=2)

    kb, vb = band(k), band(v)
    sc = jnp.einsum('bnqhgd,bnkhd->bnhgqk', qb, kb).astype(jnp.float32) * HEAD_DIM ** -0.5
    n = jnp.arange(nb)[:, None, None]
    i = jnp.arange(blk)[None, :, None]
    j = jnp.arange(2 * blk)[None, None, :]
    dist = blk + i - j
    mask = (dist >= 0) & (dist < A_WINDOW) & ((n - 1) * blk + j >= 0)
    sc = jnp.where(mask[None, :, None, None], sc, -jnp.inf)
    sink = sinks.astype(jnp.float32).reshape(A_KV_HEADS, g)[None, None, :, :, None, None]
    m = jnp.maximum(jnp.max(sc, -1, keepdims=True), sink)
    e = jnp.exp(sc - m)
    p = e / (jnp.sum(e, -1, keepdims=True) + jnp.exp(sink - m))
    o = jnp.einsum('bnhgqk,bnkhd->bnqhgd', p.astype(vb.dtype), vb)
    return o.reshape(b, s, qd) @ w_o


def mla(x, w_down, q_norm, kv_norm, w_uq, w_ukv, w_o):
    b, s, _ = x.shape
    c = x @ w_down
    cq = rms_norm(c[..., :B_Q_RANK], q_norm)
    ckv = rms_norm(c[..., B_Q_RANK:B_Q_RANK + B_KV_RANK], kv_norm)
    kr = c[..., B_Q_RANK + B_KV_RANK:]
    q = (cq @ w_uq).reshape(b, s, B_HEADS, B_NOPE + B_ROPE)
    q = jnp.concatenate([q[..., :B_NOPE], rope(q[..., B_NOPE:], B_ROPE)], -1)
    kv = (ckv @ w_ukv).reshape(b, s, B_HEADS, B_NOPE + B_V)
    k_rope = rope(kr[:, :, None, :], B_ROPE)
    k = jnp.concatenate([kv[..., :B_NOPE], jnp.broadcast_to(k_rope, (b, s, B_HEADS, B_ROPE))], -1)
    o = causal_attention(q, k, kv[..., B_NOPE:], (B_NOPE + B_ROPE) ** -0.5)
    return o.reshape(b, s, B_HEADS * B_V) @ w_o


def nsa(x, w_in, pos_k, pos_v, wk1, wk2, wv1, wv2, w_o):
    b, s, _ = x.shape
    g = C_HEADS // C_KV_HEADS
    qd, kd = C_HEADS * HEAD_DIM, C_KV_HEADS * HEAD_DIM
    h = x @ w_in
    q = rope(h[..., :qd].reshape(b, s, C_HEADS, HEAD_DIM), ROT_DIM)
    kv = h[..., qd:qd + 6 * kd].reshape(b, s, 6, C_KV_HEADS, HEAD_DIM)
    k_c, v_c = kv[:, :, 0], kv[:, :, 1]
    k_s, v_s = rope(kv[:, :, 2], ROT_DIM), kv[:, :, 3]
    k_w, v_w = rope(kv[:, :, 4], ROT_DIM), kv[:, :, 5]
    gates = jax.nn.sigmoid(h[..., qd + 6 * kd:].astype(jnp.float32)).reshape(b, s, C_HEADS, 3)

    nc = (s - C_CMP_LEN) // C_CMP_STRIDE + 1
    idx = jnp.arange(nc)[:, None] * C_CMP_STRIDE + jnp.arange(C_CMP_LEN)[None, :]

    def compress(t, pe, w1, w2):
        blocks = t[:, idx] + pe[None, None, :, None, :]
        flat = blocks.transpose(0, 1, 3, 2, 4).reshape(b, nc, C_KV_HEADS, C_CMP_LEN * HEAD_DIM)
        return jax.nn.gelu(flat @ w1) @ w2

    k_cmp = compress(k_c, pos_k, wk1, wk2)
    v_cmp = compress(v_c, pos_v, wv1, wv2)
    cmp_start = jnp.arange(nc) * C_CMP_STRIDE
    cmp_end = cmp_start + C_CMP_LEN - 1

    nsb = s // C_SEL_LEN
    n_sel = min(C_N_SEL, nsb)
    sel_start = jnp.arange(nsb) * C_SEL_LEN
    overlap = ((cmp_start[:, None] <= sel_start[None, :] + C_SEL_LEN - 1)
               & (sel_start[None, :] <= cmp_end[:, None])).astype(jnp.float32)
    ks_blk = k_s.reshape(b, nsb, C_SEL_LEN, C_KV_HEADS, HEAD_DIM).transpose(0, 3, 1, 2, 4)
    vs_blk = v_s.reshape(b, nsb, C_SEL_LEN, C_KV_HEADS, HEAD_DIM).transpose(0, 3, 1, 2, 4)
    kw_pad = jnp.pad(k_w, ((0, 0), (C_WINDOW, 0), (0, 0), (0, 0)))
    vw_pad = jnp.pad(v_w, ((0, 0), (C_WINDOW, 0), (0, 0), (0, 0)))

    nqb = s // Q_BLOCK
    qf = q.reshape(b * nqb, Q_BLOCK, C_KV_HEADS, g, HEAD_DIM)
    b_ids = jnp.repeat(jnp.arange(b), nqb)
    blk_ids = jnp.tile(jnp.arange(nqb), b)
    scale = HEAD_DIM ** -0.5
    heads = jnp.arange(C_KV_HEADS)[None, :, None]

    def block(args):
        qi, bi, blk = args
        t = blk * Q_BLOCK + jnp.arange(Q_BLOCK)
        sc = jnp.einsum('qhgd,nhd->qhgn', qi, k_cmp[bi]).astype(jnp.float32) * scale
        p_cmp = masked_softmax(sc, (cmp_end[None, :] <= t[:, None])[:, None, None, :])
        o_cmp = jnp.einsum('qhgn,nhd->qhgd', p_cmp.astype(v_cmp.dtype), v_cmp[bi])
        imp = jnp.einsum('qhgn,nj->qhj', p_cmp, overlap)
        cur = (t // C_SEL_LEN)[:, None, None]
        jb = jnp.arange(nsb)[None, None, :]
        imp = jnp.where((jb == 0) | (jb == cur) | (jb == cur - 1), jnp.inf, imp)
        imp = jnp.where(jb <= cur, imp, -jnp.inf)
        _, sel = lax.top_k(imp, n_sel)
        kg = ks_blk[bi][heads, sel]
        vg = vs_blk[bi][heads, sel].reshape(Q_BLOCK, C_KV_HEADS, n_sel * C_SEL_LEN, HEAD_DIM)
        sc = jnp.einsum('qhgd,qhnld->qhgnl', qi, kg).astype(jnp.float32)
        sc = sc.reshape(Q_BLOCK, C_KV_HEADS, g, n_sel * C_SEL_LEN) * scale
        kpos = (sel[..., None] * C_SEL_LEN + jnp.arange(C_SEL_LEN)).reshape(
            Q_BLOCK, C_KV_HEADS, 1, n_sel * C_SEL_LEN)
        p_slc = masked_softmax(sc, kpos <= t[:, None, None, None])
        o_slc = jnp.einsum('qhgk,qhkd->qhgd', p_slc.astype(vg.dtype), vg)
        kwb = lax.dynamic_slice_in_dim(kw_pad[bi], blk * Q_BLOCK, Q_BLOCK + C_WINDOW, axis=0)
        vwb = lax.dynamic_slice_in_dim(vw_pad[bi], blk * Q_BLOCK, Q_BLOCK + C_WINDOW, axis=0)
        wpos = blk * Q_BLOCK - C_WINDOW + jnp.arange(Q_BLOCK + C_WINDOW)
        dist = t[:, None] - wpos[None, :]
        wmask = (dist >= 0) & (dist < C_WINDOW) & (wpos[None, :] >= 0)
        sc = jnp.einsum('qhgd,khd->qhgk', qi, kwb).astype(jnp.float32) * scale
        p_win = masked_softmax(sc, wmask[:, None, None, :])
        o_win = jnp.einsum('qhgk,khd->qhgd', p_win.astype(vwb.dtype), vwb)
        return jnp.stack([o_cmp, o_slc, o_win], axis=-1)

    o = lax.map(block, (qf, b_ids, blk_ids)).reshape(b, s, C_HEADS, HEAD_DIM, 3)
    o = jnp.sum(o * gates[:, :, :, None, :].astype(o.dtype), axis=-1)
    return o.reshape(b, s, qd) @ w_o


def diff_attention(x, w_in, lq1, lk1, lq2, lk2, subln, w_o, layer_idx):
    b, s, _ = x.shape
    qd = D_HEADS * 2 * D_SUB
    h = x @ w_in
    q = rope(h[..., :qd].reshape(b, s, 2 * D_HEADS, D_SUB), ROT_DIM).reshape(b, s, D_HEADS, 2, D_SUB)
    k = rope(h[..., qd:2 * qd].reshape(b, s, 2 * D_HEADS, D_SUB), ROT_DIM).reshape(b, s, D_HEADS, 2, D_SUB)
    v = h[..., 2 * qd:].reshape(b, s, D_HEADS, 2 * D_SUB)
    lam_init = 0.8 - 0.6 * math.exp(-0.3 * layer_idx)
    lam = (jnp.exp(jnp.sum(lq1.astype(jnp.float32) * lk1.astype(jnp.float32)))
           - jnp.exp(jnp.sum(lq2.astype(jnp.float32) * lk2.astype(jnp.float32))) + lam_init)
    nb = s // Q_BLOCK
    qb = q.reshape(b, nb, Q_BLOCK, D_HEADS, 2, D_SUB).transpose(1, 0, 2, 3, 4, 5)
    kpos = jnp.arange(s)
    scale = D_SUB ** -0.5

    def block(args):
        qi, blk = args
        sc = jnp.einsum('bqhcd,bkhcd->cbhqk', qi, k).astype(jnp.float32) * scale
        qpos = blk * Q_BLOCK + jnp.arange(Q_BLOCK)
        sc = jnp.where(kpos[None, :] <= qpos[:, None], sc, -jnp.inf)
        p = jax.nn.softmax(sc, axis=-1)
        a = p[0] - lam * p[1]
        return jnp.einsum('bhqk,bkhe->bqhe', a.astype(v.dtype), v)

    o = lax.map(block, (qb, jnp.arange(nb))).transpose(1, 0, 2, 3, 4).reshape(b, s, D_HEADS, 2 * D_SUB)
    o = rms_norm(o, subln) * (1.0 - lam_init)
    return o.reshape(b, s, qd) @ w_o


def hier_moe(x, w_group, w_expert, w_gate, w_up, w_down):
    b, s, d = x.shape
    n_tok = b * s
    xt = x.reshape(n_tok, d)
    p_group = jax.nn.softmax((xt @ w_group).astype(jnp.float32), axis=-1)
    g_w, g_idx = lax.top_k(p_group, 1)
    e_logits = (xt @ w_expert).astype(jnp.float32).reshape(n_tok, M_GROUPS, M_PER_GROUP)
    e_logits = e_logits[jnp.arange(n_tok), g_idx[:, 0]]
    e_w, e_idx = lax.top_k(jax.nn.softmax(e_logits, axis=-1), M_TOPK)
    weight = g_w * e_w / jnp.sum(e_w, -1, keepdims=True)
    expert = (g_idx * M_PER_GROUP + e_idx).reshape(-1)
    token = jnp.repeat(jnp.arange(n_tok), M_TOPK)
    order = jnp.argsort(expert)
    tok_sorted = token[order]
    sizes = jnp.bincount(expert, length=M_EXPERTS).astype(jnp.int32)
    xs = xt[tok_sorted]
    hid = jax.nn.silu(lax.ragged_dot(xs, w_gate, sizes)) * lax.ragged_dot(xs, w_up, sizes)
    ys = lax.ragged_dot(hid, w_down, sizes) * weight.reshape(-1)[order][:, None].astype(x.dtype)
    return jax.ops.segment_sum(ys, tok_sorted, num_segments=n_tok).reshape(b, s, d)


def setup_inputs(seed: int = 0) -> dict:
    key = jax.random.key(seed)
    keys = iter(jax.random.split(key, 40))
    beta = (8 * DEPTH) ** -0.25
    n_a, n_b, n_c, n_d = (len(range(m, DEPTH, N_MIXERS)) for m in range(N_MIXERS))
    d = D_MODEL

    def normal(shape, scale):
        return jax.random.normal(next(keys), shape, jnp.float32) * scale

    def gain(shape):
        return 1.0 + normal(shape, 0.05)

    a_cols = (A_HEADS + 2 * A_KV_HEADS) * HEAD_DIM
    b_cols = B_Q_RANK + B_KV_RANK + B_ROPE
    c_cols = (C_HEADS + 6 * C_KV_HEADS) * HEAD_DIM + 3 * C_HEADS
    d_cols = 3 * D_HEADS * 2 * D_SUB
    cmp_in = C_CMP_LEN * HEAD_DIM
    return {
        'x': normal((BATCH, SEQ, d), 1.0),
        'a_w_in': normal((n_a, d, a_cols), d ** -0.5),
        'a_sinks': normal((n_a, A_HEADS), 1.0),
        'a_w_o': normal((n_a, A_HEADS * HEAD_DIM, d), (A_HEADS * HEAD_DIM) ** -0.5 * beta),
        'b_w_down': normal((n_b, d, b_cols), d ** -0.5),
        'b_q_norm': gain((n_b, B_Q_RANK)),
        'b_kv_norm': gain((n_b, B_KV_RANK)),
        'b_w_uq': normal((n_b, B_Q_RANK, B_HEADS * (B_NOPE + B_ROPE)), B_Q_RANK ** -0.5),
        'b_w_ukv': normal((n_b, B_KV_RANK, B_HEADS * (B_NOPE + B_V)), B_KV_RANK ** -0.5),
        'b_w_o': normal((n_b, B_HEADS * B_V, d), (B_HEADS * B_V) ** -0.5 * beta),
        'c_w_in': normal((n_c, d, c_cols), d ** -0.5),
        'c_pos_k': normal((n_c, C_CMP_LEN, HEAD_DIM), 0.1),
        'c_pos_v': normal((n_c, C_CMP_LEN, HEAD_DIM), 0.1),
        'c_wk1': normal((n_c, cmp_in, C_CMP_HIDDEN), cmp_in ** -0.5),
        'c_wk2': normal((n_c, C_CMP_HIDDEN, HEAD_DIM), C_CMP_HIDDEN ** -0.5),
        'c_wv1': normal((n_c, cmp_in, C_CMP_HIDDEN), cmp_in ** -0.5),
        'c_wv2': normal((n_c, C_CMP_HIDDEN, HEAD_DIM), C_CMP_HIDDEN ** -0.5),
        'c_w_o': normal((n_c, C_HEADS * HEAD_DIM, d), (C_HEADS * HEAD_DIM) ** -0.5 * beta),
        'd_w_in': normal((n_d, d, d_cols), d ** -0.5),
        'd_lq1': normal((n_d, D_SUB), 0.1),
        'd_lk1': normal((n_d, D_SUB), 0.1),
        'd_lq2': normal((n_d, D_SUB), 0.1),
        'd_lk2': normal((n_d, D_SUB), 0.1),
        'd_subln': gain((n_d, 2 * D_SUB)),
        'd_w_o': normal((n_d, D_HEADS * 2 * D_SUB, d), (D_HEADS * 2 * D_SUB) ** -0.5 * beta),
        'moe_w_group': normal((DEPTH, d, M_GROUPS), d ** -0.5),
        'moe_w_expert': normal((DEPTH, d, M_EXPERTS), d ** -0.5),
        'moe_w_gate': normal((DEPTH, M_EXPERTS, d, M_HIDDEN), d ** -0.5),
        'moe_w_up': normal((DEPTH, M_EXPERTS, d, M_HIDDEN), d ** -0.5),
        'moe_w_down': normal((DEPTH, M_EXPERTS, M_HIDDEN, d), M_HIDDEN ** -0.5 * beta),
        'ln_g': gain((DEPTH, 2, d)),
        'ln_b': normal((DEPTH, 2, d), 0.02),
    }


def reference(x, a_w_in, a_sinks, a_w_o,
              b_w_down, b_q_norm, b_kv_norm, b_w_uq, b_w_ukv, b_w_o,
              c_w_in, c_pos_k, c_pos_v, c_wk1, c_wk2, c_wv1, c_wv2, c_w_o,
              d_w_in, d_lq1, d_lk1, d_lq2, d_lk2, d_subln, d_w_o,
              moe_w_group, moe_w_expert, moe_w_gate, moe_w_up, moe_w_down,
              ln_g, ln_b):
    alpha = (2 * DEPTH) ** 0.25
    h = x
    for i in range(DEPTH):
        kind, j = i % N_MIXERS, i // N_MIXERS
        if kind == 0:
            y = swa_sink_gqa(h, a_w_in[j], a_sinks[j], a_w_o[j])
        elif kind == 1:
            y = mla(h, b_w_down[j], b_q_norm[j], b_kv_norm[j], b_w_uq[j], b_w_ukv[j], b_w_o[j])
        elif kind == 2:
            y = nsa(h, c_w_in[j], c_pos_k[j], c_pos_v[j], c_wk1[j], c_wk2[j], c_wv1[j], c_wv2[j], c_w_o[j])
        else:
            y = diff_attention(h, d_w_in[j], d_lq1[j], d_lk1[j], d_lq2[j], d_lk2[j], d_subln[j], d_w_o[j], i)
        h = layer_norm(alpha * h + y, ln_g[i, 0], ln_b[i, 0])
        y = hier_moe(h, moe_w_group[i], moe_w_expert[i], moe_w_gate[i], moe_w_up[i], moe_w_down[i])
        h = layer_norm(alpha * h + y, ln_g[i, 1], ln_b[i, 1])
    return h
```

```python
import os
import numpy as np
from contextlib import ExitStack
import concourse.bass as bass
import concourse.mybir as mybir
from concourse.bass_utils import run_bass_kernel_spmd

F32 = mybir.dt.float32
BF16 = mybir.dt.bfloat16
AF = mybir.ActivationFunctionType
ALU = mybir.AluOpType
AX = mybir.AxisListType

S = 4096
D = 1024
NT = S // 128
KC = D // 128
DEPTH = 4
ALPHA = (2 * DEPTH) ** 0.25
LN_EPS = 1e-5
RMS_EPS = 1e-6
NEG = -30000.0
KSTOP = int(os.environ.get('KSTOP', '0'))
ATT_N = int(os.environ.get('ATT_N', '32'))
ATT_FIN = int(os.environ.get('ATT_FIN', '3'))
ATT_VAR = int(os.environ.get('ATT_VAR', '0'))
EPI_LVL = int(os.environ.get('EPI_LVL', '3'))
EPI_NT = int(os.environ.get('EPI_NT', '32'))
EPI_R = int(os.environ.get('EPI_R', '9'))


class Tok:
    __slots__ = ("w", "r", "lane")

    def __init__(self):
        self.w = None
        self.r = {}
        self.lane = None


class KB:
    def __init__(self, nc, es):
        self.nc = nc
        self.E = dict(pe=nc.tensor, act=nc.scalar, dve=nc.vector, pool=nc.gpsimd, sp=nc.sync)
        self.sem = {k: es.enter_context(nc.semaphore("s_" + k)) for k in self.E}
        self.cnt = {k: 0 for k in self.E}
        self.seen = {k: {} for k in self.E}
        self.es = es
        self.lanes = []
        self.free_lanes = {'hw': [], 'sw': []}
        self.pe_pending = []
        self.n_inst = 0
        self.log = {k: [] for k in self.E}

    def _deps(self, e, reads, writes, dma=False):
        need = {}

        def add(d, raw):
            if d is None:
                return
            key, sem, val = d
            if key == e and not dma and e == "pe":
                return
            cur = need.get(key)
            if cur is None or cur[1] < val:
                need[key] = (sem, val)

        for t in reads:
            add(t.w, True)
        for t in writes:
            add(t.w, False)
            for d in t.r.values():
                add(d, False)
        return need

    def _wait(self, e, need):
        seen = self.seen[e]
        for key, (sem, val) in need.items():
            if seen.get(key, 0) >= val:
                continue
            self.E[e].wait_ge(sem, val)
            self.log[e].append(('w', key, val))
            self.n_inst += 1
            seen[key] = val

    def _commit(self, e, ins, reads, writes):
        self.cnt[e] += 1
        ins.then_inc(self.sem[e], 1)
        self.log[e].append(('i', e, 1))
        d = (e, self.sem[e], self.cnt[e])
        for t in reads:
            t.r[e] = d
        for t in writes:
            t.w = d
            t.r = {}

    def op(self, e, fn, reads=(), writes=()):
        self._wait(e, self._deps(e, reads, writes))
        ins = fn(self.E[e])
        self.n_inst += 1
        if e == "pe":
            self._commit(e, ins, list(reads) + self.pe_pending, writes)
            self.pe_pending = []
        else:
            self._commit(e, ins, reads, writes)
        return ins

    def mm(self, out_tok, out, lhsT, rhs, start, stop, reads, **kw):
        self._wait("pe", self._deps("pe", reads, [out_tok]))
        ins = self.nc.tensor.matmul(out, lhsT=lhsT, rhs=rhs, start=start, stop=stop, **kw)
        self.n_inst += 1
        self.pe_pending.extend(reads)
        if stop:
            self._commit("pe", ins, self.pe_pending, [out_tok])
            self.pe_pending = []
        return ins

    def _lane(self, tok, q):
        kind = "sw" if q == "pool" else "hw"
        if tok.lane is None:
            tok.lane = {}
        if kind not in tok.lane:
            fl = self.free_lanes[kind]
            if fl:
                tok.lane[kind] = fl.pop()
            else:
                i = len(self.lanes)
                sem = self.es.enter_context(self.nc.semaphore("l%d" % i))
                ln = [sem, 0, "L%d" % i, kind]
                self.lanes.append(ln)
                tok.lane[kind] = ln
        return tok.lane[kind]

    def dma(self, q, out, in_, sb_tok, load, extra_reads=(), extra_writes=(), **kw):
        reads = list(extra_reads) + ([] if load else [sb_tok])
        writes = list(extra_writes) + ([sb_tok] if load else [])
        self._wait(q, self._deps(q, reads, writes, dma=True))
        ln = self._lane(sb_tok, q)
        ins = self.E[q].dma_start(out=out, in_=in_, **kw)
        self.n_inst += 1
        ln[1] += 16
        ins.then_inc(ln[0], 16)
        self.log[q].append(('i', ln[2], 16))
        d = (ln[2], ln[0], ln[1])
        for t in reads:
            t.r[d[0]] = d
        for t in writes:
            t.w = d
            t.r = {}
        return ins

    def barrier(self, toks=()):
        for e in self.E:
            need = {}
            for p in self.E:
                if p != e and self.cnt[p] > 0:
                    need[p] = (self.sem[p], self.cnt[p])
            for ln in self.lanes:
                if ln[1] > 0:
                    need[ln[2]] = (ln[0], ln[1])
            self._wait(e, need)
        self.free_lanes = {'hw': [l for l in self.lanes if l[3] == 'hw'], 'sw': [l for l in self.lanes if l[3] == 'sw']}

    def final_wait(self):
        self.barrier()


class Ctx:
    pass


_UNIQ = [0]


def sb(nc, es, name, shape, dt):
    _UNIQ[0] += 1
    return es.enter_context(nc.sbuf_tensor("%s_%d" % (name, _UNIQ[0]), list(shape), dt))


def ps(nc, es, name, shape, dt=F32):
    _UNIQ[0] += 1
    return es.enter_context(nc.psum_tensor("%s_%d" % (name, _UNIQ[0]), list(shape), dt))


def phase_transpose_in(kb, c, x, hT_dst):
    nc = kb.nc
    with ExitStack() as es:
        xt = [(sb(nc, es, "p0x%d" % i, [128, D], F32), Tok()) for i in range(2)]
        hb = [(sb(nc, es, "p0h%d" % i, [128, KC, 128], BF16), Tok()) for i in range(2)]
        pt = [(ps(nc, es, "p0t%d" % i, [128, D]), Tok()) for i in range(2)]
        hT3 = hT_dst.rearrange("(c p) t -> p c t", p=128)
        for tt in range(NT):
            xb, xk = xt[tt % 2]
            hbb, hk = hb[tt % 2]
            pb, pk = pt[tt % 2]
            kb.dma("sp", xb[:], x[tt * 128:(tt + 1) * 128, :], xk, True)
            for cc in range(KC):
                kb.op("pe", lambda pe, cc=cc: pe.transpose(out=pb[:, cc * 128:(cc + 1) * 128],
                                                         in_=xb[:, cc * 128:(cc + 1) * 128], identity=c.ident[:]),
                      reads=[xk, c.ident_k], writes=[pk])
            kb.op("act", lambda a: a.activation(out=hbb[:].rearrange("p c t -> p (c t)"), in_=pb[:], func=AF.Copy),
                  reads=[pk], writes=[hk])
            kb.dma("sp", hT3[:, :, tt * 128:(tt + 1) * 128], hbb[:], hk, False)
        kb.barrier()


class Epilogue:
    def __init__(self, kb, c, es, src_h, dst_h, dst_hT, ln_g, ln_b, router=None, rw_dst=None, n_ps_t=1):
        nc = kb.nc
        self.kb, self.c = kb, c
        self.src_h, self.dst_h, self.dst_hT = src_h, dst_h, dst_hT
        self.router = router
        self.rw_dst = rw_dst
        self.G = sb(nc, es, "epG", [128, D], F32)
        self.B = sb(nc, es, "epB", [128, D], F32)
        self.gb_k = Tok()
        kb.dma("sp", self.G[:], ln_g.partition_broadcast(128), self.gb_k, True)
        kb.dma("sp", self.B[:], ln_b.partition_broadcast(128), self.gb_k, True)
        self.ht = [(sb(nc, es, "epht%d" % i, [128, D], F32), Tok()) for i in range(2)]
        self.z = [(sb(nc, es, "epz%d" % i, [128, D], F32), Tok()) for i in range(2)]
        self.zn = [(sb(nc, es, "epzn%d" % i, [128, D], F32), Tok()) for i in range(2)]
        self.st = [(sb(nc, es, "epst%d" % i, [128, 2, 6], F32), Tok()) for i in range(2)]
        self.mv = [(sb(nc, es, "epmv%d" % i, [128, 4], F32), Tok()) for i in range(2)]
        self.pt = [(ps(nc, es, "eppt%d" % i, [128, D]), Tok()) for i in range(n_ps_t)]
        if dst_hT is not None:
            self.hb = [(sb(nc, es, "ephb%d" % i, [128, KC, 128], BF16), Tok()) for i in range(2)]
            self.hT3 = dst_hT.rearrange("(c p) t -> p c t", p=128)
        if router is not None:
            self.wr = sb(nc, es, "epwr", [128, KC, 36], F32)
            self.wr_k = Tok()
            wg, we = router
            kb.dma("sp", self.wr[:, :, 0:4], wg.rearrange("(c p) n -> p c n", p=128), self.wr_k, True)
            kb.dma("sp", self.wr[:, :, 4:36], we.rearrange("(c p) n -> p c n", p=128), self.wr_k, True)
            self.wrh = sb(nc, es, "epwrh", [128, KC, 36], BF16)
            self.wrl = sb(nc, es, "epwrl", [128, KC, 36], BF16)
            self.wrh_k = Tok()
            kb.op("dve", lambda v: v.tensor_copy(out=self.wrh[:], in_=self.wr[:]), reads=[self.wr_k], writes=[self.wrh_k])
            kb.op("dve", lambda v: v.tensor_tensor(out=self.wrl[:], in0=self.wr[:], in1=self.wrh[:], op=ALU.subtract),
                  reads=[self.wr_k, self.wrh_k], writes=[self.wrh_k])
            self.hlo = [(sb(nc, es, "ephlo%d" % i, [128, KC, 128], BF16), Tok()) for i in range(2)]
            self.pr = (ps(nc, es, "eppr", [128, 512]), Tok())
            self.rt = [(sb(nc, es, "eprt%d" % i, [128, 160], F32), Tok()) for i in range(2)]
            self.rw = [(sb(nc, es, "eprw%d" % i, [128, 32], F32), Tok()) for i in range(2)]
        self.n = 0

    def prefetch(self, tt):
        hb_, hk = self.ht[tt % 2]
        self.kb.dma("sp", hb_[:], self.src_h[tt * 128:(tt + 1) * 128, :], hk, True)

    def run(self, tt, y_ap, y_tok, prefetched=False):
        kb, c = self.kb, self.c
        i = self.n % 2
        self.n += 1
        if not prefetched:
            self.prefetch(tt)
        hb_, hk = self.ht[tt % 2]
        z, zk = self.z[i]
        zn, znk = self.zn[i]
        st, stk = self.st[i]
        mv, mvk = self.mv[i]
        kb.op("dve", lambda v: v.scalar_tensor_tensor(out=z[:], in0=hb_[:], scalar=ALPHA, in1=y_ap,
                                                      op0=ALU.mult, op1=ALU.add), reads=[hk, y_tok], writes=[zk])
        kb.op("dve", lambda v: v.bn_stats(out=st[:, 0, :], in_=z[:, 0:512]), reads=[zk], writes=[stk])
        kb.op("dve", lambda v: v.bn_stats(out=st[:, 1, :], in_=z[:, 512:1024]), reads=[zk], writes=[stk])
        kb.op("dve", lambda v: v.bn_aggr(out=mv[:, 0:2], in_=st[:].rearrange("p a b -> p (a b)")),
              reads=[stk], writes=[mvk])
        kb.op("dve", lambda v: v.tensor_scalar(out=mv[:, 2:3], in0=mv[:, 1:2], scalar1=LN_EPS, scalar2=None,
                                               op0=ALU.add), reads=[mvk], writes=[mvk])
        kb.op("act", lambda a: a.activation(out=mv[:, 2:3], in_=mv[:, 2:3], func=AF.Sqrt), reads=[mvk], writes=[mvk])
        kb.op("dve", lambda v: v.reciprocal(out=mv[:, 2:3], in_=mv[:, 2:3]), reads=[mvk], writes=[mvk])
        kb.op("dve", lambda v: v.scalar_tensor_tensor(out=mv[:, 3:4], in0=mv[:, 0:1], scalar=-1.0, in1=mv[:, 2:3],
                                                      op0=ALU.mult, op1=ALU.mult), reads=[mvk], writes=[mvk])
        kb.op("act", lambda a: a.activation(out=zn[:], in_=z[:], func=AF.Identity, scale=mv[:, 2:3], bias=mv[:, 3:4]),
              reads=[zk, mvk], writes=[znk])
        kb.op("pool", lambda g: g.tensor_tensor(out=zn[:], in0=zn[:], in1=self.G[:], op=ALU.mult),
              reads=[znk, self.gb_k], writes=[znk])
        kb.op("pool", lambda g: g.tensor_tensor(out=zn[:], in0=zn[:], in1=self.B[:], op=ALU.add),
              reads=[znk, self.gb_k], writes=[znk])
        kb.dma("sp", self.dst_h[tt * 128:(tt + 1) * 128, :], zn[:], znk, False)
        if self.dst_hT is None or EPI_LVL < 2:
            return
        pb, pk = self.pt[self.n % len(self.pt)]
        for cc in range(KC):
            kb.op("pe", lambda pe, cc=cc: pe.transpose(out=pb[:, cc * 128:(cc + 1) * 128],
                                                     in_=zn[:, cc * 128:(cc + 1) * 128], identity=c.ident[:]),
                  reads=[znk, c.ident_k], writes=[pk])
        hbb, hbk = self.hb[i]
        kb.op("act", lambda a: a.activation(out=hbb[:].rearrange("p c t -> p (c t)"), in_=pb[:], func=AF.Copy),
              reads=[pk], writes=[hbk])
        kb.dma("sp", self.hT3[:, :, tt * 128:(tt + 1) * 128], hbb[:], hbk, False)
        if self.router is None or EPI_LVL < 3:
            return
        hlo, hlok = self.hlo[i]
        kb.op("dve", lambda v: v.tensor_tensor(out=hlo[:].rearrange("p c t -> p (c t)"), in0=pb[:],
                                               in1=hbb[:].rearrange("p c t -> p (c t)"), op=ALU.subtract),
              reads=[pk, hbk], writes=[hlok])
        pr, prk = self.pr
        n_mm = 0
        for cc in range(KC):
            for (lh, lk, rh) in [(hbb, hbk, self.wrh), (hlo, hlok, self.wrh), (hbb, hbk, self.wrl)]:
                kb.mm(prk, pr[:, 0:36], lh[:, cc, :], rh[:, cc, :], n_mm == 0, n_mm == 3 * KC - 1, [lk, self.wrh_k])
                n_mm += 1
        self._route(tt, i)

    def _route(self, tt, i):
        kb = self.kb
        pr, prk = self.pr
        rt, rtk = self.rt[i]
        rw, rwk = self.rw[i]
        L = rt[:, 0:36]
        gmax, ngmax, gsum, gw = rt[:, 36:37], rt[:, 37:38], rt[:, 38:39], rt[:, 39:40]
        ge, gmask, pen = rt[:, 40:44], rt[:, 44:48], rt[:, 48:52]
        ml, mask1, ml2 = rt[:, 52:84], rt[:, 84:116], rt[:, 116:148]
        RK = [rtk]

        def dv(fn, reads=RK, writes=RK):
            kb.op("dve", fn, reads=reads, writes=writes)

        kb.op("act", lambda a: a.activation(out=L, in_=pr[:, 0:36], func=AF.Copy), reads=[prk], writes=[rtk])
        if EPI_R <= 1:
            return
        dv(lambda v: v.tensor_reduce(out=gmax, in_=rt[:, 0:4], axis=AX.X, op=ALU.max))
        dv(lambda v: v.tensor_scalar(out=ngmax, in0=gmax, scalar1=-1.0, scalar2=None, op0=ALU.mult))
        kb.op("act", lambda a: a.activation(out=ge, in_=rt[:, 0:4], func=AF.Exp, bias=ngmax, scale=1.0, accum_out=gsum),
              reads=RK, writes=RK)
        dv(lambda v: v.reciprocal(out=gw, in_=gsum))
        if EPI_R <= 2:
            return
        dv(lambda v: v.tensor_scalar(out=gmask, in0=rt[:, 0:4], scalar1=gmax, scalar2=None, op0=ALU.is_ge))
        dv(lambda v: v.tensor_scalar(out=pen, in0=gmask, scalar1=1e30, scalar2=-1e30, op0=ALU.mult, op1=ALU.add))
        dv(lambda v: v.tensor_tensor(out=ml.rearrange("p (g e) -> p g e", g=4),
                                     in0=rt[:, 4:36].rearrange("p (g e) -> p g e", g=4),
                                     in1=pen.unsqueeze(2).to_broadcast([128, 4, 8]), op=ALU.add))
        s_m1, s_m2, s_nm1, s_r, s_w1, s_w2, s_den = (rt[:, 40:41], rt[:, 41:42], rt[:, 42:43], rt[:, 43:44],
                                                     rt[:, 44:45], rt[:, 45:46], rt[:, 46:47])
        if EPI_R <= 3:
            return
        dv(lambda v: v.tensor_reduce(out=s_m1, in_=ml, axis=AX.X, op=ALU.max))
        dv(lambda v: v.tensor_scalar(out=mask1, in0=ml, scalar1=s_m1, scalar2=None, op0=ALU.is_ge))
        dv(lambda v: v.scalar_tensor_tensor(out=ml2, in0=mask1, scalar=-1e30, in1=ml, op0=ALU.mult, op1=ALU.add))
        dv(lambda v: v.tensor_reduce(out=s_m2, in_=ml2, axis=AX.X, op=ALU.max))
        dv(lambda v: v.tensor_scalar(out=ml, in0=ml2, scalar1=s_m2, scalar2=None, op0=ALU.is_ge))
        dv(lambda v: v.tensor_scalar(out=s_nm1, in0=s_m1, scalar1=-1.0, scalar2=None, op0=ALU.mult))
        kb.op("act", lambda a: a.activation(out=s_r, in_=s_m2, func=AF.Exp, bias=s_nm1, scale=1.0), reads=RK, writes=RK)
        dv(lambda v: v.tensor_scalar(out=s_den, in0=s_r, scalar1=1.0, scalar2=None, op0=ALU.add))
        dv(lambda v: v.reciprocal(out=s_den, in_=s_den))
        dv(lambda v: v.tensor_tensor(out=s_w1, in0=s_den, in1=gw, op=ALU.mult))
        dv(lambda v: v.tensor_tensor(out=s_w2, in0=s_w1, in1=s_r, op=ALU.mult))
        dv(lambda v: v.tensor_scalar(out=mask1, in0=mask1, scalar1=s_w1, scalar2=None, op0=ALU.mult))
        kb.op("dve", lambda v: v.scalar_tensor_tensor(out=rw[:], in0=ml, scalar=s_w2, in1=mask1, op0=ALU.mult, op1=ALU.add),
              reads=RK, writes=[rwk])
        kb.dma("sp", self.rw_dst[tt * 128:(tt + 1) * 128, :], rw[:], rwk, False)


def phase_moe(kb, c, hmid, hT_src, rw_src, w_gate, w_up, w_down, ln_g, ln_b, dst_h, dst_hT):
    nc = kb.nc
    GT = 8
    GN = GT * 128
    with ExitStack() as es:
        epi = Epilogue(kb, c, es, hmid, dst_h, dst_hT, ln_g, ln_b)
        hTg = (sb(nc, es, "mhT", [128, KC, GN], BF16), Tok())
        rwg = (sb(nc, es, "mrw", [128, GT, 32], F32), Tok())
        yacc = [(sb(nc, es, "myacc%d" % t, [128, D], F32), Tok()) for t in range(GT)]
        wg = [(sb(nc, es, "mwg%d" % i, [128, KC, 512], BF16), Tok()) for i in range(2)]
        wu = [(sb(nc, es, "mwu%d" % i, [128, KC, 512], BF16), Tok()) for i in range(2)]
        wd = [(sb(nc, es, "mwd%d" % i, [128, 4, D], BF16), Tok()) for i in range(2)]
        sg = [(sb(nc, es, "msg%d" % i, [128, 512], F32), Tok()) for i in range(2)]
        h1 = [(sb(nc, es, "mh1%d" % i, [128, 4, 512], BF16), Tok()) for i in range(2)]
        pg = [(ps(nc, es, "mpg%d" % i, [128, 512]), Tok()) for i in range(2)]
        pu = [(ps(nc, es, "mpu%d" % i, [128, 512]), Tok()) for i in range(2)]
        pd = [(ps(nc, es, "mpd%d" % i, [128, 512]), Tok()) for i in range(2)]
        hT3 = hT_src.rearrange("(c p) t -> p c t", p=128)
        n_e = 32
        seq = [(g, e) for g in range(S // GN) for e in range(n_e)]

        def load_w(idx):
            g, e = seq[idx]
            b = idx % 2
            kb.dma("pool", wg[b][0][:], w_gate[e].rearrange("(c p) n -> p c n", p=128), wg[b][1], True)
            kb.dma("pool", wu[b][0][:], w_up[e].rearrange("(c p) n -> p c n", p=128), wu[b][1], True)
            kb.dma("pool", wd[b][0][:], w_down[e].rearrange("(c p) n -> p c n", p=128), wd[b][1], True)

        load_w(0)
        cnt_gu = 0
        cnt_d = 0
        cnt_h1 = 0
        for idx, (g, e) in enumerate(seq):
            if e == 0:
                kb.dma("sp", hTg[0][:], hT3[:, :, g * GN:(g + 1) * GN], hTg[1], True)
                kb.dma("sp", rwg[0][:], rw_src[g * GN:(g + 1) * GN, :].rearrange("(t p) e -> p t e", p=128), rwg[1], True)
            if idx + 1 < len(seq):
                load_w(idx + 1)
            b = idx % 2
            wgb, wgk = wg[b]
            wub, wuk = wu[b]
            wdb, wdk = wd[b]
            for s_ in range(GN // 512):
                h1b, h1k = h1[cnt_h1 % 2]
                cnt_h1 += 1
                for hc in range(4):
                    pgb, pgk = pg[cnt_gu % 2]
                    pub, puk = pu[cnt_gu % 2]
                    sgb, sgk = sg[cnt_gu % 2]
                    cnt_gu += 1
                    for kc in range(KC):
                        kb.mm(pgk, pgb[:], wgb[:, kc, hc * 128:(hc + 1) * 128], hTg[0][:, kc, s_ * 512:(s_ + 1) * 512],
                              kc == 0, kc == KC - 1, [wgk, hTg[1]])
                    for kc in range(KC):
                        kb.mm(puk, pub[:], wub[:, kc, hc * 128:(hc + 1) * 128], hTg[0][:, kc, s_ * 512:(s_ + 1) * 512],
                              kc == 0, kc == KC - 1, [wuk, hTg[1]])
                    kb.op("act", lambda a: a.activation(out=sgb[:], in_=pgb[:], func=AF.Silu), reads=[pgk], writes=[sgk])
                    kb.op("dve", lambda v, hc=hc: v.tensor_tensor(out=h1b[:, hc, :], in0=sgb[:], in1=pub[:], op=ALU.mult),
                          reads=[sgk, puk], writes=[h1k])
                for t4 in range(4):
                    tl = s_ * 4 + t4
                    ya, yk = yacc[tl]
                    for half in range(2):
                        pdb, pdk = pd[cnt_d % 2]
                        cnt_d += 1
                        for hc in range(4):
                            kb.mm(pdk, pdb[:], h1b[:, hc, t4 * 128:(t4 + 1) * 128], wdb[:, hc, half * 512:(half + 1) * 512],
                                  hc == 0, hc == 3, [h1k, wdk])
                        wcol = rwg[0][:, tl, e:e + 1]
                        if e == 0:
                            kb.op("dve", lambda v, half=half, wcol=wcol, pdb=pdb, ya=ya: v.tensor_scalar(
                                out=ya[:, half * 512:(half + 1) * 512], in0=pdb[:], scalar1=wcol, scalar2=None, op0=ALU.mult),
                                reads=[pdk, rwg[1]], writes=[yk])
                        else:
                            kb.op("dve", lambda v, half=half, wcol=wcol, pdb=pdb, ya=ya: v.scalar_tensor_tensor(
                                out=ya[:, half * 512:(half + 1) * 512], in0=pdb[:], scalar=wcol,
                                in1=ya[:, half * 512:(half + 1) * 512], op0=ALU.mult, op1=ALU.add),
                                reads=[pdk, rwg[1], yk], writes=[yk])
            if e == n_e - 1:
                for tl in range(GT):
                    epi.run(g * GT + tl, yacc[tl][0][:], yacc[tl][1])
        kb.barrier()


def load_rope(kb, c, es, dram_tab):
    nc = kb.nc
    cos = sb(nc, es, "ropec", [128, S], F32)
    sin = sb(nc, es, "ropes", [128, S], F32)
    k = Tok()
    kb.dma("sp", cos[:], dram_tab[0], k, True)
    kb.dma("sp", sin[:], dram_tab[1], k, True)
    return cos, sin, k


def make_partner(kb, wsrc, wk_, wdst, wdk, ncols, rot_half, head_dim):
    nh = ncols // head_dim
    kb.op("pool", lambda g: g.tensor_copy(out=wdst[:, :, 0:ncols], in_=wsrc[:, :, 0:ncols]), reads=[wk_], writes=[wdk])
    for kc in range(KC):
        sv = wsrc[:, kc, 0:ncols].rearrange("p (h d) -> p h d", d=head_dim)
        dv = wdst[:, kc, 0:ncols].rearrange("p (h d) -> p h d", d=head_dim)
        kb.op("pool", lambda g, sv=sv, dv=dv: g.tensor_copy(out=dv[:, :, 0:rot_half], in_=sv[:, :, rot_half:2 * rot_half]),
              reads=[wk_], writes=[wdk])
        kb.op("pool", lambda g, sv=sv, dv=dv: g.tensor_copy(out=dv[:, :, rot_half:2 * rot_half], in_=sv[:, :, 0:rot_half]),
              reads=[wk_], writes=[wdk])


def load_half_weight(kb, dst, dk_, w3, col0, half, ncols=64):
    kb.op("pool", lambda g: g.memset(dst[:], 0.0), writes=[dk_])
    kb.dma("pool", dst[:, :, half * 64:half * 64 + ncols], w3[:, :, col0:col0 + ncols], dk_, True)


def proj_rope(kb, w, wk_, wp, wpk, col0, hblk, hblk_k, pq, pqp, cos_blk, sin_blk, tab_k, tmp1, tmp2, out_ap, out_k, nrows=128):
    for kc in range(KC):
        kb.mm(pq[1], pq[0][0:nrows, :], w[:, kc, col0:col0 + nrows], hblk[:, kc, :], kc == 0, kc == KC - 1, [wk_, hblk_k])
    for kc in range(KC):
        kb.mm(pqp[1], pqp[0][0:nrows, :], wp[:, kc, col0:col0 + nrows], hblk[:, kc, :], kc == 0, kc == KC - 1, [wpk, hblk_k])
    kb.op("dve", lambda v: v.tensor_tensor(out=tmp1[0][0:nrows, :], in0=pq[0][0:nrows, :], in1=cos_blk, op=ALU.mult),
          reads=[pq[1], tab_k], writes=[tmp1[1]])
    kb.op("dve", lambda v: v.tensor_tensor(out=tmp2[0][0:nrows, :], in0=pqp[0][0:nrows, :], in1=sin_blk, op=ALU.mult),
          reads=[pqp[1], tab_k], writes=[tmp2[1]])
    kb.op("pool", lambda g: g.tensor_tensor(out=out_ap, in0=tmp1[0][0:nrows, :], in1=tmp2[0][0:nrows, :], op=ALU.add),
          reads=[tmp1[1], tmp2[1]], writes=[out_k])


def outproj_epilogue(kb, c, OT, w_o_rows_ap, src_h, dst_h, dst_hT, ln_g, ln_b, router, rw_dst, ot_dram=None):
    nc = kb.nc
    with ExitStack() as es:
        epi = Epilogue(kb, c, es, src_h, dst_h, dst_hT, ln_g, ln_b, router=router, rw_dst=rw_dst)
        wo = (sb(nc, es, "wo", [128, KC, D], BF16), Tok())
        kb.dma("pool", wo[0][:], w_o_rows_ap, wo[1], True)
        py = [(ps(nc, es, "py%d" % i, [128, D]), Tok()) for i in range(2)]
        if ot_dram is not None:
            otb = [(sb(nc, es, "otb%d" % i, [128, KC, 128], BF16), Tok()) for i in range(2)]
            ot3 = ot_dram.rearrange("(c p) t -> p c t", p=128)
            kb.dma("sp", otb[0][0][:], ot3[:, :, 0:128], otb[0][1], True)
        for tt in range(min(NT, EPI_NT)):
            epi.prefetch(tt)
            if ot_dram is not None and tt + 1 < NT:
                kb.dma("sp", otb[(tt + 1) % 2][0][:], ot3[:, :, (tt + 1) * 128:(tt + 2) * 128], otb[(tt + 1) % 2][1], True)
            pyb, pyk = py[tt % 2]
            for half in range(2):
                for cc in range(KC):
                    if ot_dram is not None:
                        lhsT, lk = otb[tt % 2][0][:, cc, :], otb[tt % 2][1]
                    else:
                        lhsT, lk = OT[0][:, cc, tt * 128:(tt + 1) * 128], OT[1]
                    kb.mm(pyk, pyb[:, half * 512:(half + 1) * 512], lhsT,
                          wo[0][:, cc, half * 512:(half + 1) * 512], cc == 0, cc == KC - 1, [lk, wo[1]])
            if EPI_LVL >= 1:
                epi.run(tt, pyb[:], pyk, prefetched=True)
        kb.barrier()


def phase_swa(kb, c, src_h, hT_src, w_in, sinks, w_o, ln_g, ln_b, router, dst_h, dst_hT, rw_dst):
    nc = kb.nc
    hT3 = hT_src.rearrange("(c p) t -> p c t", p=128)
    w3 = w_in.rearrange("(c p) n -> p c n", p=128)
    with ExitStack() as es0:
        OT = (sb(nc, es0, "OT", [128, KC, S], BF16), Tok())
        with ExitStack() as es:
            cos, sin, tab_k = load_rope(kb, c, es, c.rope16)
            hblk = [(sb(nc, es, "hblk%d" % i, [128, KC, 512], BF16), Tok()) for i in range(2)]
            QT = (sb(nc, es, "QT", [128, 2, S], BF16), Tok())
            KT2 = [(sb(nc, es, "KT%d" % i, [128, S], BF16), Tok()) for i in range(2)]
            V = (sb(nc, es, "V", [128, NT, 128], BF16), Tok())
            wq = (sb(nc, es, "wq", [128, KC, 256], BF16), Tok())
            wqp = (sb(nc, es, "wqp", [128, KC, 256], BF16), Tok())
            wk2 = [(sb(nc, es, "wk%d" % i, [128, KC, 128], BF16), Tok()) for i in range(2)]
            wkp2 = [(sb(nc, es, "wkp%d" % i, [128, KC, 128], BF16), Tok()) for i in range(2)]
            wv = (sb(nc, es, "wv", [128, KC, 128], BF16), Tok())
            tmp1 = [(sb(nc, es, "t1_%d" % i, [128, 512], F32), Tok()) for i in range(2)]
            tmp2 = [(sb(nc, es, "t2_%d" % i, [128, 512], F32), Tok()) for i in range(2)]
            es16 = (sb(nc, es, "es16", [128, 16], F32), Tok())
            esx = (sb(nc, es, "esx", [128, 4, 512], F32), Tok())
            PT = [(sb(nc, es, "PT%d" % i, [128, 512], BF16), Tok()) for i in range(3)]
            den = [(sb(nc, es, "den%d" % i, [128, 512], F32), Tok()) for i in range(2)]
            pq = [(ps(nc, es, "pq%d" % i, [128, 512]), Tok()) for i in range(2)]
            pqp = [(ps(nc, es, "pqp%d" % i, [128, 512]), Tok()) for i in range(2)]
            pss = [(ps(nc, es, "pss%d" % i, [128, 512]), Tok()) for i in range(2)]
            pso = (ps(nc, es, "pso", [128, 512]), Tok())
            psm = (ps(nc, es, "psm", [128, 512]), Tok())
            kb.dma("sp", es16[0][:], sinks.partition_broadcast(128), es16[1], True)
            kb.op("act", lambda a: a.activation(out=es16[0][:], in_=es16[0][:], func=AF.Exp), reads=[es16[1]], writes=[es16[1]])
            for hk in range(4):
                kb.op("dve", lambda v, hk=hk: v.tensor_copy(
                    out=esx[0][:, hk, :].rearrange("p (g i) -> p g i", g=4),
                    in_=es16[0][:, 4 * hk:4 * hk + 4].unsqueeze(2).to_broadcast([128, 4, 128])),
                    reads=[es16[1]], writes=[esx[1]])
            nq = 0
            npt = 0
            for hk in range(4 if KSTOP != 5 else 0):
                kb.dma("pool", wq[0][:], w3[:, :, hk * 256:(hk + 1) * 256], wq[1], True)
                for hf in range(2):
                    load_half_weight(kb, wk2[hf][0], wk2[hf][1], w3, 1024 + hk * 64, hf)
                    kb.dma("pool", wv[0][:, :, hf * 64:(hf + 1) * 64], w3[:, :, 1280 + hk * 64:1280 + (hk + 1) * 64], wv[1], True)
                make_partner(kb, wq[0], wq[1], wqp[0], wqp[1], 256, 8, 64)
                for hf in range(2):
                    make_partner(kb, wk2[hf][0], wk2[hf][1], wkp2[hf][0], wkp2[hf][1], 128, 8, 64)
                if KSTOP == 1:
                    kb.barrier(); return
                kb.dma("sp", hblk[0][0][:], hT3[:, :, 0:512], hblk[0][1], True)
                for blk in range(8):
                    if blk + 1 < 8:
                        kb.dma("sp", hblk[(blk + 1) % 2][0][:], hT3[:, :, (blk + 1) * 512:(blk + 2) * 512], hblk[(blk + 1) % 2][1], True)
                    hb_, hbk = hblk[blk % 2]
                    bs = slice(blk * 512, (blk + 1) * 512)
                    for cc in range(2 if KSTOP != 23 else 0):
                        proj_rope(kb, wq[0], wq[1], wqp[0], wqp[1], cc * 128, hb_, hbk, pq[nq % 2], pqp[nq % 2],
                                  cos[:, bs], sin[:, bs], tab_k, tmp1[nq % 2], tmp2[nq % 2], QT[0][:, cc, bs], QT[1])
                        nq += 1
                    if KSTOP == 21:
                        continue
                    for hf in range(2):
                        proj_rope(kb, wk2[hf][0], wk2[hf][1], wkp2[hf][0], wkp2[hf][1], 0, hb_, hbk, pq[nq % 2], pqp[nq % 2],
                                  cos[:, bs], sin[:, bs], tab_k, tmp1[nq % 2], tmp2[nq % 2], KT2[hf][0][:, bs], KT2[hf][1])
                        nq += 1
                    if KSTOP == 22:
                        continue
                    pv = pq[nq % 2]
                    nq += 1
                    for t4 in range(4):
                        for kc in range(KC):
                            kb.mm(pv[1], pv[0][:, t4 * 128:(t4 + 1) * 128], hb_[:, kc, t4 * 128:(t4 + 1) * 128], wv[0][:, kc, :],
                                  kc == 0, kc == KC - 1, [hbk, wv[1]])
                    kb.op("act", lambda a, pv=pv, blk=blk: a.activation(
                        out=V[0][:, blk * 4:(blk + 1) * 4, :].rearrange("p t d -> p (t d)"), in_=pv[0][:], func=AF.Copy),
                        reads=[pv[1]], writes=[V[1]])
                if KSTOP in (2, 21, 22, 23):
                    kb.barrier(); return
                for n in range(min(NT, ATT_N)):
                    kts = ([n - 1] if n > 0 else []) + [n]
                    qs = slice(n * 128, (n + 1) * 128)
                    for ki, kt in enumerate(kts):
                        ks = slice(kt * 128, (kt + 1) * 128)
                        sb_, sk = pss[npt % 2]
                        mb = c.mb_cur if kt == n else c.mb_prev
                        kb.mm(sk, sb_[:], c.identb[:], mb[:], True, False, [c.const_k])
                        for g in range(4):
                            kb.mm(sk, sb_[:, g * 128:(g + 1) * 128], KT2[g % 2][0][:, ks], QT[0][:, g // 2, qs], False, g == 3,
                                  [KT2[g % 2][1], QT[1]])
                        pt_, ptk = PT[npt % 3]
                        npt += 1
                        kb.op("act", lambda a, pt_=pt_, sb_=sb_: a.activation(out=pt_[:], in_=sb_[:], func=AF.Exp, scale=0.125),
                              reads=[sk], writes=[ptk])
                        kb.mm(pso[1], pso[0][:], V[0][:, kt, :], pt_[:], ki == 0, ki == len(kts) - 1, [V[1], ptk])
                        kb.mm(psm[1], psm[0][:], c.onesb[:], pt_[:], ki == 0, ki == len(kts) - 1, [c.const_k, ptk])
                    if ATT_FIN < 1:
                        continue
                    dn, dnk = den[n % 2]
                    kb.op("dve", lambda v, dn=dn, hk=hk: v.tensor_tensor(out=dn[:], in0=psm[0][:], in1=esx[0][:, hk, :], op=ALU.add),
                          reads=[psm[1], esx[1]], writes=[dnk])
                    kb.op("dve", lambda v, dn=dn: v.reciprocal(out=dn[:], in_=dn[:]), reads=[dnk], writes=[dnk])
                    for hf in range(2 if ATT_FIN >= 3 else (1 if ATT_FIN == 2 else 0)):
                        hs = slice(hf * 64, hf * 64 + 64)
                        kb.op("dve", lambda v, dn=dn, hs=hs, hf=hf, hk=hk, qs=qs: v.tensor_tensor(
                            out=OT[0][hs, 2 * hk:2 * hk + 2, qs],
                            in0=pso[0][hs, :].rearrange("p (a b i) -> p a b i", a=2, b=2)[:, :, hf, :],
                            in1=dn[hs, :].rearrange("p (a b i) -> p a b i", a=2, b=2)[:, :, hf, :], op=ALU.mult),
                            reads=[pso[1], dnk], writes=[OT[1]])
                if KSTOP == 3:
                    kb.barrier(); return
            kb.barrier()
            if KSTOP == 4:
                return
        outproj_epilogue(kb, c, OT, w_o.rearrange("(c p) n -> p c n", p=128), src_h, dst_h, dst_hT, ln_g, ln_b, router, rw_dst)


class AttnBufs:
    def __init__(self, kb, es, n_pss=2):
        nc = kb.nc
        self.pss = [(ps(nc, es, "pss%d" % i, [128, 512]), Tok()) for i in range(n_pss)]
        self.pso = (ps(nc, es, "pso", [128, 512]), Tok())
        self.psm = (ps(nc, es, "psm", [128, 512]), Tok())
        self.PT = [(sb(nc, es, "PT%d" % i, [128, 512], BF16), Tok()) for i in range(3)]
        self.n = 0


def causal_attention(kb, c, ab, kt_lhsT, q_rhs, v_lhsT, scale, k_toks, v_toks, on_done, qgroups=range(8), bias_fn=None):
    for qg in qgroups:
        nk = 4 * qg + 4
        for kt in range(nk):
            j = kt - 4 * qg
            lo = max(j, 0) * 128
            R = slice(lo, 512)
            sb_, sk = ab.pss[ab.n % len(ab.pss)]
            pt_, ptk = ab.PT[ab.n % 3]
            ab.n += 1
            first = True
            if j >= 0:
                kb.mm(sk, sb_[:, R], c.identb[:], c.mb_diag[:, 0:512 - lo], True, False, [c.const_k])
                first = False
            if bias_fn is not None:
                first = bias_fn(sk, sb_, kt, qg, lo, first)
            kb.mm(sk, sb_[:, R], kt_lhsT(kt), q_rhs(qg * 512 + lo, (qg + 1) * 512), first, True, k_toks)
            kb.op("act", lambda a, pt_=pt_, sb_=sb_, R=R: a.activation(out=pt_[:, R], in_=sb_[:, R], func=AF.Exp, scale=scale),
                  reads=[sk], writes=[ptk])
            kb.mm(ab.pso[1], ab.pso[0][:, R], v_lhsT(kt), pt_[:, R], kt == 0, kt == nk - 1, list(v_toks) + [ptk])
            kb.mm(ab.psm[1], ab.psm[0][:, R], c.onesb[:], pt_[:, R], kt == 0, kt == nk - 1, [c.const_k, ptk])
        on_done(qg)


def phase_mla(kb, c, src_h, hT_src, w_down, q_norm, kv_norm, w_uq, w_ukv, w_o, ln_g, ln_b, router, dst_h, dst_hT, rw_dst):
    nc = kb.nc
    hT3 = hT_src.rearrange("(c p) t -> p c t", p=128)
    wd3 = w_down.rearrange("(c p) n -> p c n", p=128)
    with ExitStack() as es0:
        OT = (sb(nc, es0, "OT", [128, KC, S], BF16), Tok())
        with ExitStack() as es1:
            cqn = (sb(nc, es1, "cqn", [128, 3, S], BF16), Tok())
            ckvn = (sb(nc, es1, "ckvn", [128, 2, S], BF16), Tok())
            KRT = (sb(nc, es1, "KRT", [128, S], BF16), Tok())
            cosb = sb(nc, es1, "cosb", [128, S], BF16)
            sinb = sb(nc, es1, "sinb", [128, S], BF16)
            tab_k = Tok()
            kb.dma("pool", cosb[:], c.rope32[0], tab_k, True)
            kb.dma("pool", sinb[:], c.rope32[1], tab_k, True)
            with ExitStack() as es:
                hblk = [(sb(nc, es, "hblk%d" % i, [128, KC, 512], BF16), Tok()) for i in range(2)]
                wd = (sb(nc, es, "wd", [128, KC, 640], BF16), Tok())
                wkr = (sb(nc, es, "wkr", [128, KC, 128], BF16), Tok())
                wkrp = (sb(nc, es, "wkrp", [128, KC, 128], BF16), Tok())
                gq = (sb(nc, es, "gq", [128, 5], F32), Tok())
                cf = [(sb(nc, es, "cf%d" % i, [128, 512], F32), Tok()) for i in range(5)]
                sq = [(sb(nc, es, "sq%d" % i, [128, 512], BF16), Tok()) for i in range(2)]
                rs = [(sb(nc, es, "rs%d" % i, [128, 512], F32), Tok()) for i in range(2)]
                t1 = (sb(nc, es, "t1", [128, 512], F32), Tok())
                t2 = (sb(nc, es, "t2", [128, 512], F32), Tok())
                pc = [(ps(nc, es, "pc%d" % i, [128, 512]), Tok()) for i in range(3)]
                pssq = [(ps(nc, es, "pssq%d" % i, [128, 512]), Tok()) for i in range(2)]
                pkr = (ps(nc, es, "pkr", [128, 512]), Tok())
                pkrp = (ps(nc, es, "pkrp", [128, 512]), Tok())
                kb.dma("pool", wd[0][:], wd3[:, :, 0:640], wd[1], True)
                kb.op("pool", lambda g: g.memset(wkr[0][:], 0.0), writes=[wkr[1]])
                kb.op("pool", lambda g: g.memset(wkrp[0][:], 0.0), writes=[wkrp[1]])
                kb.dma("pool", wkr[0][:, :, 64:96], wd3[:, :, 640:672], wkr[1], True)
                kb.dma("pool", wkrp[0][:, :, 64:80], wd3[:, :, 656:672], wkrp[1], True)
                kb.dma("pool", wkrp[0][:, :, 80:96], wd3[:, :, 640:656], wkrp[1], True)
                kb.dma("sp", gq[0][:, 0:3], q_norm.rearrange("o (c p) -> p (o c)", p=128), gq[1], True, allow_slow_non_contiguous=True)
                kb.dma("sp", gq[0][:, 3:5], kv_norm.rearrange("o (c p) -> p (o c)", p=128), gq[1], True, allow_slow_non_contiguous=True)
                kb.dma("sp", hblk[0][0][:], hT3[:, :, 0:512], hblk[0][1], True)
                npc = 0
                for blk in range(8):
                    if blk + 1 < 8:
                        kb.dma("sp", hblk[(blk + 1) % 2][0][:], hT3[:, :, (blk + 1) * 512:(blk + 2) * 512], hblk[(blk + 1) % 2][1], True)
                    hb_, hbk = hblk[blk % 2]
                    bs = slice(blk * 512, (blk + 1) * 512)
                    for grp, (ocs, dst, nrm) in enumerate([((0, 1, 2), cqn, 384.0), ((3, 4), ckvn, 256.0)]):
                        pq_, pqk = pssq[grp]
                        for ii, oc in enumerate(ocs):
                            pcb, pck = pc[npc % 3]
                            npc += 1
                            for kc in range(KC):
                                kb.mm(pck, pcb[:], wd[0][:, kc, oc * 128:(oc + 1) * 128], hb_[:, kc, :], kc == 0, kc == KC - 1, [wd[1], hbk])
                            cfb, cfk = cf[oc]
                            sqb, sqk = sq[npc % 2]
                            kb.op("act", lambda a, cfb=cfb, pcb=pcb: a.activation(out=cfb[:], in_=pcb[:], func=AF.Copy), reads=[pck], writes=[cfk])
                            kb.op("dve", lambda v, sqb=sqb, pcb=pcb, cfb=cfb: v.tensor_tensor(out=sqb[:], in0=pcb[:], in1=cfb[:], op=ALU.mult),
                                  reads=[pck, cfk], writes=[sqk])
                            kb.mm(pqk, pq_[:], c.onesb[:], sqb[:], ii == 0, ii == len(ocs) - 1, [c.const_k, sqk])
                        rsb, rsk = rs[grp]
                        kb.op("dve", lambda v, rsb=rsb, pq_=pq_, nrm=nrm: v.tensor_scalar(out=rsb[:], in0=pq_[:], scalar1=1.0 / nrm, scalar2=RMS_EPS,
                                                                                         op0=ALU.mult, op1=ALU.add), reads=[pqk], writes=[rsk])
                        kb.op("act", lambda a, rsb=rsb: a.activation(out=rsb[:], in_=rsb[:], func=AF.Sqrt), reads=[rsk], writes=[rsk])
                        kb.op("dve", lambda v, rsb=rsb: v.reciprocal(out=rsb[:], in_=rsb[:]), reads=[rsk], writes=[rsk])
                        for ii, oc in enumerate(ocs):
                            cfb, cfk = cf[oc]
                            kb.op("dve", lambda v, cfb=cfb, rsb=rsb, oc=oc, ii=ii, dst=dst: v.scalar_tensor_tensor(
                                out=dst[0][:, ii, bs], in0=cfb[:], scalar=gq[0][:, oc:oc + 1], in1=rsb[:], op0=ALU.mult, op1=ALU.mult),
                                reads=[cfk, rsk, gq[1]], writes=[dst[1]])
                    for kc in range(KC):
                        kb.mm(pkr[1], pkr[0][:], wkr[0][:, kc, :], hb_[:, kc, :], kc == 0, kc == KC - 1, [wkr[1], hbk])
                    for kc in range(KC):
                        kb.mm(pkrp[1], pkrp[0][:], wkrp[0][:, kc, :], hb_[:, kc, :], kc == 0, kc == KC - 1, [wkrp[1], hbk])
                    rr = slice(64, 96)
                    kb.op("dve", lambda v: v.tensor_tensor(out=t1[0][rr, :], in0=pkr[0][rr, :], in1=cosb[rr, bs], op=ALU.mult),
                          reads=[pkr[1], tab_k], writes=[t1[1]])
                    kb.op("dve", lambda v: v.tensor_tensor(out=t2[0][rr, :], in0=pkrp[0][rr, :], in1=sinb[rr, bs], op=ALU.mult),
                          reads=[pkrp[1], tab_k], writes=[t2[1]])
                    kb.op("pool", lambda g: g.tensor_tensor(out=KRT[0][rr, bs], in0=t1[0][rr, :], in1=t2[0][rr, :], op=ALU.add),
                          reads=[t1[1], t2[1]], writes=[KRT[1]])
                kb.barrier()
            with ExitStack() as es:
                wuq = (sb(nc, es, "wuq", [128, 3, 1536], BF16), Tok())
                wukv = (sb(nc, es, "wukv", [128, 2, 2048], BF16), Tok())
                wqp = [(sb(nc, es, "wqp%d" % i, [128, 3, 96], BF16), Tok()) for i in range(2)]
                wvd = [(sb(nc, es, "wvd%d" % i, [128, 2, 128], BF16), Tok()) for i in range(2)]
                QT = [(sb(nc, es, "QT%d" % i, [128, S], BF16), Tok()) for i in range(1)]
                KT = [(sb(nc, es, "KT%d" % i, [128, S], BF16), Tok()) for i in range(2)]
                V = [(sb(nc, es, "V%d" % i, [128, NT, 128], BF16), Tok()) for i in range(2)]
                t1 = (sb(nc, es, "t1", [128, 512], F32), Tok())
                t2 = (sb(nc, es, "t2", [128, 512], F32), Tok())
                den = [(sb(nc, es, "den%d" % i, [128, 512], F32), Tok()) for i in range(1)]
                ab = AttnBufs(kb, es)
                pq = (ps(nc, es, "pq", [128, 512]), Tok())
                pqp = (ps(nc, es, "pqp", [128, 512]), Tok())
                pk = (ps(nc, es, "pk", [128, 512]), Tok())
                pv = (ps(nc, es, "pv", [128, 512]), Tok())
                kb.dma("pool", wuq[0][:], w_uq.rearrange("(c p) n -> p c n", p=128), wuq[1], True)
                kb.dma("pool", wukv[0][:], w_ukv.rearrange("(c p) n -> p c n", p=128), wukv[1], True)
                scale = 96.0 ** -0.5
                for h in range(16):
                    b = h % 2
                    wqpb, wqpk = wqp[b]
                    wvdb, wvdk = wvd[b]
                    QTb, QTk = QT[0]
                    KTb, KTk = KT[b]
                    Vb, Vk = V[b]
                    c0 = h * 96
                    kb.op("pool", lambda g: g.tensor_copy(out=wqpb[:, :, 0:64], in_=wuq[0][:, :, c0:c0 + 64]), reads=[wuq[1]], writes=[wqpk])
                    kb.op("pool", lambda g: g.tensor_copy(out=wqpb[:, :, 64:80], in_=wuq[0][:, :, c0 + 80:c0 + 96]), reads=[wuq[1]], writes=[wqpk])
                    kb.op("pool", lambda g: g.tensor_copy(out=wqpb[:, :, 80:96], in_=wuq[0][:, :, c0 + 64:c0 + 80]), reads=[wuq[1]], writes=[wqpk])
                    for hf in range(2):
                        kb.op("pool", lambda g, hf=hf: g.tensor_copy(out=wvdb[:, :, hf * 64:(hf + 1) * 64],
                                                                     in_=wukv[0][:, :, h * 128 + 64:h * 128 + 128]), reads=[wukv[1]], writes=[wvdk])
                    kb.op("pool", lambda g: g.tensor_copy(out=KTb[64:96, :], in_=KRT[0][64:96, :]), reads=[KRT[1]], writes=[KTk])
                    for blk in range(8):
                        bs = slice(blk * 512, (blk + 1) * 512)
                        for kc in range(3):
                            kb.mm(pq[1], pq[0][0:96, :], wuq[0][:, kc, c0:c0 + 96], cqn[0][:, kc, bs], kc == 0, kc == 2, [wuq[1], cqn[1]])
                        for kc in range(3):
                            kb.mm(pqp[1], pqp[0][0:96, :], wqpb[:, kc, :], cqn[0][:, kc, bs], kc == 0, kc == 2, [wqpk, cqn[1]])
                        r96 = slice(0, 96)
                        kb.op("dve", lambda v, bs=bs: v.tensor_tensor(out=t1[0][r96, :], in0=pq[0][r96, :], in1=cosb[r96, bs], op=ALU.mult),
                              reads=[pq[1], tab_k], writes=[t1[1]])
                        kb.op("dve", lambda v, bs=bs: v.tensor_tensor(out=t2[0][r96, :], in0=pqp[0][r96, :], in1=sinb[r96, bs], op=ALU.mult),
                              reads=[pqp[1], tab_k], writes=[t2[1]])
                        kb.op("pool", lambda g, bs=bs: g.tensor_tensor(out=QTb[r96, bs], in0=t1[0][r96, :], in1=t2[0][r96, :], op=ALU.add),
                              reads=[t1[1], t2[1]], writes=[QTk])
                        for kc in range(2):
                            kb.mm(pk[1], pk[0][0:64, :], wukv[0][:, kc, h * 128:h * 128 + 64], ckvn[0][:, kc, bs], kc == 0, kc == 1, [wukv[1], ckvn[1]])
                        kb.op("act", lambda a, bs=bs: a.activation(out=KTb[0:64, bs], in_=pk[0][0:64, :], func=AF.Copy), reads=[pk[1]], writes=[KTk])
                        for t4 in range(4):
                            ts_ = slice(blk * 512 + t4 * 128, blk * 512 + (t4 + 1) * 128)
                            for kc in range(2):
                                kb.mm(pv[1], pv[0][:, t4 * 128:(t4 + 1) * 128], ckvn[0][:, kc, ts_], wvdb[:, kc, :], kc == 0, kc == 1, [ckvn[1], wvdk])
                        kb.op("act", lambda a, blk=blk: a.activation(out=Vb[:, blk * 4:(blk + 1) * 4, :].rearrange("p t d -> p (t d)"),
                                                                     in_=pv[0][:], func=AF.Copy), reads=[pv[1]], writes=[Vk])
                    hs = slice((h % 2) * 64, (h % 2) * 64 + 64)

                    def on_done(qg, hs=hs, h=h):
                        dn, dnk = den[0]
                        kb.op("dve", lambda v: v.reciprocal(out=dn[hs, :], in_=ab.psm[0][hs, :]), reads=[ab.psm[1]], writes=[dnk])
                        kb.op("dve", lambda v: v.tensor_tensor(out=OT[0][hs, h // 2, qg * 512:(qg + 1) * 512], in0=ab.pso[0][hs, :],
                                                               in1=dn[hs, :], op=ALU.mult), reads=[ab.pso[1], dnk], writes=[OT[1]])

                    causal_attention(kb, c, ab,
                                     lambda kt: KTb[0:96, kt * 128:(kt + 1) * 128],
                                     lambda a, b_: QTb[0:96, a:b_],
                                     lambda kt: Vb[:, kt, :],
                                     scale, [KTk, QTk], [Vk], on_done)
                kb.barrier()
        outproj_epilogue(kb, c, OT, w_o.rearrange("(c p) n -> p c n", p=128), src_h, dst_h, dst_hT, ln_g, ln_b, router, rw_dst)


def phase_diff(kb, c, layer_idx, src_h, hT_src, w_in, lq1, lk1, lq2, lk2, subln, w_o, ln_g, ln_b, router, dst_h, dst_hT, rw_dst):
    import math
    nc = kb.nc
    lam_init = 0.8 - 0.6 * math.exp(-0.3 * layer_idx)
    hT3 = hT_src.rearrange("(c p) t -> p c t", p=128)
    w3 = w_in.rearrange("(c p) n -> p c n", p=128)
    with ExitStack() as es0:
        OT = (sb(nc, es0, "OT", [128, KC, S], BF16), Tok())
        with ExitStack() as es:
            cosb = sb(nc, es, "cosb", [128, S], BF16)
            sinb = sb(nc, es, "sinb", [128, S], BF16)
            tab_k = Tok()
            kb.dma("pool", cosb[:], c.rope16[0], tab_k, True)
            kb.dma("pool", sinb[:], c.rope16[1], tab_k, True)
            hblk = [(sb(nc, es, "hblk%d" % i, [128, KC, 512], BF16), Tok()) for i in range(2)]
            wq = [(sb(nc, es, "wq%d" % i, [128, KC, 128], BF16), Tok()) for i in range(2)]
            wqp = [(sb(nc, es, "wqp%d" % i, [128, KC, 128], BF16), Tok()) for i in range(2)]
            wk = [(sb(nc, es, "wk%d" % i, [128, KC, 128], BF16), Tok()) for i in range(2)]
            wkp = [(sb(nc, es, "wkp%d" % i, [128, KC, 128], BF16), Tok()) for i in range(2)]
            wv = [(sb(nc, es, "wv%d" % i, [128, KC, 128], BF16), Tok()) for i in range(2)]
            QT = (sb(nc, es, "QT", [128, S], BF16), Tok())
            KT = [(sb(nc, es, "KT%d" % i, [128, S], BF16), Tok()) for i in range(2)]
            V = [(sb(nc, es, "V%d" % i, [128, NT, 128], BF16), Tok()) for i in range(1)]
            tmp1 = [(sb(nc, es, "t1_%d" % i, [128, 512], F32), Tok()) for i in range(2)]
            tmp2 = [(sb(nc, es, "t2_%d" % i, [128, 512], F32), Tok()) for i in range(2)]
            lp = (sb(nc, es, "lp", [128, 4, 64], F32), Tok())
            ls = (sb(nc, es, "ls", [128, 8], F32), Tok())
            gcol = (sb(nc, es, "gcol", [128, 1], F32), Tok())
            den = (sb(nc, es, "den", [128, 512], F32), Tok())
            o1 = (sb(nc, es, "o1", [128, 512], F32), Tok())
            o2 = (sb(nc, es, "o2", [128, 512], F32), Tok())
            sqb = (sb(nc, es, "sqb", [128, 512], BF16), Tok())
            ab = AttnBufs(kb, es)
            pq = [(ps(nc, es, "pq%d" % i, [128, 512]), Tok()) for i in range(2)]
            pqp = [(ps(nc, es, "pqp%d" % i, [128, 512]), Tok()) for i in range(2)]
            for i, v_ in enumerate([lq1, lk1, lq2, lk2]):
                kb.dma("sp", lp[0][:, i, :], v_.partition_broadcast(128), lp[1], True)
            kb.op("dve", lambda v: v.tensor_tensor(out=lp[0][:, 0, :], in0=lp[0][:, 0, :], in1=lp[0][:, 1, :], op=ALU.mult), reads=[lp[1]], writes=[lp[1]])
            kb.op("dve", lambda v: v.tensor_tensor(out=lp[0][:, 2, :], in0=lp[0][:, 2, :], in1=lp[0][:, 3, :], op=ALU.mult), reads=[lp[1]], writes=[lp[1]])
            kb.op("dve", lambda v: v.tensor_reduce(out=ls[0][:, 0:1], in_=lp[0][:, 0, :], axis=AX.X, op=ALU.add), reads=[lp[1]], writes=[ls[1]])
            kb.op("dve", lambda v: v.tensor_reduce(out=ls[0][:, 1:2], in_=lp[0][:, 2, :], axis=AX.X, op=ALU.add), reads=[lp[1]], writes=[ls[1]])
            kb.op("act", lambda a: a.activation(out=ls[0][:, 2:4], in_=ls[0][:, 0:2], func=AF.Exp), reads=[ls[1]], writes=[ls[1]])
            kb.op("dve", lambda v: v.scalar_tensor_tensor(out=ls[0][:, 4:5], in0=ls[0][:, 3:4], scalar=-lam_init, in1=ls[0][:, 2:3],
                                                          op0=ALU.add, op1=ALU.subtract), reads=[ls[1]], writes=[ls[1]])
            kb.dma("sp", gcol[0][:], subln.rearrange("o p -> p o"), gcol[1], True, allow_slow_non_contiguous=True)
            kb.op("dve", lambda v: v.tensor_scalar(out=gcol[0][:], in0=gcol[0][:], scalar1=1.0 - lam_init, scalar2=None, op0=ALU.mult),
                  reads=[gcol[1]], writes=[gcol[1]])
            nq = 0
            scale = 64.0 ** -0.5
            for h in range(8):
                b = h % 2
                kb.dma("pool", wq[b][0][:], w3[:, :, h * 128:(h + 1) * 128], wq[b][1], True)
                for hf in range(2):
                    load_half_weight(kb, wk[hf][0], wk[hf][1], w3, 1024 + h * 128 + hf * 64, hf)
                kb.dma("pool", wv[b][0][:], w3[:, :, 2048 + h * 128:2048 + (h + 1) * 128], wv[b][1], True)
                make_partner(kb, wq[b][0], wq[b][1], wqp[b][0], wqp[b][1], 128, 8, 64)
                for hf in range(2):
                    make_partner(kb, wk[hf][0], wk[hf][1], wkp[hf][0], wkp[hf][1], 128, 8, 64)
                Vb, Vk = V[0]
                kb.dma("sp", hblk[0][0][:], hT3[:, :, 0:512], hblk[0][1], True)
                for blk in range(8):
                    if blk + 1 < 8:
                        kb.dma("sp", hblk[(blk + 1) % 2][0][:], hT3[:, :, (blk + 1) * 512:(blk + 2) * 512], hblk[(blk + 1) % 2][1], True)
                    hb_, hbk = hblk[blk % 2]
                    bs = slice(blk * 512, (blk + 1) * 512)
                    proj_rope(kb, wq[b][0], wq[b][1], wqp[b][0], wqp[b][1], 0, hb_, hbk, pq[nq % 2], pqp[nq % 2],
                              cosb[:, bs], sinb[:, bs], tab_k, tmp1[nq % 2], tmp2[nq % 2], QT[0][:, bs], QT[1])
                    nq += 1
                    for hf in range(2):
                        proj_rope(kb, wk[hf][0], wk[hf][1], wkp[hf][0], wkp[hf][1], 0, hb_, hbk, pq[nq % 2], pqp[nq % 2],
                                  cosb[:, bs], sinb[:, bs], tab_k, tmp1[nq % 2], tmp2[nq % 2], KT[hf][0][:, bs], KT[hf][1])
                        nq += 1
                    pv = pq[nq % 2]
                    nq += 1
                    for t4 in range(4):
                        for kc in range(KC):
                            kb.mm(pv[1], pv[0][:, t4 * 128:(t4 + 1) * 128], hb_[:, kc, t4 * 128:(t4 + 1) * 128], wv[b][0][:, kc, :],
                                  kc == 0, kc == KC - 1, [hbk, wv[b][1]])
                    kb.op("act", lambda a, pv=pv, blk=blk, Vb=Vb: a.activation(
                        out=Vb[:, blk * 4:(blk + 1) * 4, :].rearrange("p t d -> p (t d)"), in_=pv[0][:], func=AF.Copy),
                        reads=[pv[1]], writes=[Vk])
                for qg in range(8):
                    qs = slice(qg * 512, (qg + 1) * 512)
                    for m_ in range(2):
                        ms = slice(m_ * 64, m_ * 64 + 64)
                        om = o1 if m_ == 0 else o2

                        def on_done(qg_, om=om):
                            kb.op("dve", lambda v: v.reciprocal(out=den[0][:], in_=ab.psm[0][:]), reads=[ab.psm[1]], writes=[den[1]])
                            kb.op("dve", lambda v: v.tensor_tensor(out=om[0][:], in0=ab.pso[0][:], in1=den[0][:], op=ALU.mult),
                                  reads=[ab.pso[1], den[1]], writes=[om[1]])

                        causal_attention(kb, c, ab,
                                         lambda kt, m_=m_: KT[m_][0][:, kt * 128:(kt + 1) * 128],
                                         lambda a, b_: QT[0][:, a:b_],
                                         lambda kt: Vb[:, kt, :],
                                         scale, [KT[m_][1], QT[1]], [Vk], on_done, qgroups=[qg])
                    kb.op("dve", lambda v: v.scalar_tensor_tensor(out=o1[0][:], in0=o2[0][:], scalar=ls[0][:, 4:5], in1=o1[0][:],
                                                                  op0=ALU.mult, op1=ALU.add), reads=[o1[1], o2[1], ls[1]], writes=[o1[1]])
                    kb.op("act", lambda a: a.activation(out=sqb[0][:], in_=o1[0][:], func=AF.Square), reads=[o1[1]], writes=[sqb[1]])
                    pssq, pssqk = ab.pss[ab.n % 2]
                    ab.n += 1
                    kb.mm(pssqk, pssq[:], c.onesb[:], sqb[0][:], True, True, [c.const_k, sqb[1]])
                    kb.op("dve", lambda v, pssq=pssq: v.tensor_scalar(out=den[0][:], in0=pssq[:], scalar1=1.0 / 128.0, scalar2=RMS_EPS,
                                                                     op0=ALU.mult, op1=ALU.add), reads=[pssqk], writes=[den[1]])
                    kb.op("act", lambda a: a.activation(out=den[0][:], in_=den[0][:], func=AF.Sqrt), reads=[den[1]], writes=[den[1]])
                    kb.op("dve", lambda v: v.reciprocal(out=den[0][:], in_=den[0][:]), reads=[den[1]], writes=[den[1]])
                    kb.op("dve", lambda v, h=h, qs=qs: v.scalar_tensor_tensor(out=OT[0][:, h, qs], in0=o1[0][:], scalar=gcol[0][:, 0:1], in1=den[0][:],
                                                                            op0=ALU.mult, op1=ALU.mult), reads=[o1[1], den[1], gcol[1]], writes=[OT[1]])
            kb.barrier()
        outproj_epilogue(kb, c, OT, w_o.rearrange("(c p) n -> p c n", p=128), src_h, dst_h, dst_hT, ln_g, ln_b, router, rw_dst)


def phase_nsa(kb, c, src_h, hT_src, w_in, pos_k, pos_v, wk1, wk2, wv1, wv2, w_o, ln_g, ln_b, router, dst_h, dst_hT, rw_dst, gsc, ot_dram):
    nc = kb.nc
    hT3 = hT_src.rearrange("(c p) t -> p c t", p=128)
    w3 = w_in.rearrange("(c p) n -> p c n", p=128)
    NCMP = 255
    with ExitStack() as es0:
        for hk in range(4):
            with ExitStack() as esu:
                QT = (sb(nc, esu, "QT", [128, 2, S], BF16), Tok())
                KVc = (sb(nc, esu, "KVc", [128, S], BF16), Tok())
                KsT2 = [(sb(nc, esu, "KsT%d" % i, [128, S], BF16), Tok()) for i in range(2)]
                KwT2 = [(sb(nc, esu, "KwT%d" % i, [128, S], BF16), Tok()) for i in range(2)]
                Vs = (sb(nc, esu, "Vs", [128, NT, 128], BF16), Tok())
                Vw = (sb(nc, esu, "Vw", [128, NT, 128], BF16), Tok())
                with ExitStack() as es:
                    cosb = sb(nc, es, "cosb", [128, S], BF16)
                    sinb = sb(nc, es, "sinb", [128, S], BF16)
                    tab_k = Tok()
                    kb.dma("pool", cosb[:], c.rope16[0], tab_k, True)
                    kb.dma("pool", sinb[:], c.rope16[1], tab_k, True)
                    hblk = [(sb(nc, es, "hblk%d" % i, [128, KC, 512], BF16), Tok()) for i in range(2)]
                    wq = (sb(nc, es, "wq", [128, KC, 256], BF16), Tok())
                    wqp = (sb(nc, es, "wqp", [128, KC, 256], BF16), Tok())
                    wkv = [(sb(nc, es, "wkv%d" % j, [128, KC, 128], BF16), Tok()) for j in range(3)]
                    wks2 = [(sb(nc, es, "wks%d" % j, [128, KC, 128], BF16), Tok()) for j in range(2)]
                    wksp2 = [(sb(nc, es, "wksp%d" % j, [128, KC, 128], BF16), Tok()) for j in range(2)]
                    wkw2 = [(sb(nc, es, "wkw%d" % j, [128, KC, 128], BF16), Tok()) for j in range(2)]
                    wkwp2 = [(sb(nc, es, "wkwp%d" % j, [128, KC, 128], BF16), Tok()) for j in range(2)]
                    wgt = (sb(nc, es, "wgt", [128, KC, 48], BF16), Tok())
                    gsb = [(sb(nc, es, "gsb%d" % i, [128, 512], F32), Tok()) for i in range(2)]
                    tmp1 = [(sb(nc, es, "t1_%d" % i, [128, 512], F32), Tok()) for i in range(2)]
                    tmp2 = [(sb(nc, es, "t2_%d" % i, [128, 512], F32), Tok()) for i in range(2)]
                    pq = [(ps(nc, es, "pq%d" % i, [128, 512]), Tok()) for i in range(2)]
                    pqp = [(ps(nc, es, "pqp%d" % i, [128, 512]), Tok()) for i in range(2)]
                    kb.dma("pool", wq[0][:], w3[:, :, hk * 256:(hk + 1) * 256], wq[1], True)
                    cb = lambda j: 1024 + j * 256 + hk * 64
                    kb.dma("pool", wkv[0][0][:, :, 0:64], w3[:, :, cb(0):cb(0) + 64], wkv[0][1], True)
                    kb.dma("pool", wkv[0][0][:, :, 64:128], w3[:, :, cb(1):cb(1) + 64], wkv[0][1], True)
                    for jj, j in enumerate([3, 5]):
                        for hf in range(2):
                            kb.dma("pool", wkv[1 + jj][0][:, :, hf * 64:(hf + 1) * 64], w3[:, :, cb(j):cb(j) + 64], wkv[1 + jj][1], True)
                    for hf in range(2):
                        load_half_weight(kb, wks2[hf][0], wks2[hf][1], w3, cb(2), hf)
                        load_half_weight(kb, wkw2[hf][0], wkw2[hf][1], w3, cb(4), hf)
                    if hk == 0:
                        kb.dma("pool", wgt[0][:], w3[:, :, 2560:2608], wgt[1], True)
                    make_partner(kb, wq[0], wq[1], wqp[0], wqp[1], 256, 8, 64)
                    for hf in range(2):
                        make_partner(kb, wks2[hf][0], wks2[hf][1], wksp2[hf][0], wksp2[hf][1], 128, 8, 64)
                        make_partner(kb, wkw2[hf][0], wkw2[hf][1], wkwp2[hf][0], wkwp2[hf][1], 128, 8, 64)
                    kb.dma("sp", hblk[0][0][:], hT3[:, :, 0:512], hblk[0][1], True)
                    nq = 0
                    for blk in range(8):
                        if blk + 1 < 8:
                            kb.dma("sp", hblk[(blk + 1) % 2][0][:], hT3[:, :, (blk + 1) * 512:(blk + 2) * 512], hblk[(blk + 1) % 2][1], True)
                        hb_, hbk = hblk[blk % 2]
                        bs = slice(blk * 512, (blk + 1) * 512)
                        for cc in range(2):
                            proj_rope(kb, wq[0], wq[1], wqp[0], wqp[1], cc * 128, hb_, hbk, pq[nq % 2], pqp[nq % 2],
                                      cosb[:, bs], sinb[:, bs], tab_k, tmp1[nq % 2], tmp2[nq % 2], QT[0][:, cc, bs], QT[1])
                            nq += 1
                        for (wi, wpi, dst) in [(wks2[0], wksp2[0], KsT2[0]), (wks2[1], wksp2[1], KsT2[1]),
                                               (wkw2[0], wkwp2[0], KwT2[0]), (wkw2[1], wkwp2[1], KwT2[1])]:
                            proj_rope(kb, wi[0], wi[1], wpi[0], wpi[1], 0, hb_, hbk, pq[nq % 2], pqp[nq % 2],
                                      cosb[:, bs], sinb[:, bs], tab_k, tmp1[nq % 2], tmp2[nq % 2], dst[0][:, bs], dst[1])
                            nq += 1
                        pc_ = pq[nq % 2]
                        nq += 1
                        for kc in range(KC):
                            kb.mm(pc_[1], pc_[0][:], wkv[0][0][:, kc, :], hb_[:, kc, :], kc == 0, kc == KC - 1, [wkv[0][1], hbk])
                        kb.op("act", lambda a, pc_=pc_, bs=bs: a.activation(out=KVc[0][:, bs], in_=pc_[0][:], func=AF.Copy), reads=[pc_[1]], writes=[KVc[1]])
                        for (wi, dst) in [(wkv[1], Vs), (wkv[2], Vw)]:
                            pv = pqp[nq % 2]
                            nq += 1
                            for t4 in range(4):
                                for kc in range(KC):
                                    kb.mm(pv[1], pv[0][:, t4 * 128:(t4 + 1) * 128], hb_[:, kc, t4 * 128:(t4 + 1) * 128], wi[0][:, kc, :],
                                          kc == 0, kc == KC - 1, [hbk, wi[1]])
                            kb.op("act", lambda a, pv=pv, blk=blk, dst=dst: a.activation(
                                out=dst[0][:, blk * 4:(blk + 1) * 4, :].rearrange("p t d -> p (t d)"), in_=pv[0][:], func=AF.Copy),
                                reads=[pv[1]], writes=[dst[1]])
                        if hk == 0:
                            pg_ = pq[nq % 2]
                            nq += 1
                            for kc in range(KC):
                                kb.mm(pg_[1], pg_[0][0:48, :], wgt[0][:, kc, :], hb_[:, kc, :], kc == 0, kc == KC - 1, [wgt[1], hbk])
                            gb_, gbk = gsb[blk % 2]
                            kb.op("act", lambda a, pg_=pg_, gb_=gb_: a.activation(out=gb_[0:48, :], in_=pg_[0][0:48, :], func=AF.Sigmoid),
                                  reads=[pg_[1]], writes=[gbk])
                            kb.dma("sp", gsc[:, bs], gb_[0:48, :], gbk, False)
                    kb.barrier()
                with ExitStack() as es:
                    cm0 = sb(nc, es, "cm0", [128, 17, 128], BF16)
                    cm1 = sb(nc, es, "cm1", [128, 16, 128], BF16)
                    ovl = sb(nc, es, "ovl", [128, 2, 64], BF16)
                    Fm = sb(nc, es, "Fm", [128, 32, 64], F32)
                    Em = sb(nc, es, "Em", [128, 32, 128], BF16)
                    ck = Tok()
                    kb.dma("pool", cm0[:], c.n_cm0, ck, True)
                    kb.dma("pool", cm1[:], c.n_cm1, ck, True)
                    kb.dma("pool", ovl[:], c.n_ovl, ck, True)
                    kb.dma("sp", Fm[:], c.n_F, ck, True)
                    kb.dma("pool", Em[0:64, :, :], c.n_E, ck, True)
                    w1p = [(sb(nc, es, "w1p%d" % i, [128, 32, 128], BF16), Tok()) for i in range(2)]
                    w2d = (sb(nc, es, "w2d", [128, 3, 128], BF16), Tok())
                    peT = (sb(nc, es, "peT", [128, 32], BF16), Tok())
                    bias2 = (sb(nc, es, "bias2", [128, 2], F32), Tok())
                    hx = (sb(nc, es, "hx", [128, 2, 256], F32), Tok())
                    hy = (sb(nc, es, "hy", [128, 2, 256], F32), Tok())
                    hg = (sb(nc, es, "hg", [128, 2, 256], BF16), Tok())
                    KcT2 = [(sb(nc, es, "KcT%d" % i, [128, 256], BF16), Tok()) for i in range(2)]
                    ost = [(sb(nc, es, "ost%d" % i, [128, 512], BF16), Tok()) for i in range(2)]
                    Vc = (sb(nc, es, "Vc", [128, 2, 128], BF16), Tok())
                    penT = (sb(nc, es, "penT", [128, S], BF16), Tok())
                    ocmp = (sb(nc, es, "ocmp", [128, 4, 512], F32), Tok())
                    owin = (sb(nc, es, "owin", [128, 4, 512], F32), Tok())
                    den = [(sb(nc, es, "den%d" % i, [128, 512], F32), Tok()) for i in range(2)]
                    impn = (sb(nc, es, "impn", [128, 512], F32), Tok())
                    impT = (sb(nc, es, "impT", [128, 128], F32), Tok())
                    tk = (sb(nc, es, "tk", [128, 160], F32), Tok())
                    penb = (sb(nc, es, "penb", [128, 64], BF16), Tok())
                    Gb = [(sb(nc, es, "Gb%d" % i, [128, 512], F32), Tok()) for i in range(6)]
                    acc = (sb(nc, es, "acc", [128, 512], F32), Tok())
                    tmpa = (sb(nc, es, "tmpa", [128, 512], F32), Tok())
                    ab = AttnBufs(kb, es)
                    pimp = (ps(nc, es, "pimp", [128, 512]), Tok())
                    pmisc = (ps(nc, es, "pmisc", [128, 512]), Tok())
                    pmb = (ps(nc, es, "pmb", [128, 1024], BF16), Tok())
                    for i_ in range(2):
                        kb.op("pool", lambda g, i_=i_: g.memset(w1p[i_][0][:], 0.0), writes=[w1p[i_][1]])
                    kb.op("pool", lambda g: g.memset(w2d[0][:], 0.0), writes=[w2d[1]])
                    kb.dma("pool", w1p[0][0][0:64, :, :], wk1.rearrange("(l d) j -> d l j", d=64), w1p[0][1], True)
                    kb.dma("pool", w1p[1][0][64:128, :, :], wv1.rearrange("(l d) j -> d l j", d=64), w1p[1][1], True)
                    kb.dma("pool", w2d[0][:, 0, 0:64], wk2, w2d[1], True)
                    kb.dma("pool", w2d[0][:, 1, 64:128], wk2, w2d[1], True)
                    for hf in range(2):
                        kb.dma("pool", w2d[0][:, 2, hf * 64:(hf + 1) * 64], wv2, w2d[1], True)
                    kb.dma("pool", peT[0][0:64, :], pos_k.rearrange("l d -> d l"), peT[1], True, allow_slow_non_contiguous=True)
                    kb.dma("pool", peT[0][64:128, :], pos_v.rearrange("l d -> d l"), peT[1], True, allow_slow_non_contiguous=True)
                    for kv in range(2):
                        for l in range(32):
                            kb.mm(pmisc[1], pmisc[0][:, kv:kv + 1], w1p[kv][0][:, l, :], peT[0][:, l:l + 1], l == 0, l == 31, [w1p[kv][1], peT[1]])
                    kb.op("act", lambda a: a.activation(out=bias2[0][:], in_=pmisc[0][:, 0:2], func=AF.Copy), reads=[pmisc[1]], writes=[bias2[1]])
                    for kv in range(2):
                        for l in range(32):
                            kb.mm(pimp[1], pimp[0][:, kv * 256:kv * 256 + NCMP], w1p[kv][0][:, l, :], KVc[0][:, l:l + 16 * (NCMP - 1) + 1:16],
                                  l == 0, l == 31, [w1p[kv][1], KVc[1]])
                    C0 = 0.7978845608028654
                    for kv in range(2):
                        x_ = hx[0][:, kv, 0:NCMP]
                        y_ = hy[0][:, kv, 0:NCMP]
                        kb.op("act", lambda a, kv=kv, x_=x_: a.activation(out=x_, in_=pimp[0][:, kv * 256:kv * 256 + NCMP], func=AF.Identity,
                                                                       bias=bias2[0][:, kv:kv + 1], scale=1.0), reads=[pimp[1], bias2[1]], writes=[hx[1]])
                        kb.op("dve", lambda v, x_=x_, y_=y_: v.tensor_tensor(out=y_, in0=x_, in1=x_, op=ALU.mult), reads=[hx[1]], writes=[hy[1]])
                        kb.op("dve", lambda v, y_=y_: v.tensor_scalar(out=y_, in0=y_, scalar1=0.044715, scalar2=1.0, op0=ALU.mult, op1=ALU.add),
                              reads=[hy[1]], writes=[hy[1]])
                        kb.op("dve", lambda v, x_=x_, y_=y_: v.tensor_tensor(out=y_, in0=y_, in1=x_, op=ALU.mult), reads=[hx[1], hy[1]], writes=[hy[1]])
                        kb.op("act", lambda a, y_=y_: a.activation(out=y_, in_=y_, func=AF.Tanh, scale=C0), reads=[hy[1]], writes=[hy[1]])
                        kb.op("dve", lambda v, x_=x_, y_=y_: v.scalar_tensor_tensor(out=y_, in0=y_, scalar=1.0, in1=x_, op0=ALU.add, op1=ALU.mult),
                              reads=[hx[1], hy[1]], writes=[hy[1]])
                        kb.op("dve", lambda v, kv=kv, y_=y_: v.tensor_scalar(out=hg[0][:, kv, 0:NCMP], in0=y_, scalar1=0.5, scalar2=None, op0=ALU.mult),
                              reads=[hy[1]], writes=[hg[1]])
                    for i_ in range(2):
                        kb.mm(pmisc[1], pmisc[0][:, 0:NCMP], w2d[0][:, i_, :], hg[0][:, 0, 0:NCMP], True, True, [w2d[1], hg[1]])
                        kb.op("act", lambda a, i_=i_: a.activation(out=KcT2[i_][0][:, 0:NCMP], in_=pmisc[0][:, 0:NCMP], func=AF.Copy),
                              reads=[pmisc[1]], writes=[KcT2[i_][1]])
                    kb.mm(pimp[1], pimp[0][:, 0:128], hg[0][:, 1, 0:128], w2d[0][:, 2, :], True, True, [w2d[1], hg[1]])
                    kb.mm(pimp[1], pimp[0][0:127, 128:256], hg[0][:, 1, 128:NCMP], w2d[0][:, 2, :], True, True, [w2d[1], hg[1]])
                    kb.op("act", lambda a: a.activation(out=Vc[0][:, 0, :], in_=pimp[0][:, 0:128], func=AF.Copy), reads=[pimp[1]], writes=[Vc[1]])
                    kb.op("act", lambda a: a.activation(out=Vc[0][0:127, 1, :], in_=pimp[0][0:127, 128:256], func=AF.Copy), reads=[pimp[1]], writes=[Vc[1]])
                    ngb = 0
                    for qg in range(8):
                        for b4 in range(4):
                            n = 4 * qg + b4
                            qs = slice(n * 128, (n + 1) * 128)
                            c2s = [0] + ([1] if n >= 16 else [])
                            for ci, c2 in enumerate(c2s):
                                nk = 128 if c2 == 0 else 127
                                rows = slice(0, nk)
                                mk = None
                                if c2 == 0 and n < 17:
                                    mk = cm0[rows, n, :]
                                if c2 == 1:
                                    mk = cm1[rows, n - 16, :]
                                sb_, sk = ab.pss[ab.n % 2]
                                pt_, ptk = ab.PT[ab.n % 3]
                                ab.n += 1
                                for g in range(4):
                                    gc = slice(g * 128, (g + 1) * 128)
                                    if mk is not None:
                                        kb.mm(sk, sb_[rows, gc], c.identb[rows, 0:nk], mk, True, False, [c.const_k, ck])
                                    kb.mm(sk, sb_[rows, gc], KcT2[g % 2][0][:, c2 * 128:c2 * 128 + nk], QT[0][:, g // 2, qs], mk is None, True,
                                          [KcT2[g % 2][1], QT[1]])
                                kb.op("act", lambda a, pt_=pt_, sb_=sb_, rows=rows: a.activation(out=pt_[rows, :], in_=sb_[rows, :], func=AF.Exp, scale=0.125),
                                      reads=[sk], writes=[ptk])
                                last = ci == len(c2s) - 1
                                kb.mm(ab.pso[1], ab.pso[0][:], Vc[0][rows, c2, :], pt_[rows, :], ci == 0, last, [Vc[1], ptk])
                                kb.mm(ab.psm[1], ab.psm[0][:], c.onesb[rows, :], pt_[rows, :], ci == 0, last, [c.const_k, ptk])
                                kb.mm(pimp[1], pimp[0][0:64, :], ovl[rows, c2, :], pt_[rows, :], ci == 0, last, [ck, ptk])
                            dn, dnk = den[0]
                            kb.op("dve", lambda v, dn=dn: v.tensor_scalar(out=dn[:], in0=ab.psm[0][:], scalar1=1e-30, scalar2=None, op0=ALU.max),
                                  reads=[ab.psm[1]], writes=[dnk])
                            kb.op("dve", lambda v, dn=dn: v.reciprocal(out=dn[:], in_=dn[:]), reads=[dnk], writes=[dnk])
                            kb.op("dve", lambda v, dn=dn, b4=b4: v.tensor_tensor(out=ocmp[0][:, b4, :], in0=ab.pso[0][:], in1=dn[:], op=ALU.mult),
                                  reads=[ab.pso[1], dnk], writes=[ocmp[1]])
                            kb.op("dve", lambda v, dn=dn: v.tensor_tensor(out=impn[0][0:64, :], in0=pimp[0][0:64, :], in1=dn[0:64, :], op=ALU.mult),
                                  reads=[pimp[1], dnk], writes=[impn[1]])
                            kb.op("dve", lambda v: v.tensor_reduce(out=impT[0][0:64, :], in_=impn[0][0:64, :].rearrange("p (g i) -> p i g", g=4),
                                                                   axis=AX.X, op=ALU.add), reads=[impn[1]], writes=[impT[1]])
                            kb.op("pe", lambda pe: pe.transpose(out=pmisc[0][:, 0:64], in_=impT[0][0:64, :], identity=c.ident[0:64, 0:64]),
                                  reads=[impT[1], c.ident_k], writes=[pmisc[1]])
                            impF, m8a, m8b, wrk = tk[0][:, 0:64], tk[0][:, 64:72], tk[0][:, 72:80], tk[0][:, 80:144]
                            kb.op("dve", lambda v, n=n: v.tensor_tensor(out=impF, in0=pmisc[0][:, 0:64], in1=Fm[:, n, :], op=ALU.add),
                                  reads=[pmisc[1], ck], writes=[tk[1]])
                            kb.op("dve", lambda v: v.max(out=m8a, in_=impF), reads=[tk[1]], writes=[tk[1]])
                            kb.op("dve", lambda v: v.match_replace(out=wrk, in_to_replace=m8a, in_values=impF, imm_value=-3.0e38), reads=[tk[1]], writes=[tk[1]])
                            kb.op("dve", lambda v: v.max(out=m8b, in_=wrk), reads=[tk[1]], writes=[tk[1]])
                            kb.op("dve", lambda v: v.tensor_scalar(out=wrk, in0=impF, scalar1=m8b[:, 7:8], scalar2=None, op0=ALU.is_ge), reads=[tk[1]], writes=[tk[1]])
                            kb.op("dve", lambda v: v.tensor_scalar(out=penb[0][:], in0=wrk, scalar1=-NEG, scalar2=NEG, op0=ALU.mult, op1=ALU.add),
                                  reads=[tk[1]], writes=[penb[1]])
                            kb.op("pe", lambda pe: pe.transpose(out=pmb[0][0:64, 0:128], in_=penb[0][:, :], identity=c.identb[:, :]),
                                  reads=[penb[1], c.const_k], writes=[pmb[1]])
                            kb.op("act", lambda a, qs=qs: a.activation(out=penT[0][0:64, qs], in_=pmb[0][0:64, 0:128], func=AF.Copy), reads=[pmb[1]], writes=[penT[1]])
                            kts = list(range(max(0, n - 4), n + 1))
                            for ki, kt in enumerate(kts):
                                ks = slice(kt * 128, (kt + 1) * 128)
                                mk = c.mb_cur if kt == n else (c.mb_prev if kt == n - 4 else None)
                                sb_, sk = ab.pss[ab.n % 2]
                                pt_, ptk = ab.PT[ab.n % 3]
                                ab.n += 1
                                if mk is not None:
                                    kb.mm(sk, sb_[:], c.identb[:], mk[:], True, False, [c.const_k])
                                for g in range(4):
                                    kb.mm(sk, sb_[:, g * 128:(g + 1) * 128], KwT2[g % 2][0][:, ks], QT[0][:, g // 2, qs], mk is None, mk is None or g == 3,
                                          [KwT2[g % 2][1], QT[1]])
                                kb.op("act", lambda a, pt_=pt_, sb_=sb_: a.activation(out=pt_[:], in_=sb_[:], func=AF.Exp, scale=0.125), reads=[sk], writes=[ptk])
                                kb.mm(ab.pso[1], ab.pso[0][:], Vw[0][:, kt, :], pt_[:], ki == 0, ki == len(kts) - 1, [Vw[1], ptk])
                                kb.mm(ab.psm[1], ab.psm[0][:], c.onesb[:], pt_[:], ki == 0, ki == len(kts) - 1, [c.const_k, ptk])
                            dn, dnk = den[1]
                            kb.op("dve", lambda v, dn=dn: v.reciprocal(out=dn[:], in_=ab.psm[0][:]), reads=[ab.psm[1]], writes=[dnk])
                            kb.op("dve", lambda v, dn=dn, b4=b4: v.tensor_tensor(out=owin[0][:, b4, :], in0=ab.pso[0][:], in1=dn[:], op=ALU.mult),
                                  reads=[ab.pso[1], dnk], writes=[owin[1]])
                        for g in range(4):
                            hs = slice((g % 2) * 64, (g % 2) * 64 + 64)
                            head = 4 * hk + g
                            gbs = []
                            for br in range(3):
                                gt_, gtk = Gb[ngb % 6]
                                ngb += 1
                                row = head * 3 + br
                                kb.dma("sp", gt_[:], gsc[row:row + 1, qg * 512:(qg + 1) * 512].partition_broadcast(128), gtk, True)
                                gbs.append((gt_, gtk))

                            def bias_fn(sk, sb_, kt, qg_, lo, first):
                                kb.mm(sk, sb_[:, lo:512], Em[0:64, kt, :], penT[0][0:64, qg_ * 512 + lo:(qg_ + 1) * 512], first, False, [ck, penT[1]])
                                return False

                            def on_done(qg_, hs=hs, g=g, gbs=gbs):
                                dn, dnk = den[0]
                                qsl = slice(qg_ * 512, (qg_ + 1) * 512)
                                kb.op("dve", lambda v: v.reciprocal(out=dn[hs, :], in_=ab.psm[0][hs, :]), reads=[ab.psm[1]], writes=[dnk])
                                kb.op("dve", lambda v: v.tensor_tensor(out=dn[hs, :], in0=dn[hs, :], in1=gbs[1][0][hs, :], op=ALU.mult),
                                      reads=[dnk, gbs[1][1]], writes=[dnk])
                                kb.op("dve", lambda v: v.tensor_tensor(out=acc[0][hs, :], in0=ab.pso[0][hs, :], in1=dn[hs, :], op=ALU.mult),
                                      reads=[ab.pso[1], dnk], writes=[acc[1]])
                                for (src, gi) in [(ocmp, 0), (owin, 2)]:
                                    kb.op("dve", lambda v, src=src, gi=gi: v.tensor_tensor(
                                        out=tmpa[0][hs, :].rearrange("p (b i) -> p b i", b=4), in0=src[0][hs, :, g * 128:(g + 1) * 128],
                                        in1=gbs[gi][0][hs, :].rearrange("p (b i) -> p b i", b=4), op=ALU.mult),
                                        reads=[src[1], gbs[gi][1]], writes=[tmpa[1]])
                                    kb.op("dve", lambda v: v.tensor_tensor(out=acc[0][hs, :], in0=acc[0][hs, :], in1=tmpa[0][hs, :], op=ALU.add),
                                          reads=[acc[1], tmpa[1]], writes=[acc[1]])
                                ob, obk = ost[g % 2]
                                kb.op("act", lambda a: a.activation(out=ob[hs, :], in_=acc[0][hs, :], func=AF.Copy), reads=[acc[1]], writes=[obk])
                                r0 = (2 * hk + g // 2) * 128 + hs.start
                                kb.dma("sp", ot_dram[r0:r0 + 64, qsl], ob[hs, :], obk, False)

                            causal_attention(kb, c, ab,
                                             lambda kt, g=g: KsT2[g % 2][0][:, kt * 128:(kt + 1) * 128],
                                             lambda a, b_, g=g: QT[0][:, g // 2, a:b_],
                                             lambda kt: Vs[0][:, kt, :],
                                             0.125, [KsT2[g % 2][1], QT[1]], [Vs[1]], on_done, qgroups=[qg], bias_fn=bias_fn)
                    kb.barrier()
        outproj_epilogue(kb, c, None, w_o.rearrange("(c p) n -> p c n", p=128), src_h, dst_h, dst_hT, ln_g, ln_b, router, rw_dst,
                         ot_dram=ot_dram)


W_SHAPES = {
    "a_w_in": (1024, 1536), "a_sinks": (1, 16), "a_w_o": (1024, 1024),
    "b_w_down": (1024, 672), "b_q_norm": (1, 384), "b_kv_norm": (1, 256), "b_w_uq": (384, 1536),
    "b_w_ukv": (256, 2048), "b_w_o": (1024, 1024),
    "c_w_in": (1024, 2608), "c_pos_k": (32, 64), "c_pos_v": (32, 64), "c_wk1": (2048, 128), "c_wk2": (128, 64),
    "c_wv1": (2048, 128), "c_wv2": (128, 64), "c_w_o": (1024, 1024),
    "d_w_in": (1024, 3072), "d_lq1": (1, 64), "d_lk1": (1, 64), "d_lq2": (1, 64), "d_lk2": (1, 64),
    "d_subln": (1, 128), "d_w_o": (1024, 1024),
}
LAYER_W = {0: ["a_w_in", "a_sinks", "a_w_o"],
           1: ["b_w_down", "b_q_norm", "b_kv_norm", "b_w_uq", "b_w_ukv", "b_w_o"],
           2: ["c_w_in", "c_pos_k", "c_pos_v", "c_wk1", "c_wk2", "c_wv1", "c_wv2", "c_w_o"],
           3: ["d_w_in", "d_lq1", "d_lk1", "d_lq2", "d_lk2", "d_subln", "d_w_o"]}


def host_constants():
    cst = {}
    cst["ident"] = np.eye(128, dtype=np.float32)
    t = np.arange(S, dtype=np.float32)

    def rope_tab(rot_dim, base_part, period):
        half = rot_dim // 2
        inv = (1.0 / (np.float32(500000.0) ** (np.arange(half, dtype=np.float32) * np.float32(2.0 / rot_dim)))).astype(np.float32)
        ang = t[None, :] * inv[:, None]
        cos = np.ones((128, S), np.float32)
        sin = np.zeros((128, S), np.float32)
        for p in range(128):
            d = (p - base_part) % period
            if p < base_part:
                continue
            if d < half:
                cos[p] = np.cos(ang[d]); sin[p] = -np.sin(ang[d])
            elif d < rot_dim:
                cos[p] = np.cos(ang[d - half]); sin[p] = np.sin(ang[d - half])
        return np.stack([cos, sin]).astype(np.float32)

    cst["rope16"] = rope_tab(16, 0, 64)
    cst["rope32"] = rope_tab(32, 64, 64)
    jj = np.arange(128)[:, None]
    ii = np.arange(128)[None, :]
    cur = np.where(jj <= ii, 0.0, NEG).astype(np.float32)
    prev = np.where(jj > ii, 0.0, NEG).astype(np.float32)
    diag = np.concatenate([cur, np.zeros((128, 384), np.float32)], axis=1)
    nn = np.arange(256)
    cend = 16 * nn + 31
    tq = np.arange(S).reshape(32, 128)
    vis = (cend[:, None, None] <= tq[None, :, :]) & (nn[:, None, None] < 255)
    cmk = np.where(vis, 0.0, NEG).astype(np.float32)
    cst["ncm0"] = np.ascontiguousarray(cmk[0:128, 0:17, :])
    cst["ncm1"] = np.ascontiguousarray(cmk[128:256, 16:32, :])
    cstart = 16 * nn
    sstart = 64 * np.arange(64)
    ov = ((cstart[:, None] <= sstart[None, :] + 63) & (sstart[None, :] <= cend[:, None]) & (nn[:, None] < 255)).astype(np.float32)
    cst["novl"] = np.ascontiguousarray(ov.reshape(2, 128, 64).transpose(1, 0, 2))
    curb = (tq // 64)[:, :, None]
    jb = np.arange(64)[None, None, :]
    Fm = np.where((jb == 0) | (jb == curb) | (jb == curb - 1), 1e30, 0.0)
    Fm = np.where(jb <= curb, Fm, -1e30).astype(np.float32)
    cst["nF"] = np.ascontiguousarray(Fm.transpose(1, 0, 2))
    Em = np.zeros((64, 32, 128), np.float32)
    for kt in range(32):
        for r in range(128):
            Em[2 * kt + r // 64, kt, r] = 1.0
    cst["nE"] = Em
    cst["maskb"] = np.stack([np.tile(cur, (1, 4)), np.tile(prev, (1, 4)), diag]).astype(np.float32)
    return cst


def build_program(n_layers=DEPTH, debug=False, layer_kinds=None, upto=99):
    nc = bass.Bass("TRN2", target_bir_lowering=False)
    kinds = layer_kinds if layer_kinds is not None else [i % 4 for i in range(n_layers)]
    c = Ctx()
    dk = "ExternalOutput" if debug else "Internal"

    def din(name, shape, dt=F32):
        return nc.dram_tensor(name, list(shape), dt, kind="ExternalInput").ap()

    x = din("x", [S, D])
    W = {}
    for kind in sorted(set(kinds)):
        for nm in LAYER_W[kind]:
            W[nm] = din(nm, W_SHAPES[nm])
    moe = []
    for i in range(n_layers):
        moe.append(dict(wg=din("moe_w_group_%d" % i, [D, 4]), we=din("moe_w_expert_%d" % i, [D, 32]),
                        gate=din("moe_w_gate_%d" % i, [32, D, 512]), up=din("moe_w_up_%d" % i, [32, D, 512]),
                        down=din("moe_w_down_%d" % i, [32, 512, D])))
    ln_g = din("ln_g", [n_layers * 2, D])
    ln_b = din("ln_b", [n_layers * 2, D])
    c_ident = din("c_ident", [128, 128])
    c.rope16 = din("c_rope16", [2, 128, S])
    c.rope32 = din("c_rope32", [2, 128, S])
    c_maskb = din("c_maskb", [3, 128, 512])
    if 2 in kinds:
        c.n_cm0 = din("c_ncm0", [128, 17, 128])
        c.n_cm1 = din("c_ncm1", [128, 16, 128])
        c.n_ovl = din("c_novl", [128, 2, 64])
        c.n_F = din("c_nF", [128, 32, 64])
        c.n_E = din("c_nE", [64, 32, 128])
    gsc = nc.dram_tensor("gsc", [48, S], F32, kind="Internal").ap()
    ot_dram = nc.dram_tensor("ot_dram", [D, S], BF16, kind="Internal").ap()
    out = nc.dram_tensor("out", [S, D], F32, kind="ExternalOutput").ap()
    hmid = nc.dram_tensor("hmid", [S, D], F32, kind=dk).ap()
    hcur = nc.dram_tensor("hcur", [S, D], F32, kind=dk).ap()
    hT_a = nc.dram_tensor("hT_a", [D, S], BF16, kind=dk).ap()
    hT_b = nc.dram_tensor("hT_b", [D, S], BF16, kind=dk).ap()
    rw = nc.dram_tensor("rw", [S, 32], F32, kind=dk).ap()

    with ExitStack() as es:
        kb = KB(nc, es)
        c.ident = sb(nc, es, "ident", [128, 128], F32)
        c.ident_k = Tok()
        c.identb = sb(nc, es, "identb", [128, 128], BF16)
        c.onesb = sb(nc, es, "onesb", [128, 128], BF16)
        c.mb_cur = sb(nc, es, "mbcur", [128, 512], BF16)
        c.mb_prev = sb(nc, es, "mbprev", [128, 512], BF16)
        c.mb_diag = sb(nc, es, "mbdiag", [128, 512], BF16)
        c.const_k = Tok()
        kb.dma("sp", c.ident[:], c_ident, c.ident_k, True)
        kb.dma("pool", c.identb[:], c_ident, c.const_k, True)
        kb.dma("pool", c.mb_cur[:], c_maskb[0], c.const_k, True)
        kb.dma("pool", c.mb_prev[:], c_maskb[1], c.const_k, True)
        kb.dma("pool", c.mb_diag[:], c_maskb[2], c.const_k, True)
        kb.op("pool", lambda g: g.memset(c.onesb[:], 1.0), writes=[c.const_k])
        phase_transpose_in(kb, c, x, hT_a)
        src = x
        for i in range(n_layers):
            if upto < 1:
                break
            kind = kinds[i]
            router = (moe[i]["wg"], moe[i]["we"])
            g0, b0 = ln_g[2 * i:2 * i + 1, :], ln_b[2 * i:2 * i + 1, :]
            g1, b1 = ln_g[2 * i + 1:2 * i + 2, :], ln_b[2 * i + 1:2 * i + 2, :]
            if kind == 0:
                phase_swa(kb, c, src, hT_a, W["a_w_in"], W["a_sinks"], W["a_w_o"], g0, b0, router, hmid, hT_b, rw)
            elif kind == 1:
                phase_mla(kb, c, src, hT_a, W["b_w_down"], W["b_q_norm"], W["b_kv_norm"], W["b_w_uq"], W["b_w_ukv"], W["b_w_o"],
                          g0, b0, router, hmid, hT_b, rw)
            elif kind == 3:
                phase_diff(kb, c, 3, src, hT_a, W["d_w_in"], W["d_lq1"], W["d_lk1"], W["d_lq2"], W["d_lk2"], W["d_subln"], W["d_w_o"],
                           g0, b0, router, hmid, hT_b, rw)
            elif kind == 2:
                phase_nsa(kb, c, src, hT_a, W["c_w_in"], W["c_pos_k"], W["c_pos_v"], W["c_wk1"], W["c_wk2"], W["c_wv1"], W["c_wv2"],
                          W["c_w_o"], g0, b0, router, hmid, hT_b, rw, gsc, ot_dram)
            else:
                raise NotImplementedError
            if upto < 2:
                break
            last = i == n_layers - 1
            dst = out if last else hcur
            phase_moe(kb, c, hmid, hT_b, rw, moe[i]["gate"], moe[i]["up"], moe[i]["down"], g1, b1, dst,
                      None if last else hT_a)
            src = hcur
        kb.final_wait()
        c.n_inst = kb.n_inst
        c.log = kb.log
    return nc, c


def make_inputs(inputs, n_layers=DEPTH, layer_kinds=None):
    kinds = layer_kinds if layer_kinds is not None else [i % 4 for i in range(n_layers)]
    cst = host_constants()
    shared = {}
    for kind in sorted(set(kinds)):
        for nm in LAYER_W[kind]:
            shared[nm] = np.ascontiguousarray(np.asarray(inputs[nm], np.float32).reshape(W_SHAPES[nm]))
    for i in range(n_layers):
        shared["moe_w_group_%d" % i] = np.ascontiguousarray(inputs["moe_w_group"][i])
        shared["moe_w_expert_%d" % i] = np.ascontiguousarray(inputs["moe_w_expert"][i])
        shared["moe_w_gate_%d" % i] = np.ascontiguousarray(inputs["moe_w_gate"][i])
        shared["moe_w_up_%d" % i] = np.ascontiguousarray(inputs["moe_w_up"][i])
        shared["moe_w_down_%d" % i] = np.ascontiguousarray(inputs["moe_w_down"][i])
    shared["ln_g"] = np.ascontiguousarray(np.asarray(inputs["ln_g"], np.float32)[:n_layers].reshape(n_layers * 2, D))
    shared["ln_b"] = np.ascontiguousarray(np.asarray(inputs["ln_b"], np.float32)[:n_layers].reshape(n_layers * 2, D))
    shared["c_ident"] = cst["ident"]
    shared["c_rope16"] = cst["rope16"]
    shared["c_rope32"] = cst["rope32"]
    shared["c_maskb"] = cst["maskb"]
    if 2 in kinds:
        for nm in ["ncm0", "ncm1", "novl", "nF", "nE"]:
            shared["c_" + nm] = cst[nm]
    return shared


def kernel(**inputs):
    x = np.asarray(inputs["x"], np.float32)
    nb = x.shape[0]
    nc, _ = build_program()
    shared = make_inputs(inputs)
    in_maps = []
    for b in range(nb):
        m = dict(shared)
        m["x"] = np.ascontiguousarray(x[b])
        in_maps.append(m)
    res = run_bass_kernel_spmd(nc, in_maps, core_ids=list(range(nb)))
    return np.stack([np.asarray(r["out"], np.float32) for r in res.results], axis=0)
```

```python
import os
import numpy as np
from contextlib import ExitStack
import concourse.bass as bass
import concourse.mybir as mybir
from concourse.bass_utils import run_bass_kernel_spmd

F32 = mybir.dt.float32
BF16 = mybir.dt.bfloat16
AF = mybir.ActivationFunctionType
ALU = mybir.AluOpType
AX = mybir.AxisListType

S = 4096
D = 1024
NT = S // 128
KC = D // 128
DEPTH = 4
ALPHA = (2 * DEPTH) ** 0.25
LN_EPS = 1e-5
RMS_EPS = 1e-6
NEG = -30000.0
KSTOP = int(os.environ.get('KSTOP', '0'))
ATT_N = int(os.environ.get('ATT_N', '32'))
ATT_FIN = int(os.environ.get('ATT_FIN', '3'))
ATT_VAR = int(os.environ.get('ATT_VAR', '0'))
EPI_LVL = int(os.environ.get('EPI_LVL', '3'))
EPI_NT = int(os.environ.get('EPI_NT', '32'))
EPI_R = int(os.environ.get('EPI_R', '9'))


class Tok:
    __slots__ = ("w", "r", "lane")

    def __init__(self):
        self.w = None
        self.r = {}
        self.lane = None


class KB:
    def __init__(self, nc, es):
        self.nc = nc
        self.E = dict(pe=nc.tensor, act=nc.scalar, dve=nc.vector, pool=nc.gpsimd, sp=nc.sync)
        self.sem = {k: es.enter_context(nc.semaphore("s_" + k)) for k in self.E}
        self.cnt = {k: 0 for k in self.E}
        self.seen = {k: {} for k in self.E}
        self.es = es
        self.lanes = []
        self.free_lanes = {'hw': [], 'sw': []}
        self.pe_pending = []
        self.n_inst = 0
        self.log = {k: [] for k in self.E}

    def _deps(self, e, reads, writes, dma=False):
        need = {}

        def add(d, raw):
            if d is None:
                return
            key, sem, val = d
            if key == e and not dma and e == "pe":
                return
            cur = need.get(key)
            if cur is None or cur[1] < val:
                need[key] = (sem, val)

        for t in reads:
            add(t.w, True)
        for t in writes:
            add(t.w, False)
            for d in t.r.values():
                add(d, False)
        return need

    def _wait(self, e, need):
        seen = self.seen[e]
        for key, (sem, val) in need.items():
            if seen.get(key, 0) >= val:
                continue
            self.E[e].wait_ge(sem, val)
            self.log[e].append(('w', key, val))
            self.n_inst += 1
            seen[key] = val

    def _commit(self, e, ins, reads, writes):
        self.cnt[e] += 1
        ins.then_inc(self.sem[e], 1)
        self.log[e].append(('i', e, 1))
        d = (e, self.sem[e], self.cnt[e])
        for t in reads:
            t.r[e] = d
        for t in writes:
            t.w = d
            t.r = {}

    def op(self, e, fn, reads=(), writes=()):
        self._wait(e, self._deps(e, reads, writes))
        ins = fn(self.E[e])
        self.n_inst += 1
        if e == "pe":
            self._commit(e, ins, list(reads) + self.pe_pending, writes)
            self.pe_pending = []
        else:
            self._commit(e, ins, reads, writes)
        return ins

    def mm(self, out_tok, out, lhsT, rhs, start, stop, reads, **kw):
        self._wait("pe", self._deps("pe", reads, [out_tok]))
        ins = self.nc.tensor.matmul(out, lhsT=lhsT, rhs=rhs, start=start, stop=stop, **kw)
        self.n_inst += 1
        self.pe_pending.extend(reads)
        if stop:
            self._commit("pe", ins, self.pe_pending, [out_tok])
            self.pe_pending = []
        return ins

    def _lane(self, tok, q):
        kind = "sw" if q == "pool" else "hw"
        if tok.lane is None:
            tok.lane = {}
        if kind not in tok.lane:
            fl = self.free_lanes[kind]
            if fl:
                tok.lane[kind] = fl.pop()
            else:
                i = len(self.lanes)
                sem = self.es.enter_context(self.nc.semaphore("l%d" % i))
                ln = [sem, 0, "L%d" % i, kind]
                self.lanes.append(ln)
                tok.lane[kind] = ln
        return tok.lane[kind]

    def dma(self, q, out, in_, sb_tok, load, extra_reads=(), extra_writes=(), **kw):
        reads = list(extra_reads) + ([] if load else [sb_tok])
        writes = list(extra_writes) + ([sb_tok] if load else [])
        self._wait(q, self._deps(q, reads, writes, dma=True))
        ln = self._lane(sb_tok, q)
        ins = self.E[q].dma_start(out=out, in_=in_, **kw)
        self.n_inst += 1
        ln[1] += 16
        ins.then_inc(ln[0], 16)
        self.log[q].append(('i', ln[2], 16))
        d = (ln[2], ln[0], ln[1])
        for t in reads:
            t.r[d[0]] = d
        for t in writes:
            t.w = d
            t.r = {}
        return ins

    def barrier(self, toks=()):
        for e in self.E:
            need = {}
            for p in self.E:
                if p != e and self.cnt[p] > 0:
                    need[p] = (self.sem[p], self.cnt[p])
            for ln in self.lanes:
                if ln[1] > 0:
                    need[ln[2]] = (ln[0], ln[1])
            self._wait(e, need)
        self.free_lanes = {'hw': [l for l in self.lanes if l[3] == 'hw'], 'sw': [l for l in self.lanes if l[3] == 'sw']}

    def final_wait(self):
        self.barrier()


class Ctx:
    pass


_UNIQ = [0]


def sb(nc, es, name, shape, dt):
    _UNIQ[0] += 1
    return es.enter_context(nc.sbuf_tensor("%s_%d" % (name, _UNIQ[0]), list(shape), dt))


def ps(nc, es, name, shape, dt=F32):
    _UNIQ[0] += 1
    return es.enter_context(nc.psum_tensor("%s_%d" % (name, _UNIQ[0]), list(shape), dt))


def phase_transpose_in(kb, c, x, hT_dst):
    nc = kb.nc
    with ExitStack() as es:
        xt = [(sb(nc, es, "p0x%d" % i, [128, D], F32), Tok()) for i in range(2)]
        hb = [(sb(nc, es, "p0h%d" % i, [128, KC, 128], BF16), Tok()) for i in range(2)]
        pt = [(ps(nc, es, "p0t%d" % i, [128, D]), Tok()) for i in range(2)]
        hT3 = hT_dst.rearrange("(c p) t -> p c t", p=128)
        for tt in range(NT):
            xb, xk = xt[tt % 2]
            hbb, hk = hb[tt % 2]
            pb, pk = pt[tt % 2]
            kb.dma("sp", xb[:], x[tt * 128:(tt + 1) * 128, :], xk, True)
            for cc in range(KC):
                kb.op("pe", lambda pe, cc=cc: pe.transpose(out=pb[:, cc * 128:(cc + 1) * 128],
                                                         in_=xb[:, cc * 128:(cc + 1) * 128], identity=c.ident[:]),
                      reads=[xk, c.ident_k], writes=[pk])
            kb.op("act", lambda a: a.activation(out=hbb[:].rearrange("p c t -> p (c t)"), in_=pb[:], func=AF.Copy),
                  reads=[pk], writes=[hk])
            kb.dma("sp", hT3[:, :, tt * 128:(tt + 1) * 128], hbb[:], hk, False)
        kb.barrier()


class Epilogue:
    def __init__(self, kb, c, es, src_h, dst_h, dst_hT, ln_g, ln_b, router=None, rw_dst=None, n_ps_t=1):
        nc = kb.nc
        self.kb, self.c = kb, c
        self.src_h, self.dst_h, self.dst_hT = src_h, dst_h, dst_hT
        self.router = router
        self.rw_dst = rw_dst
        self.G = sb(nc, es, "epG", [128, D], F32)
        self.B = sb(nc, es, "epB", [128, D], F32)
        self.gb_k = Tok()
        kb.dma("sp", self.G[:], ln_g.partition_broadcast(128), self.gb_k, True)
        kb.dma("sp", self.B[:], ln_b.partition_broadcast(128), self.gb_k, True)
        self.ht = [(sb(nc, es, "epht%d" % i, [128, D], F32), Tok()) for i in range(2)]
        self.z = [(sb(nc, es, "epz%d" % i, [128, D], F32), Tok()) for i in range(2)]
        self.zn = [(sb(nc, es, "epzn%d" % i, [128, D], F32), Tok()) for i in range(2)]
        self.st = [(sb(nc, es, "epst%d" % i, [128, 2, 6], F32), Tok()) for i in range(2)]
        self.mv = [(sb(nc, es, "epmv%d" % i, [128, 4], F32), Tok()) for i in range(2)]
        self.pt = [(ps(nc, es, "eppt%d" % i, [128, D]), Tok()) for i in range(n_ps_t)]
        if dst_hT is not None:
            self.hb = [(sb(nc, es, "ephb%d" % i, [128, KC, 128], BF16), Tok()) for i in range(2)]
            self.hT3 = dst_hT.rearrange("(c p) t -> p c t", p=128)
        if router is not None:
            self.wr = sb(nc, es, "epwr", [128, KC, 36], F32)
            self.wr_k = Tok()
            wg, we = router
            kb.dma("sp", self.wr[:, :, 0:4], wg.rearrange("(c p) n -> p c n", p=128), self.wr_k, True)
            kb.dma("sp", self.wr[:, :, 4:36], we.rearrange("(c p) n -> p c n", p=128), self.wr_k, True)
            self.wrh = sb(nc, es, "epwrh", [128, KC, 36], BF16)
            self.wrl = sb(nc, es, "epwrl", [128, KC, 36], BF16)
            self.wrh_k = Tok()
            kb.op("dve", lambda v: v.tensor_copy(out=self.wrh[:], in_=self.wr[:]), reads=[self.wr_k], writes=[self.wrh_k])
            kb.op("dve", lambda v: v.tensor_tensor(out=self.wrl[:], in0=self.wr[:], in1=self.wrh[:], op=ALU.subtract),
                  reads=[self.wr_k, self.wrh_k], writes=[self.wrh_k])
            self.hlo = [(sb(nc, es, "ephlo%d" % i, [128, KC, 128], BF16), Tok()) for i in range(2)]
            self.pr = (ps(nc, es, "eppr", [128, 512]), Tok())
            self.rt = [(sb(nc, es, "eprt%d" % i, [128, 160], F32), Tok()) for i in range(2)]
            self.rw = [(sb(nc, es, "eprw%d" % i, [128, 32], F32), Tok()) for i in range(2)]
        self.n = 0

    def prefetch(self, tt):
        hb_, hk = self.ht[tt % 2]
        self.kb.dma("sp", hb_[:], self.src_h[tt * 128:(tt + 1) * 128, :], hk, True)

    def run(self, tt, y_ap, y_tok, prefetched=False):
        kb, c = self.kb, self.c
        i = self.n % 2
        self.n += 1
        if not prefetched:
            self.prefetch(tt)
        hb_, hk = self.ht[tt % 2]
        z, zk = self.z[i]
        zn, znk = self.zn[i]
        st, stk = self.st[i]
        mv, mvk = self.mv[i]
        kb.op("dve", lambda v: v.scalar_tensor_tensor(out=z[:], in0=hb_[:], scalar=ALPHA, in1=y_ap,
                                                      op0=ALU.mult, op1=ALU.add), reads=[hk, y_tok], writes=[zk])
        kb.op("dve", lambda v: v.bn_stats(out=st[:, 0, :], in_=z[:, 0:512]), reads=[zk], writes=[stk])
        kb.op("dve", lambda v: v.bn_stats(out=st[:, 1, :], in_=z[:, 512:1024]), reads=[zk], writes=[stk])
        kb.op("dve", lambda v: v.bn_aggr(out=mv[:, 0:2], in_=st[:].rearrange("p a b -> p (a b)")),
              reads=[stk], writes=[mvk])
        kb.op("dve", lambda v: v.tensor_scalar(out=mv[:, 2:3], in0=mv[:, 1:2], scalar1=LN_EPS, scalar2=None,
                                               op0=ALU.add), reads=[mvk], writes=[mvk])
        kb.op("act", lambda a: a.activation(out=mv[:, 2:3], in_=mv[:, 2:3], func=AF.Sqrt), reads=[mvk], writes=[mvk])
        kb.op("dve", lambda v: v.reciprocal(out=mv[:, 2:3], in_=mv[:, 2:3]), reads=[mvk], writes=[mvk])
        kb.op("dve", lambda v: v.scalar_tensor_tensor(out=mv[:, 3:4], in0=mv[:, 0:1], scalar=-1.0, in1=mv[:, 2:3],
                                                      op0=ALU.mult, op1=ALU.mult), reads=[mvk], writes=[mvk])
        kb.op("act", lambda a: a.activation(out=zn[:], in_=z[:], func=AF.Identity, scale=mv[:, 2:3], bias=mv[:, 3:4]),
              reads=[zk, mvk], writes=[znk])
        kb.op("pool", lambda g: g.tensor_tensor(out=zn[:], in0=zn[:], in1=self.G[:], op=ALU.mult),
              reads=[znk, self.gb_k], writes=[znk])
        kb.op("pool", lambda g: g.tensor_tensor(out=zn[:], in0=zn[:], in1=self.B[:], op=ALU.add),
              reads=[znk, self.gb_k], writes=[znk])
        kb.dma("sp", self.dst_h[tt * 128:(tt + 1) * 128, :], zn[:], znk, False)
        if self.dst_hT is None or EPI_LVL < 2:
            return
        pb, pk = self.pt[self.n % len(self.pt)]
        for cc in range(KC):
            kb.op("pe", lambda pe, cc=cc: pe.transpose(out=pb[:, cc * 128:(cc + 1) * 128],
                                                     in_=zn[:, cc * 128:(cc + 1) * 128], identity=c.ident[:]),
                  reads=[znk, c.ident_k], writes=[pk])
        hbb, hbk = self.hb[i]
        kb.op("act", lambda a: a.activation(out=hbb[:].rearrange("p c t -> p (c t)"), in_=pb[:], func=AF.Copy),
              reads=[pk], writes=[hbk])
        kb.dma("sp", self.hT3[:, :, tt * 128:(tt + 1) * 128], hbb[:], hbk, False)
        if self.router is None or EPI_LVL < 3:
            return
        hlo, hlok = self.hlo[i]
        kb.op("dve", lambda v: v.tensor_tensor(out=hlo[:].rearrange("p c t -> p (c t)"), in0=pb[:],
                                               in1=hbb[:].rearrange("p c t -> p (c t)"), op=ALU.subtract),
              reads=[pk, hbk], writes=[hlok])
        pr, prk = self.pr
        n_mm = 0
        for cc in range(KC):
            for (lh, lk, rh) in [(hbb, hbk, self.wrh), (hlo, hlok, self.wrh), (hbb, hbk, self.wrl)]:
                kb.mm(prk, pr[:, 0:36], lh[:, cc, :], rh[:, cc, :], n_mm == 0, n_mm == 3 * KC - 1, [lk, self.wrh_k])
                n_mm += 1
        self._route(tt, i)

    def _route(self, tt, i):
        kb = self.kb
        pr, prk = self.pr
        rt, rtk = self.rt[i]
        rw, rwk = self.rw[i]
        L = rt[:, 0:36]
        gmax, ngmax, gsum, gw = rt[:, 36:37], rt[:, 37:38], rt[:, 38:39], rt[:, 39:40]
        ge, gmask, pen = rt[:, 40:44], rt[:, 44:48], rt[:, 48:52]
        ml, mask1, ml2 = rt[:, 52:84], rt[:, 84:116], rt[:, 116:148]
        RK = [rtk]

        def dv(fn, reads=RK, writes=RK):
            kb.op("dve", fn, reads=reads, writes=writes)

        kb.op("act", lambda a: a.activation(out=L, in_=pr[:, 0:36], func=AF.Copy), reads=[prk], writes=[rtk])
        if EPI_R <= 1:
            return
        dv(lambda v: v.tensor_reduce(out=gmax, in_=rt[:, 0:4], axis=AX.X, op=ALU.max))
        dv(lambda v: v.tensor_scalar(out=ngmax, in0=gmax, scalar1=-1.0, scalar2=None, op0=ALU.mult))
        kb.op("act", lambda a: a.activation(out=ge, in_=rt[:, 0:4], func=AF.Exp, bias=ngmax, scale=1.0, accum_out=gsum),
              reads=RK, writes=RK)
        dv(lambda v: v.reciprocal(out=gw, in_=gsum))
        if EPI_R <= 2:
            return
        dv(lambda v: v.tensor_scalar(out=gmask, in0=rt[:, 0:4], scalar1=gmax, scalar2=None, op0=ALU.is_ge))
        dv(lambda v: v.tensor_scalar(out=pen, in0=gmask, scalar1=1e30, scalar2=-1e30, op0=ALU.mult, op1=ALU.add))
        dv(lambda v: v.tensor_tensor(out=ml.rearrange("p (g e) -> p g e", g=4),
                                     in0=rt[:, 4:36].rearrange("p (g e) -> p g e", g=4),
                                     in1=pen.unsqueeze(2).to_broadcast([128, 4, 8]), op=ALU.add))
        s_m1, s_m2, s_nm1, s_r, s_w1, s_w2, s_den = (rt[:, 40:41], rt[:, 41:42], rt[:, 42:43], rt[:, 43:44],
                                                     rt[:, 44:45], rt[:, 45:46], rt[:, 46:47])
        if EPI_R <= 3:
            return
        dv(lambda v: v.tensor_reduce(out=s_m1, in_=ml, axis=AX.X, op=ALU.max))
        dv(lambda v: v.tensor_scalar(out=mask1, in0=ml, scalar1=s_m1, scalar2=None, op0=ALU.is_ge))
        dv(lambda v: v.scalar_tensor_tensor(out=ml2, in0=mask1, scalar=-1e30, in1=ml, op0=ALU.mult, op1=ALU.add))
        dv(lambda v: v.tensor_reduce(out=s_m2, in_=ml2, axis=AX.X, op=ALU.max))
        dv(lambda v: v.tensor_scalar(out=ml, in0=ml2, scalar1=s_m2, scalar2=None, op0=ALU.is_ge))
        dv(lambda v: v.tensor_scalar(out=s_nm1, in0=s_m1, scalar1=-1.0, scalar2=None, op0=ALU.mult))
        kb.op("act", lambda a: a.activation(out=s_r, in_=s_m2, func=AF.Exp, bias=s_nm1, scale=1.0), reads=RK, writes=RK)
        dv(lambda v: v.tensor_scalar(out=s_den, in0=s_r, scalar1=1.0, scalar2=None, op0=ALU.add))
        dv(lambda v: v.reciprocal(out=s_den, in_=s_den))
        dv(lambda v: v.tensor_tensor(out=s_w1, in0=s_den, in1=gw, op=ALU.mult))
        dv(lambda v: v.tensor_tensor(out=s_w2, in0=s_w1, in1=s_r, op=ALU.mult))
        dv(lambda v: v.tensor_scalar(out=mask1, in0=mask1, scalar1=s_w1, scalar2=None, op0=ALU.mult))
        kb.op("dve", lambda v: v.scalar_tensor_tensor(out=rw[:], in0=ml, scalar=s_w2, in1=mask1, op0=ALU.mult, op1=ALU.add),
              reads=RK, writes=[rwk])
        kb.dma("sp", self.rw_dst[tt * 128:(tt + 1) * 128, :], rw[:], rwk, False)


def phase_moe(kb, c, hmid, hT_src, rw_src, w_gate, w_up, w_down, ln_g, ln_b, dst_h, dst_hT):
    nc = kb.nc
    GT = 8
    GN = GT * 128
    with ExitStack() as es:
        epi = Epilogue(kb, c, es, hmid, dst_h, dst_hT, ln_g, ln_b)
        hTg = (sb(nc, es, "mhT", [128, KC, GN], BF16), Tok())
        rwg = (sb(nc, es, "mrw", [128, GT, 32], F32), Tok())
        yacc = [(sb(nc, es, "myacc%d" % t, [128, D], F32), Tok()) for t in range(GT)]
        wg = [(sb(nc, es, "mwg%d" % i, [128, KC, 512], BF16), Tok()) for i in range(2)]
        wu = [(sb(nc, es, "mwu%d" % i, [128, KC, 512], BF16), Tok()) for i in range(2)]
        wd = [(sb(nc, es, "mwd%d" % i, [128, 4, D], BF16), Tok()) for i in range(2)]
        sg = [(sb(nc, es, "msg%d" % i, [128, 512], F32), Tok()) for i in range(2)]
        h1 = [(sb(nc, es, "mh1%d" % i, [128, 4, 512], BF16), Tok()) for i in range(2)]
        pg = [(ps(nc, es, "mpg%d" % i, [128, 512]), Tok()) for i in range(2)]
        pu = [(ps(nc, es, "mpu%d" % i, [128, 512]), Tok()) for i in range(2)]
        pd = [(ps(nc, es, "mpd%d" % i, [128, 512]), Tok()) for i in range(2)]
        hT3 = hT_src.rearrange("(c p) t -> p c t", p=128)
        n_e = 32
        seq = [(g, e) for g in range(S // GN) for e in range(n_e)]

        def load_w(idx):
            g, e = seq[idx]
            b = idx % 2
            kb.dma("pool", wg[b][0][:], w_gate[e].rearrange("(c p) n -> p c n", p=128), wg[b][1], True)
            kb.dma("pool", wu[b][0][:], w_up[e].rearrange("(c p) n -> p c n", p=128), wu[b][1], True)
            kb.dma("pool", wd[b][0][:], w_down[e].rearrange("(c p) n -> p c n", p=128), wd[b][1], True)

        load_w(0)
        cnt_gu = 0
        cnt_d = 0
        cnt_h1 = 0
        for idx, (g, e) in enumerate(seq):
            if e == 0:
                kb.dma("sp", hTg[0][:], hT3[:, :, g * GN:(g + 1) * GN], hTg[1], True)
                kb.dma("sp", rwg[0][:], rw_src[g * GN:(g + 1) * GN, :].rearrange("(t p) e -> p t e", p=128), rwg[1], True)
            if idx + 1 < len(seq):
                load_w(idx + 1)
            b = idx % 2
            wgb, wgk = wg[b]
            wub, wuk = wu[b]
            wdb, wdk = wd[b]
            for s_ in range(GN // 512):
                h1b, h1k = h1[cnt_h1 % 2]
                cnt_h1 += 1
                for hc in range(4):
                    pgb, pgk = pg[cnt_gu % 2]
                    pub, puk = pu[cnt_gu % 2]
                    sgb, sgk = sg[cnt_gu % 2]
                    cnt_gu += 1
                    for kc in range(KC):
                        kb.mm(pgk, pgb[:], wgb[:, kc, hc * 128:(hc + 1) * 128], hTg[0][:, kc, s_ * 512:(s_ + 1) * 512],
                              kc == 0, kc == KC - 1, [wgk, hTg[1]])
                    for kc in range(KC):
                        kb.mm(puk, pub[:], wub[:, kc, hc * 128:(hc + 1) * 128], hTg[0][:, kc, s_ * 512:(s_ + 1) * 512],
                              kc == 0, kc == KC - 1, [wuk, hTg[1]])
                    kb.op("act", lambda a: a.activation(out=sgb[:], in_=pgb[:], func=AF.Silu), reads=[pgk], writes=[sgk])
                    kb.op("dve", lambda v, hc=hc: v.tensor_tensor(out=h1b[:, hc, :], in0=sgb[:], in1=pub[:], op=ALU.mult),
                          reads=[sgk, puk], writes=[h1k])
                for t4 in range(4):
                    tl = s_ * 4 + t4
                    ya, yk = yacc[tl]
                    for half in range(2):
                        pdb, pdk = pd[cnt_d % 2]
                        cnt_d += 1
                        for hc in range(4):
                            kb.mm(pdk, pdb[:], h1b[:, hc, t4 * 128:(t4 + 1) * 128], wdb[:, hc, half * 512:(half + 1) * 512],
                                  hc == 0, hc == 3, [h1k, wdk])
                        wcol = rwg[0][:, tl, e:e + 1]
                        if e == 0:
                            kb.op("dve", lambda v, half=half, wcol=wcol, pdb=pdb, ya=ya: v.tensor_scalar(
                                out=ya[:, half * 512:(half + 1) * 512], in0=pdb[:], scalar1=wcol, scalar2=None, op0=ALU.mult),
                                reads=[pdk, rwg[1]], writes=[yk])
                        else:
                            kb.op("dve", lambda v, half=half, wcol=wcol, pdb=pdb, ya=ya: v.scalar_tensor_tensor(
                                out=ya[:, half * 512:(half + 1) * 512], in0=pdb[:], scalar=wcol,
                                in1=ya[:, half * 512:(half + 1) * 512], op0=ALU.mult, op1=ALU.add),
                                reads=[pdk, rwg[1], yk], writes=[yk])
            if e == n_e - 1:
                for tl in range(GT):
                    epi.run(g * GT + tl, yacc[tl][0][:], yacc[tl][1])
        kb.barrier()


def load_rope(kb, c, es, dram_tab):
    nc = kb.nc
    cos = sb(nc, es, "ropec", [128, S], F32)
    sin = sb(nc, es, "ropes", [128, S], F32)
    k = Tok()
    kb.dma("sp", cos[:], dram_tab[0], k, True)
    kb.dma("sp", sin[:], dram_tab[1], k, True)
    return cos, sin, k


def make_partner(kb, wsrc, wk_, wdst, wdk, ncols, rot_half, head_dim):
    nh = ncols // head_dim
    kb.op("pool", lambda g: g.tensor_copy(out=wdst[:, :, 0:ncols], in_=wsrc[:, :, 0:ncols]), reads=[wk_], writes=[wdk])
    for kc in range(KC):
        sv = wsrc[:, kc, 0:ncols].rearrange("p (h d) -> p h d", d=head_dim)
        dv = wdst[:, kc, 0:ncols].rearrange("p (h d) -> p h d", d=head_dim)
        kb.op("pool", lambda g, sv=sv, dv=dv: g.tensor_copy(out=dv[:, :, 0:rot_half], in_=sv[:, :, rot_half:2 * rot_half]),
              reads=[wk_], writes=[wdk])
        kb.op("pool", lambda g, sv=sv, dv=dv: g.tensor_copy(out=dv[:, :, rot_half:2 * rot_half], in_=sv[:, :, 0:rot_half]),
              reads=[wk_], writes=[wdk])


def load_half_weight(kb, dst, dk_, w3, col0, half, ncols=64):
    kb.op("pool", lambda g: g.memset(dst[:], 0.0), writes=[dk_])
    kb.dma("pool", dst[:, :, half * 64:half * 64 + ncols], w3[:, :, col0:col0 + ncols], dk_, True)


def proj_rope(kb, w, wk_, wp, wpk, col0, hblk, hblk_k, pq, pqp, cos_blk, sin_blk, tab_k, tmp1, tmp2, out_ap, out_k, nrows=128):
    for kc in range(KC):
        kb.mm(pq[1], pq[0][0:nrows, :], w[:, kc, col0:col0 + nrows], hblk[:, kc, :], kc == 0, kc == KC - 1, [wk_, hblk_k])
    for kc in range(KC):
        kb.mm(pqp[1], pqp[0][0:nrows, :], wp[:, kc, col0:col0 + nrows], hblk[:, kc, :], kc == 0, kc == KC - 1, [wpk, hblk_k])
    kb.op("dve", lambda v: v.tensor_tensor(out=tmp1[0][0:nrows, :], in0=pq[0][0:nrows, :], in1=cos_blk, op=ALU.mult),
          reads=[pq[1], tab_k], writes=[tmp1[1]])
    kb.op("dve", lambda v: v.tensor_tensor(out=tmp2[0][0:nrows, :], in0=pqp[0][0:nrows, :], in1=sin_blk, op=ALU.mult),
          reads=[pqp[1], tab_k], writes=[tmp2[1]])
    kb.op("pool", lambda g: g.tensor_tensor(out=out_ap, in0=tmp1[0][0:nrows, :], in1=tmp2[0][0:nrows, :], op=ALU.add),
          reads=[tmp1[1], tmp2[1]], writes=[out_k])


def outproj_epilogue(kb, c, OT, w_o_rows_ap, src_h, dst_h, dst_hT, ln_g, ln_b, router, rw_dst, ot_dram=None):
    nc = kb.nc
    with ExitStack() as es:
        epi = Epilogue(kb, c, es, src_h, dst_h, dst_hT, ln_g, ln_b, router=router, rw_dst=rw_dst)
        wo = (sb(nc, es, "wo", [128, KC, D], BF16), Tok())
        kb.dma("pool", wo[0][:], w_o_rows_ap, wo[1], True)
        py = [(ps(nc, es, "py%d" % i, [128, D]), Tok()) for i in range(2)]
        if ot_dram is not None:
            otb = [(sb(nc, es, "otb%d" % i, [128, KC, 128], BF16), Tok()) for i in range(2)]
            ot3 = ot_dram.rearrange("(c p) t -> p c t", p=128)
            kb.dma("sp", otb[0][0][:], ot3[:, :, 0:128], otb[0][1], True)
        for tt in range(min(NT, EPI_NT)):
            epi.prefetch(tt)
            if ot_dram is not None and tt + 1 < NT:
                kb.dma("sp", otb[(tt + 1) % 2][0][:], ot3[:, :, (tt + 1) * 128:(tt + 2) * 128], otb[(tt + 1) % 2][1], True)
            pyb, pyk = py[tt % 2]
            for half in range(2):
                for cc in range(KC):
                    if ot_dram is not None:
                        lhsT, lk = otb[tt % 2][0][:, cc, :], otb[tt % 2][1]
                    else:
                        lhsT, lk = OT[0][:, cc, tt * 128:(tt + 1) * 128], OT[1]
                    kb.mm(pyk, pyb[:, half * 512:(half + 1) * 512], lhsT,
                          wo[0][:, cc, half * 512:(half + 1) * 512], cc == 0, cc == KC - 1, [lk, wo[1]])
            if EPI_LVL >= 1:
                epi.run(tt, pyb[:], pyk, prefetched=True)
        kb.barrier()


def phase_swa(kb, c, src_h, hT_src, w_in, sinks, w_o, ln_g, ln_b, router, dst_h, dst_hT, rw_dst):
    nc = kb.nc
    hT3 = hT_src.rearrange("(c p) t -> p c t", p=128)
    w3 = w_in.rearrange("(c p) n -> p c n", p=128)
    with ExitStack() as es0:
        OT = (sb(nc, es0, "OT", [128, KC, S], BF16), Tok())
        with ExitStack() as es:
            cos, sin, tab_k = load_rope(kb, c, es, c.rope16)
            hblk = [(sb(nc, es, "hblk%d" % i, [128, KC, 512], BF16), Tok()) for i in range(2)]
            QT = (sb(nc, es, "QT", [128, 2, S], BF16), Tok())
            KT2 = [(sb(nc, es, "KT%d" % i, [128, S], BF16), Tok()) for i in range(2)]
            V = (sb(nc, es, "V", [128, NT, 128], BF16), Tok())
            wq = (sb(nc, es, "wq", [128, KC, 256], BF16), Tok())
            wqp = (sb(nc, es, "wqp", [128, KC, 256], BF16), Tok())
            wk2 = [(sb(nc, es, "wk%d" % i, [128, KC, 128], BF16), Tok()) for i in range(2)]
            wkp2 = [(sb(nc, es, "wkp%d" % i, [128, KC, 128], BF16), Tok()) for i in range(2)]
            wv = (sb(nc, es, "wv", [128, KC, 128], BF16), Tok())
            tmp1 = [(sb(nc, es, "t1_%d" % i, [128, 512], F32), Tok()) for i in range(2)]
            tmp2 = [(sb(nc, es, "t2_%d" % i, [128, 512], F32), Tok()) for i in range(2)]
            es16 = (sb(nc, es, "es16", [128, 16], F32), Tok())
            esx = (sb(nc, es, "esx", [128, 4, 512], F32), Tok())
            PT = [(sb(nc, es, "PT%d" % i, [128, 512], BF16), Tok()) for i in range(3)]
            den = [(sb(nc, es, "den%d" % i, [128, 512], F32), Tok()) for i in range(2)]
            pq = [(ps(nc, es, "pq%d" % i, [128, 512]), Tok()) for i in range(2)]
            pqp = [(ps(nc, es, "pqp%d" % i, [128, 512]), Tok()) for i in range(2)]
            pss = [(ps(nc, es, "pss%d" % i, [128, 512]), Tok()) for i in range(2)]
            pso = (ps(nc, es, "pso", [128, 512]), Tok())
            psm = (ps(nc, es, "psm", [128, 512]), Tok())
            kb.dma("sp", es16[0][:], sinks.partition_broadcast(128), es16[1], True)
            kb.op("act", lambda a: a.activation(out=es16[0][:], in_=es16[0][:], func=AF.Exp), reads=[es16[1]], writes=[es16[1]])
            for hk in range(4):
                kb.op("dve", lambda v, hk=hk: v.tensor_copy(
                    out=esx[0][:, hk, :].rearrange("p (g i) -> p g i", g=4),
                    in_=es16[0][:, 4 * hk:4 * hk + 4].unsqueeze(2).to_broadcast([128, 4, 128])),
                    reads=[es16[1]], writes=[esx[1]])
            nq = 0
            npt = 0
            for hk in range(4 if KSTOP != 5 else 0):
                kb.dma("pool", wq[0][:], w3[:, :, hk * 256:(hk + 1) * 256], wq[1], True)
                for hf in range(2):
                    load_half_weight(kb, wk2[hf][0], wk2[hf][1], w3, 1024 + hk * 64, hf)
                    kb.dma("pool", wv[0][:, :, hf * 64:(hf + 1) * 64], w3[:, :, 1280 + hk * 64:1280 + (hk + 1) * 64], wv[1], True)
                make_partner(kb, wq[0], wq[1], wqp[0], wqp[1], 256, 8, 64)
                for hf in range(2):
                    make_partner(kb, wk2[hf][0], wk2[hf][1], wkp2[hf][0], wkp2[hf][1], 128, 8, 64)
                if KSTOP == 1:
                    kb.barrier(); return
                kb.dma("sp", hblk[0][0][:], hT3[:, :, 0:512], hblk[0][1], True)
                for blk in range(8):
                    if blk + 1 < 8:
                        kb.dma("sp", hblk[(blk + 1) % 2][0][:], hT3[:, :, (blk + 1) * 512:(blk + 2) * 512], hblk[(blk + 1) % 2][1], True)
                    hb_, hbk = hblk[blk % 2]
                    bs = slice(blk * 512, (blk + 1) * 512)
                    for cc in range(2 if KSTOP != 23 else 0):
                        proj_rope(kb, wq[0], wq[1], wqp[0], wqp[1], cc * 128, hb_, hbk, pq[nq % 2], pqp[nq % 2],
                                  cos[:, bs], sin[:, bs], tab_k, tmp1[nq % 2], tmp2[nq % 2], QT[0][:, cc, bs], QT[1])
                        nq += 1
                    if KSTOP == 21:
                        continue
                    for hf in range(2):
                        proj_rope(kb, wk2[hf][0], wk2[hf][1], wkp2[hf][0], wkp2[hf][1], 0, hb_, hbk, pq[nq % 2], pqp[nq % 2],
                                  cos[:, bs], sin[:, bs], tab_k, tmp1[nq % 2], tmp2[nq % 2], KT2[hf][0][:, bs], KT2[hf][1])
                        nq += 1
                    if KSTOP == 22:
                        continue
                    pv = pq[nq % 2]
                    nq += 1
                    for t4 in range(4):
                        for kc in range(KC):
                            kb.mm(pv[1], pv[0][:, t4 * 128:(t4 + 1) * 128], hb_[:, kc, t4 * 128:(t4 + 1) * 128], wv[0][:, kc, :],
                                  kc == 0, kc == KC - 1, [hbk, wv[1]])
                    kb.op("act", lambda a, pv=pv, blk=blk: a.activation(
                        out=V[0][:, blk * 4:(blk + 1) * 4, :].rearrange("p t d -> p (t d)"), in_=pv[0][:], func=AF.Copy),
                        reads=[pv[1]], writes=[V[1]])
                if KSTOP in (2, 21, 22, 23):
                    kb.barrier(); return
                for n in range(min(NT, ATT_N)):
                    kts = ([n - 1] if n > 0 else []) + [n]
                    qs = slice(n * 128, (n + 1) * 128)
                    for ki, kt in enumerate(kts):
                        ks = slice(kt * 128, (kt + 1) * 128)
                        sb_, sk = pss[npt % 2]
                        mb = c.mb_cur if kt == n else c.mb_prev
                        kb.mm(sk, sb_[:], c.identb[:], mb[:], True, False, [c.const_k])
                        for g in range(4):
                            kb.mm(sk, sb_[:, g * 128:(g + 1) * 128], KT2[g % 2][0][:, ks], QT[0][:, g // 2, qs], False, g == 3,
                                  [KT2[g % 2][1], QT[1]])
                        pt_, ptk = PT[npt % 3]
                        npt += 1
                        kb.op("act", lambda a, pt_=pt_, sb_=sb_: a.activation(out=pt_[:], in_=sb_[:], func=AF.Exp, scale=0.125),
                              reads=[sk], writes=[ptk])
                        kb.mm(pso[1], pso[0][:], V[0][:, kt, :], pt_[:], ki == 0, ki == len(kts) - 1, [V[1], ptk])
                        kb.mm(psm[1], psm[0][:], c.onesb[:], pt_[:], ki == 0, ki == len(kts) - 1, [c.const_k, ptk])
                    if ATT_FIN < 1:
                        continue
                    dn, dnk = den[n % 2]
                    kb.op("dve", lambda v, dn=dn, hk=hk: v.tensor_tensor(out=dn[:], in0=psm[0][:], in1=esx[0][:, hk, :], op=ALU.add),
                          reads=[psm[1], esx[1]], writes=[dnk])
                    kb.op("dve", lambda v, dn=dn: v.reciprocal(out=dn[:], in_=dn[:]), reads=[dnk], writes=[dnk])
                    for hf in range(2 if ATT_FIN >= 3 else (1 if ATT_FIN == 2 else 0)):
                        hs = slice(hf * 64, hf * 64 + 64)
                        kb.op("dve", lambda v, dn=dn, hs=hs, hf=hf, hk=hk, qs=qs: v.tensor_tensor(
                            out=OT[0][hs, 2 * hk:2 * hk + 2, qs],
                            in0=pso[0][hs, :].rearrange("p (a b i) -> p a b i", a=2, b=2)[:, :, hf, :],
                            in1=dn[hs, :].rearrange("p (a b i) -> p a b i", a=2, b=2)[:, :, hf, :], op=ALU.mult),
                            reads=[pso[1], dnk], writes=[OT[1]])
                if KSTOP == 3:
                    kb.barrier(); return
            kb.barrier()
            if KSTOP == 4:
                return
        outproj_epilogue(kb, c, OT, w_o.rearrange("(c p) n -> p c n", p=128), src_h, dst_h, dst_hT, ln_g, ln_b, router, rw_dst)


class AttnBufs:
    def __init__(self, kb, es, n_pss=2):
        nc = kb.nc
        self.pss = [(ps(nc, es, "pss%d" % i, [128, 512]), Tok()) for i in range(n_pss)]
        self.pso = (ps(nc, es, "pso", [128, 512]), Tok())
        self.psm = (ps(nc, es, "psm", [128, 512]), Tok())
        self.PT = [(sb(nc, es, "PT%d" % i, [128, 512], BF16), Tok()) for i in range(3)]
        self.n = 0


def causal_attention(kb, c, ab, kt_lhsT, q_rhs, v_lhsT, scale, k_toks, v_toks, on_done, qgroups=range(8), bias_fn=None):
    for qg in qgroups:
        nk = 4 * qg + 4
        st = {}

        def emit_scores(kt, qg=qg, st=st):
            j = kt - 4 * qg
            lo = max(j, 0) * 128
            R = slice(lo, 512)
            sb_, sk = ab.pss[ab.n % len(ab.pss)]
            pt_, ptk = ab.PT[ab.n % 3]
            ab.n += 1
            first = True
            if j >= 0:
                kb.mm(sk, sb_[:, R], c.identb[:], c.mb_diag[:, 0:512 - lo], True, False, [c.const_k])
                first = False
            if bias_fn is not None:
                first = bias_fn(sk, sb_, kt, qg, lo, first)
            kb.mm(sk, sb_[:, R], kt_lhsT(kt), q_rhs(qg * 512 + lo, (qg + 1) * 512), first, True, k_toks)
            kb.op("act", lambda a: a.activation(out=pt_[:, R], in_=sb_[:, R], func=AF.Exp, scale=scale),
                  reads=[sk], writes=[ptk])
            st[kt] = (pt_, ptk, R)

        emit_scores(0)
        for kt in range(nk):
            if kt + 1 < nk:
                emit_scores(kt + 1)
            pt_, ptk, R = st.pop(kt)
            kb.mm(ab.pso[1], ab.pso[0][:, R], v_lhsT(kt), pt_[:, R], kt == 0, kt == nk - 1, list(v_toks) + [ptk])
            kb.mm(ab.psm[1], ab.psm[0][:, R], c.onesb[:], pt_[:, R], kt == 0, kt == nk - 1, [c.const_k, ptk])
        on_done(qg)


def phase_mla(kb, c, src_h, hT_src, w_down, q_norm, kv_norm, w_uq, w_ukv, w_o, ln_g, ln_b, router, dst_h, dst_hT, rw_dst):
    nc = kb.nc
    hT3 = hT_src.rearrange("(c p) t -> p c t", p=128)
    wd3 = w_down.rearrange("(c p) n -> p c n", p=128)
    with ExitStack() as es0:
        OT = (sb(nc, es0, "OT", [128, KC, S], BF16), Tok())
        with ExitStack() as es1:
            cqn = (sb(nc, es1, "cqn", [128, 3, S], BF16), Tok())
            ckvn = (sb(nc, es1, "ckvn", [128, 2, S], BF16), Tok())
            KRT = (sb(nc, es1, "KRT", [128, S], BF16), Tok())
            cosb = sb(nc, es1, "cosb", [128, S], BF16)
            sinb = sb(nc, es1, "sinb", [128, S], BF16)
            tab_k = Tok()
            kb.dma("pool", cosb[:], c.rope32[0], tab_k, True)
            kb.dma("pool", sinb[:], c.rope32[1], tab_k, True)
            with ExitStack() as es:
                hblk = [(sb(nc, es, "hblk%d" % i, [128, KC, 512], BF16), Tok()) for i in range(2)]
                wd = (sb(nc, es, "wd", [128, KC, 640], BF16), Tok())
                wkr = (sb(nc, es, "wkr", [128, KC, 128], BF16), Tok())
                wkrp = (sb(nc, es, "wkrp", [128, KC, 128], BF16), Tok())
                gq = (sb(nc, es, "gq", [128, 5], F32), Tok())
                cf = [(sb(nc, es, "cf%d" % i, [128, 512], F32), Tok()) for i in range(5)]
                sq = [(sb(nc, es, "sq%d" % i, [128, 512], BF16), Tok()) for i in range(2)]
                rs = [(sb(nc, es, "rs%d" % i, [128, 512], F32), Tok()) for i in range(2)]
                t1 = (sb(nc, es, "t1", [128, 512], F32), Tok())
                t2 = (sb(nc, es, "t2", [128, 512], F32), Tok())
                pc = [(ps(nc, es, "pc%d" % i, [128, 512]), Tok()) for i in range(3)]
                pssq = [(ps(nc, es, "pssq%d" % i, [128, 512]), Tok()) for i in range(2)]
                pkr = (ps(nc, es, "pkr", [128, 512]), Tok())
                pkrp = (ps(nc, es, "pkrp", [128, 512]), Tok())
                kb.dma("pool", wd[0][:], wd3[:, :, 0:640], wd[1], True)
                kb.op("pool", lambda g: g.memset(wkr[0][:], 0.0), writes=[wkr[1]])
                kb.op("pool", lambda g: g.memset(wkrp[0][:], 0.0), writes=[wkrp[1]])
                kb.dma("pool", wkr[0][:, :, 64:96], wd3[:, :, 640:672], wkr[1], True)
                kb.dma("pool", wkrp[0][:, :, 64:80], wd3[:, :, 656:672], wkrp[1], True)
                kb.dma("pool", wkrp[0][:, :, 80:96], wd3[:, :, 640:656], wkrp[1], True)
                kb.dma("sp", gq[0][:, 0:3], q_norm.rearrange("o (c p) -> p (o c)", p=128), gq[1], True, allow_slow_non_contiguous=True)
                kb.dma("sp", gq[0][:, 3:5], kv_norm.rearrange("o (c p) -> p (o c)", p=128), gq[1], True, allow_slow_non_contiguous=True)
                kb.dma("sp", hblk[0][0][:], hT3[:, :, 0:512], hblk[0][1], True)
                npc = 0
                for blk in range(8):
                    if blk + 1 < 8:
                        kb.dma("sp", hblk[(blk + 1) % 2][0][:], hT3[:, :, (blk + 1) * 512:(blk + 2) * 512], hblk[(blk + 1) % 2][1], True)
                    hb_, hbk = hblk[blk % 2]
                    bs = slice(blk * 512, (blk + 1) * 512)
                    for grp, (ocs, dst, nrm) in enumerate([((0, 1, 2), cqn, 384.0), ((3, 4), ckvn, 256.0)]):
                        pq_, pqk = pssq[grp]
                        for ii, oc in enumerate(ocs):
                            pcb, pck = pc[npc % 3]
                            npc += 1
                            for kc in range(KC):
                                kb.mm(pck, pcb[:], wd[0][:, kc, oc * 128:(oc + 1) * 128], hb_[:, kc, :], kc == 0, kc == KC - 1, [wd[1], hbk])
                            cfb, cfk = cf[oc]
                            sqb, sqk = sq[npc % 2]
                            kb.op("act", lambda a, cfb=cfb, pcb=pcb: a.activation(out=cfb[:], in_=pcb[:], func=AF.Copy), reads=[pck], writes=[cfk])
                            kb.op("dve", lambda v, sqb=sqb, pcb=pcb, cfb=cfb: v.tensor_tensor(out=sqb[:], in0=pcb[:], in1=cfb[:], op=ALU.mult),
                                  reads=[pck, cfk], writes=[sqk])
                            kb.mm(pqk, pq_[:], c.onesb[:], sqb[:], ii == 0, ii == len(ocs) - 1, [c.const_k, sqk])
                        rsb, rsk = rs[grp]
                        kb.op("dve", lambda v, rsb=rsb, pq_=pq_, nrm=nrm: v.tensor_scalar(out=rsb[:], in0=pq_[:], scalar1=1.0 / nrm, scalar2=RMS_EPS,
                                                                                         op0=ALU.mult, op1=ALU.add), reads=[pqk], writes=[rsk])
                        kb.op("act", lambda a, rsb=rsb: a.activation(out=rsb[:], in_=rsb[:], func=AF.Sqrt), reads=[rsk], writes=[rsk])
                        kb.op("dve", lambda v, rsb=rsb: v.reciprocal(out=rsb[:], in_=rsb[:]), reads=[rsk], writes=[rsk])
                        for ii, oc in enumerate(ocs):
                            cfb, cfk = cf[oc]
                            kb.op("dve", lambda v, cfb=cfb, rsb=rsb, oc=oc, ii=ii, dst=dst: v.scalar_tensor_tensor(
                                out=dst[0][:, ii, bs], in0=cfb[:], scalar=gq[0][:, oc:oc + 1], in1=rsb[:], op0=ALU.mult, op1=ALU.mult),
                                reads=[cfk, rsk, gq[1]], writes=[dst[1]])
                    for kc in range(KC):
                        kb.mm(pkr[1], pkr[0][:], wkr[0][:, kc, :], hb_[:, kc, :], kc == 0, kc == KC - 1, [wkr[1], hbk])
                    for kc in range(KC):
                        kb.mm(pkrp[1], pkrp[0][:], wkrp[0][:, kc, :], hb_[:, kc, :], kc == 0, kc == KC - 1, [wkrp[1], hbk])
                    rr = slice(64, 96)
                    kb.op("dve", lambda v: v.tensor_tensor(out=t1[0][rr, :], in0=pkr[0][rr, :], in1=cosb[rr, bs], op=ALU.mult),
                          reads=[pkr[1], tab_k], writes=[t1[1]])
                    kb.op("dve", lambda v: v.tensor_tensor(out=t2[0][rr, :], in0=pkrp[0][rr, :], in1=sinb[rr, bs], op=ALU.mult),
                          reads=[pkrp[1], tab_k], writes=[t2[1]])
                    kb.op("pool", lambda g: g.tensor_tensor(out=KRT[0][rr, bs], in0=t1[0][rr, :], in1=t2[0][rr, :], op=ALU.add),
                          reads=[t1[1], t2[1]], writes=[KRT[1]])
                kb.barrier()
            with ExitStack() as es:
                wuq = (sb(nc, es, "wuq", [128, 3, 1536], BF16), Tok())
                wukv = (sb(nc, es, "wukv", [128, 2, 2048], BF16), Tok())
                wqp = [(sb(nc, es, "wqp%d" % i, [128, 3, 96], BF16), Tok()) for i in range(2)]
                wvd = [(sb(nc, es, "wvd%d" % i, [128, 2, 128], BF16), Tok()) for i in range(2)]
                QT = [(sb(nc, es, "QT%d" % i, [128, S], BF16), Tok()) for i in range(1)]
                KT = [(sb(nc, es, "KT%d" % i, [128, S], BF16), Tok()) for i in range(2)]
                V = [(sb(nc, es, "V%d" % i, [128, NT, 128], BF16), Tok()) for i in range(2)]
                t1 = (sb(nc, es, "t1", [128, 512], F32), Tok())
                t2 = (sb(nc, es, "t2", [128, 512], F32), Tok())
                den = [(sb(nc, es, "den%d" % i, [128, 512], F32), Tok()) for i in range(1)]
                ab = AttnBufs(kb, es)
                pq = (ps(nc, es, "pq", [128, 512]), Tok())
                pqp = (ps(nc, es, "pqp", [128, 512]), Tok())
                pk = (ps(nc, es, "pk", [128, 512]), Tok())
                pv = (ps(nc, es, "pv", [128, 512]), Tok())
                kb.dma("pool", wuq[0][:], w_uq.rearrange("(c p) n -> p c n", p=128), wuq[1], True)
                kb.dma("pool", wukv[0][:], w_ukv.rearrange("(c p) n -> p c n", p=128), wukv[1], True)
                scale = 96.0 ** -0.5
                for h in range(16):
                    b = h % 2
                    wqpb, wqpk = wqp[b]
                    wvdb, wvdk = wvd[b]
                    QTb, QTk = QT[0]
                    KTb, KTk = KT[b]
                    Vb, Vk = V[b]
                    c0 = h * 96
                    kb.op("pool", lambda g: g.tensor_copy(out=wqpb[:, :, 0:64], in_=wuq[0][:, :, c0:c0 + 64]), reads=[wuq[1]], writes=[wqpk])
                    kb.op("pool", lambda g: g.tensor_copy(out=wqpb[:, :, 64:80], in_=wuq[0][:, :, c0 + 80:c0 + 96]), reads=[wuq[1]], writes=[wqpk])
                    kb.op("pool", lambda g: g.tensor_copy(out=wqpb[:, :, 80:96], in_=wuq[0][:, :, c0 + 64:c0 + 80]), reads=[wuq[1]], writes=[wqpk])
                    for hf in range(2):
                        kb.op("pool", lambda g, hf=hf: g.tensor_copy(out=wvdb[:, :, hf * 64:(hf + 1) * 64],
                                                                     in_=wukv[0][:, :, h * 128 + 64:h * 128 + 128]), reads=[wukv[1]], writes=[wvdk])
                    kb.op("pool", lambda g: g.tensor_copy(out=KTb[64:96, :], in_=KRT[0][64:96, :]), reads=[KRT[1]], writes=[KTk])
                    for blk in range(8):
                        bs = slice(blk * 512, (blk + 1) * 512)
                        for kc in range(3):
                            kb.mm(pq[1], pq[0][0:96, :], wuq[0][:, kc, c0:c0 + 96], cqn[0][:, kc, bs], kc == 0, kc == 2, [wuq[1], cqn[1]])
                        for kc in range(3):
                            kb.mm(pqp[1], pqp[0][0:96, :], wqpb[:, kc, :], cqn[0][:, kc, bs], kc == 0, kc == 2, [wqpk, cqn[1]])
                        r96 = slice(0, 96)
                        kb.op("dve", lambda v, bs=bs: v.tensor_tensor(out=t1[0][r96, :], in0=pq[0][r96, :], in1=cosb[r96, bs], op=ALU.mult),
                              reads=[pq[1], tab_k], writes=[t1[1]])
                        kb.op("dve", lambda v, bs=bs: v.tensor_tensor(out=t2[0][r96, :], in0=pqp[0][r96, :], in1=sinb[r96, bs], op=ALU.mult),
                              reads=[pqp[1], tab_k], writes=[t2[1]])
                        kb.op("pool", lambda g, bs=bs: g.tensor_tensor(out=QTb[r96, bs], in0=t1[0][r96, :], in1=t2[0][r96, :], op=ALU.add),
                              reads=[t1[1], t2[1]], writes=[QTk])
                        for kc in range(2):
                            kb.mm(pk[1], pk[0][0:64, :], wukv[0][:, kc, h * 128:h * 128 + 64], ckvn[0][:, kc, bs], kc == 0, kc == 1, [wukv[1], ckvn[1]])
                        kb.op("act", lambda a, bs=bs: a.activation(out=KTb[0:64, bs], in_=pk[0][0:64, :], func=AF.Copy), reads=[pk[1]], writes=[KTk])
                        for t4 in range(4):
                            ts_ = slice(blk * 512 + t4 * 128, blk * 512 + (t4 + 1) * 128)
                            for kc in range(2):
                                kb.mm(pv[1], pv[0][:, t4 * 128:(t4 + 1) * 128], ckvn[0][:, kc, ts_], wvdb[:, kc, :], kc == 0, kc == 1, [ckvn[1], wvdk])
                        kb.op("act", lambda a, blk=blk: a.activation(out=Vb[:, blk * 4:(blk + 1) * 4, :].rearrange("p t d -> p (t d)"),
                                                                     in_=pv[0][:], func=AF.Copy), reads=[pv[1]], writes=[Vk])
                    hs = slice((h % 2) * 64, (h % 2) * 64 + 64)

                    def on_done(qg, hs=hs, h=h):
                        dn, dnk = den[0]
                        kb.op("dve", lambda v: v.reciprocal(out=dn[hs, :], in_=ab.psm[0][hs, :]), reads=[ab.psm[1]], writes=[dnk])
                        kb.op("dve", lambda v: v.tensor_tensor(out=OT[0][hs, h // 2, qg * 512:(qg + 1) * 512], in0=ab.pso[0][hs, :],
                                                               in1=dn[hs, :], op=ALU.mult), reads=[ab.pso[1], dnk], writes=[OT[1]])

                    causal_attention(kb, c, ab,
                                     lambda kt: KTb[0:96, kt * 128:(kt + 1) * 128],
                                     lambda a, b_: QTb[0:96, a:b_],
                                     lambda kt: Vb[:, kt, :],
                                     scale, [KTk, QTk], [Vk], on_done)
                kb.barrier()
        outproj_epilogue(kb, c, OT, w_o.rearrange("(c p) n -> p c n", p=128), src_h, dst_h, dst_hT, ln_g, ln_b, router, rw_dst)


def phase_diff(kb, c, layer_idx, src_h, hT_src, w_in, lq1, lk1, lq2, lk2, subln, w_o, ln_g, ln_b, router, dst_h, dst_hT, rw_dst):
    import math
    nc = kb.nc
    lam_init = 0.8 - 0.6 * math.exp(-0.3 * layer_idx)
    hT3 = hT_src.rearrange("(c p) t -> p c t", p=128)
    w3 = w_in.rearrange("(c p) n -> p c n", p=128)
    with ExitStack() as es0:
        OT = (sb(nc, es0, "OT", [128, KC, S], BF16), Tok())
        with ExitStack() as es:
            cosb = sb(nc, es, "cosb", [128, S], BF16)
            sinb = sb(nc, es, "sinb", [128, S], BF16)
            tab_k = Tok()
            kb.dma("pool", cosb[:], c.rope16[0], tab_k, True)
            kb.dma("pool", sinb[:], c.rope16[1], tab_k, True)
            hblk = [(sb(nc, es, "hblk%d" % i, [128, KC, 512], BF16), Tok()) for i in range(2)]
            wq = [(sb(nc, es, "wq%d" % i, [128, KC, 128], BF16), Tok()) for i in range(2)]
            wqp = [(sb(nc, es, "wqp%d" % i, [128, KC, 128], BF16), Tok()) for i in range(2)]
            wk = [(sb(nc, es, "wk%d" % i, [128, KC, 128], BF16), Tok()) for i in range(2)]
            wkp = [(sb(nc, es, "wkp%d" % i, [128, KC, 128], BF16), Tok()) for i in range(2)]
            wv = [(sb(nc, es, "wv%d" % i, [128, KC, 128], BF16), Tok()) for i in range(2)]
            QT = (sb(nc, es, "QT", [128, S], BF16), Tok())
            KT = [(sb(nc, es, "KT%d" % i, [128, S], BF16), Tok()) for i in range(2)]
            V = [(sb(nc, es, "V%d" % i, [128, NT, 128], BF16), Tok()) for i in range(1)]
            tmp1 = [(sb(nc, es, "t1_%d" % i, [128, 512], F32), Tok()) for i in range(2)]
            tmp2 = [(sb(nc, es, "t2_%d" % i, [128, 512], F32), Tok()) for i in range(2)]
            lp = (sb(nc, es, "lp", [128, 4, 64], F32), Tok())
            ls = (sb(nc, es, "ls", [128, 8], F32), Tok())
            gcol = (sb(nc, es, "gcol", [128, 1], F32), Tok())
            den = (sb(nc, es, "den", [128, 512], F32), Tok())
            o1 = (sb(nc, es, "o1", [128, 512], F32), Tok())
            o2 = (sb(nc, es, "o2", [128, 512], F32), Tok())
            sqb = (sb(nc, es, "sqb", [128, 512], BF16), Tok())
            ab = AttnBufs(kb, es)
            pq = [(ps(nc, es, "pq%d" % i, [128, 512]), Tok()) for i in range(2)]
            pqp = [(ps(nc, es, "pqp%d" % i, [128, 512]), Tok()) for i in range(2)]
            for i, v_ in enumerate([lq1, lk1, lq2, lk2]):
                kb.dma("sp", lp[0][:, i, :], v_.partition_broadcast(128), lp[1], True)
            kb.op("dve", lambda v: v.tensor_tensor(out=lp[0][:, 0, :], in0=lp[0][:, 0, :], in1=lp[0][:, 1, :], op=ALU.mult), reads=[lp[1]], writes=[lp[1]])
            kb.op("dve", lambda v: v.tensor_tensor(out=lp[0][:, 2, :], in0=lp[0][:, 2, :], in1=lp[0][:, 3, :], op=ALU.mult), reads=[lp[1]], writes=[lp[1]])
            kb.op("dve", lambda v: v.tensor_reduce(out=ls[0][:, 0:1], in_=lp[0][:, 0, :], axis=AX.X, op=ALU.add), reads=[lp[1]], writes=[ls[1]])
            kb.op("dve", lambda v: v.tensor_reduce(out=ls[0][:, 1:2], in_=lp[0][:, 2, :], axis=AX.X, op=ALU.add), reads=[lp[1]], writes=[ls[1]])
            kb.op("act", lambda a: a.activation(out=ls[0][:, 2:4], in_=ls[0][:, 0:2], func=AF.Exp), reads=[ls[1]], writes=[ls[1]])
            kb.op("dve", lambda v: v.scalar_tensor_tensor(out=ls[0][:, 4:5], in0=ls[0][:, 3:4], scalar=-lam_init, in1=ls[0][:, 2:3],
                                                          op0=ALU.add, op1=ALU.subtract), reads=[ls[1]], writes=[ls[1]])
            kb.dma("sp", gcol[0][:], subln.rearrange("o p -> p o"), gcol[1], True, allow_slow_non_contiguous=True)
            kb.op("dve", lambda v: v.tensor_scalar(out=gcol[0][:], in0=gcol[0][:], scalar1=1.0 - lam_init, scalar2=None, op0=ALU.mult),
                  reads=[gcol[1]], writes=[gcol[1]])
            nq = 0
            scale = 64.0 ** -0.5
            for h in range(8):
                b = h % 2
                kb.dma("pool", wq[b][0][:], w3[:, :, h * 128:(h + 1) * 128], wq[b][1], True)
                for hf in range(2):
                    load_half_weight(kb, wk[hf][0], wk[hf][1], w3, 1024 + h * 128 + hf * 64, hf)
                kb.dma("pool", wv[b][0][:], w3[:, :, 2048 + h * 128:2048 + (h + 1) * 128], wv[b][1], True)
                make_partner(kb, wq[b][0], wq[b][1], wqp[b][0], wqp[b][1], 128, 8, 64)
                for hf in range(2):
                    make_partner(kb, wk[hf][0], wk[hf][1], wkp[hf][0], wkp[hf][1], 128, 8, 64)
                Vb, Vk = V[0]
                kb.dma("sp", hblk[0][0][:], hT3[:, :, 0:512], hblk[0][1], True)
                for blk in range(8):
                    if blk + 1 < 8:
                        kb.dma("sp", hblk[(blk + 1) % 2][0][:], hT3[:, :, (blk + 1) * 512:(blk + 2) * 512], hblk[(blk + 1) % 2][1], True)
                    hb_, hbk = hblk[blk % 2]
                    bs = slice(blk * 512, (blk + 1) * 512)
                    proj_rope(kb, wq[b][0], wq[b][1], wqp[b][0], wqp[b][1], 0, hb_, hbk, pq[nq % 2], pqp[nq % 2],
                              cosb[:, bs], sinb[:, bs], tab_k, tmp1[nq % 2], tmp2[nq % 2], QT[0][:, bs], QT[1])
                    nq += 1
                    for hf in range(2):
                        proj_rope(kb, wk[hf][0], wk[hf][1], wkp[hf][0], wkp[hf][1], 0, hb_, hbk, pq[nq % 2], pqp[nq % 2],
                                  cosb[:, bs], sinb[:, bs], tab_k, tmp1[nq % 2], tmp2[nq % 2], KT[hf][0][:, bs], KT[hf][1])
                        nq += 1
                    pv = pq[nq % 2]
                    nq += 1
                    for t4 in range(4):
                        for kc in range(KC):
                            kb.mm(pv[1], pv[0][:, t4 * 128:(t4 + 1) * 128], hb_[:, kc, t4 * 128:(t4 + 1) * 128], wv[b][0][:, kc, :],
                                  kc == 0, kc == KC - 1, [hbk, wv[b][1]])
                    kb.op("act", lambda a, pv=pv, blk=blk, Vb=Vb: a.activation(
                        out=Vb[:, blk * 4:(blk + 1) * 4, :].rearrange("p t d -> p (t d)"), in_=pv[0][:], func=AF.Copy),
                        reads=[pv[1]], writes=[Vk])
                for qg in range(8):
                    qs = slice(qg * 512, (qg + 1) * 512)
                    for m_ in range(2):
                        ms = slice(m_ * 64, m_ * 64 + 64)
                        om = o1 if m_ == 0 else o2

                        def on_done(qg_, om=om):
                            kb.op("dve", lambda v: v.reciprocal(out=den[0][:], in_=ab.psm[0][:]), reads=[ab.psm[1]], writes=[den[1]])
                            kb.op("dve", lambda v: v.tensor_tensor(out=om[0][:], in0=ab.pso[0][:], in1=den[0][:], op=ALU.mult),
                                  reads=[ab.pso[1], den[1]], writes=[om[1]])

                        causal_attention(kb, c, ab,
                                         lambda kt, m_=m_: KT[m_][0][:, kt * 128:(kt + 1) * 128],
                                         lambda a, b_: QT[0][:, a:b_],
                                         lambda kt: Vb[:, kt, :],
                                         scale, [KT[m_][1], QT[1]], [Vk], on_done, qgroups=[qg])
                    kb.op("dve", lambda v: v.scalar_tensor_tensor(out=o1[0][:], in0=o2[0][:], scalar=ls[0][:, 4:5], in1=o1[0][:],
                                                                  op0=ALU.mult, op1=ALU.add), reads=[o1[1], o2[1], ls[1]], writes=[o1[1]])
                    kb.op("act", lambda a: a.activation(out=sqb[0][:], in_=o1[0][:], func=AF.Square), reads=[o1[1]], writes=[sqb[1]])
                    pssq, pssqk = ab.pss[ab.n % 2]
                    ab.n += 1
                    kb.mm(pssqk, pssq[:], c.onesb[:], sqb[0][:], True, True, [c.const_k, sqb[1]])
                    kb.op("dve", lambda v, pssq=pssq: v.tensor_scalar(out=den[0][:], in0=pssq[:], scalar1=1.0 / 128.0, scalar2=RMS_EPS,
                                                                     op0=ALU.mult, op1=ALU.add), reads=[pssqk], writes=[den[1]])
                    kb.op("act", lambda a: a.activation(out=den[0][:], in_=den[0][:], func=AF.Sqrt), reads=[den[1]], writes=[den[1]])
                    kb.op("dve", lambda v: v.reciprocal(out=den[0][:], in_=den[0][:]), reads=[den[1]], writes=[den[1]])
                    kb.op("dve", lambda v, h=h, qs=qs: v.scalar_tensor_tensor(out=OT[0][:, h, qs], in0=o1[0][:], scalar=gcol[0][:, 0:1], in1=den[0][:],
                                                                            op0=ALU.mult, op1=ALU.mult), reads=[o1[1], den[1], gcol[1]], writes=[OT[1]])
            kb.barrier()
        outproj_epilogue(kb, c, OT, w_o.rearrange("(c p) n -> p c n", p=128), src_h, dst_h, dst_hT, ln_g, ln_b, router, rw_dst)


def phase_nsa(kb, c, src_h, hT_src, w_in, pos_k, pos_v, wk1, wk2, wv1, wv2, w_o, ln_g, ln_b, router, dst_h, dst_hT, rw_dst, gsc, ot_dram):
    nc = kb.nc
    hT3 = hT_src.rearrange("(c p) t -> p c t", p=128)
    w3 = w_in.rearrange("(c p) n -> p c n", p=128)
    NCMP = 255
    with ExitStack() as es0:
        for hk in range(4):
            with ExitStack() as esu:
                QT = (sb(nc, esu, "QT", [128, 2, S], BF16), Tok())
                KVc = (sb(nc, esu, "KVc", [128, S], BF16), Tok())
                KsT2 = [(sb(nc, esu, "KsT%d" % i, [128, S], BF16), Tok()) for i in range(2)]
                KwT2 = [(sb(nc, esu, "KwT%d" % i, [128, S], BF16), Tok()) for i in range(2)]
                Vs = (sb(nc, esu, "Vs", [128, NT, 128], BF16), Tok())
                Vw = (sb(nc, esu, "Vw", [128, NT, 128], BF16), Tok())
                with ExitStack() as es:
                    cosb = sb(nc, es, "cosb", [128, S], BF16)
                    sinb = sb(nc, es, "sinb", [128, S], BF16)
                    tab_k = Tok()
                    kb.dma("pool", cosb[:], c.rope16[0], tab_k, True)
                    kb.dma("pool", sinb[:], c.rope16[1], tab_k, True)
                    hblk = [(sb(nc, es, "hblk%d" % i, [128, KC, 512], BF16), Tok()) for i in range(2)]
                    wq = (sb(nc, es, "wq", [128, KC, 256], BF16), Tok())
                    wqp = (sb(nc, es, "wqp", [128, KC, 256], BF16), Tok())
                    wkv = [(sb(nc, es, "wkv%d" % j, [128, KC, 128], BF16), Tok()) for j in range(3)]
                    wks2 = [(sb(nc, es, "wks%d" % j, [128, KC, 128], BF16), Tok()) for j in range(2)]
                    wksp2 = [(sb(nc, es, "wksp%d" % j, [128, KC, 128], BF16), Tok()) for j in range(2)]
                    wkw2 = [(sb(nc, es, "wkw%d" % j, [128, KC, 128], BF16), Tok()) for j in range(2)]
                    wkwp2 = [(sb(nc, es, "wkwp%d" % j, [128, KC, 128], BF16), Tok()) for j in range(2)]
                    wgt = (sb(nc, es, "wgt", [128, KC, 48], BF16), Tok())
                    gsb = [(sb(nc, es, "gsb%d" % i, [128, 512], F32), Tok()) for i in range(2)]
                    tmp1 = [(sb(nc, es, "t1_%d" % i, [128, 512], F32), Tok()) for i in range(2)]
                    tmp2 = [(sb(nc, es, "t2_%d" % i, [128, 512], F32), Tok()) for i in range(2)]
                    pq = [(ps(nc, es, "pq%d" % i, [128, 512]), Tok()) for i in range(2)]
                    pqp = [(ps(nc, es, "pqp%d" % i, [128, 512]), Tok()) for i in range(2)]
                    kb.dma("pool", wq[0][:], w3[:, :, hk * 256:(hk + 1) * 256], wq[1], True)
                    cb = lambda j: 1024 + j * 256 + hk * 64
                    kb.dma("pool", wkv[0][0][:, :, 0:64], w3[:, :, cb(0):cb(0) + 64], wkv[0][1], True)
                    kb.dma("pool", wkv[0][0][:, :, 64:128], w3[:, :, cb(1):cb(1) + 64], wkv[0][1], True)
                    for jj, j in enumerate([3, 5]):
                        for hf in range(2):
                            kb.dma("pool", wkv[1 + jj][0][:, :, hf * 64:(hf + 1) * 64], w3[:, :, cb(j):cb(j) + 64], wkv[1 + jj][1], True)
                    for hf in range(2):
                        load_half_weight(kb, wks2[hf][0], wks2[hf][1], w3, cb(2), hf)
                        load_half_weight(kb, wkw2[hf][0], wkw2[hf][1], w3, cb(4), hf)
                    if hk == 0:
                        kb.dma("pool", wgt[0][:], w3[:, :, 2560:2608], wgt[1], True)
                    make_partner(kb, wq[0], wq[1], wqp[0], wqp[1], 256, 8, 64)
                    for hf in range(2):
                        make_partner(kb, wks2[hf][0], wks2[hf][1], wksp2[hf][0], wksp2[hf][1], 128, 8, 64)
                        make_partner(kb, wkw2[hf][0], wkw2[hf][1], wkwp2[hf][0], wkwp2[hf][1], 128, 8, 64)
                    kb.dma("sp", hblk[0][0][:], hT3[:, :, 0:512], hblk[0][1], True)
                    nq = 0
                    for blk in range(8):
                        if blk + 1 < 8:
                            kb.dma("sp", hblk[(blk + 1) % 2][0][:], hT3[:, :, (blk + 1) * 512:(blk + 2) * 512], hblk[(blk + 1) % 2][1], True)
                        hb_, hbk = hblk[blk % 2]
                        bs = slice(blk * 512, (blk + 1) * 512)
                        for cc in range(2):
                            proj_rope(kb, wq[0], wq[1], wqp[0], wqp[1], cc * 128, hb_, hbk, pq[nq % 2], pqp[nq % 2],
                                      cosb[:, bs], sinb[:, bs], tab_k, tmp1[nq % 2], tmp2[nq % 2], QT[0][:, cc, bs], QT[1])
                            nq += 1
                        for (wi, wpi, dst) in [(wks2[0], wksp2[0], KsT2[0]), (wks2[1], wksp2[1], KsT2[1]),
                                               (wkw2[0], wkwp2[0], KwT2[0]), (wkw2[1], wkwp2[1], KwT2[1])]:
                            proj_rope(kb, wi[0], wi[1], wpi[0], wpi[1], 0, hb_, hbk, pq[nq % 2], pqp[nq % 2],
                                      cosb[:, bs], sinb[:, bs], tab_k, tmp1[nq % 2], tmp2[nq % 2], dst[0][:, bs], dst[1])
                            nq += 1
                        pc_ = pq[nq % 2]
                        nq += 1
                        for kc in range(KC):
                            kb.mm(pc_[1], pc_[0][:], wkv[0][0][:, kc, :], hb_[:, kc, :], kc == 0, kc == KC - 1, [wkv[0][1], hbk])
                        kb.op("act", lambda a, pc_=pc_, bs=bs: a.activation(out=KVc[0][:, bs], in_=pc_[0][:], func=AF.Copy), reads=[pc_[1]], writes=[KVc[1]])
                        for (wi, dst) in [(wkv[1], Vs), (wkv[2], Vw)]:
                            pv = pqp[nq % 2]
                            nq += 1
                            for t4 in range(4):
                                for kc in range(KC):
                                    kb.mm(pv[1], pv[0][:, t4 * 128:(t4 + 1) * 128], hb_[:, kc, t4 * 128:(t4 + 1) * 128], wi[0][:, kc, :],
                                          kc == 0, kc == KC - 1, [hbk, wi[1]])
                            kb.op("act", lambda a, pv=pv, blk=blk, dst=dst: a.activation(
                                out=dst[0][:, blk * 4:(blk + 1) * 4, :].rearrange("p t d -> p (t d)"), in_=pv[0][:], func=AF.Copy),
                                reads=[pv[1]], writes=[dst[1]])
                        if hk == 0:
                            pg_ = pq[nq % 2]
                            nq += 1
                            for kc in range(KC):
                                kb.mm(pg_[1], pg_[0][0:48, :], wgt[0][:, kc, :], hb_[:, kc, :], kc == 0, kc == KC - 1, [wgt[1], hbk])
                            gb_, gbk = gsb[blk % 2]
                            kb.op("act", lambda a, pg_=pg_, gb_=gb_: a.activation(out=gb_[0:48, :], in_=pg_[0][0:48, :], func=AF.Sigmoid),
                                  reads=[pg_[1]], writes=[gbk])
                            kb.dma("sp", gsc[:, bs], gb_[0:48, :], gbk, False)
                    kb.barrier()
                with ExitStack() as es:
                    cm0 = sb(nc, es, "cm0", [128, 17, 128], BF16)
                    cm1 = sb(nc, es, "cm1", [128, 16, 128], BF16)
                    ovl = sb(nc, es, "ovl", [128, 2, 64], BF16)
                    Fm = sb(nc, es, "Fm", [128, 32, 64], F32)
                    Em = sb(nc, es, "Em", [128, 32, 128], BF16)
                    ck = Tok()
                    kb.dma("pool", cm0[:], c.n_cm0, ck, True)
                    kb.dma("pool", cm1[:], c.n_cm1, ck, True)
                    kb.dma("pool", ovl[:], c.n_ovl, ck, True)
                    kb.dma("sp", Fm[:], c.n_F, ck, True)
                    kb.dma("pool", Em[0:64, :, :], c.n_E, ck, True)
                    w1p = [(sb(nc, es, "w1p%d" % i, [128, 32, 128], BF16), Tok()) for i in range(2)]
                    w2d = (sb(nc, es, "w2d", [128, 3, 128], BF16), Tok())
                    peT = (sb(nc, es, "peT", [128, 32], BF16), Tok())
                    bias2 = (sb(nc, es, "bias2", [128, 2], F32), Tok())
                    hx = (sb(nc, es, "hx", [128, 2, 256], F32), Tok())
                    hy = (sb(nc, es, "hy", [128, 2, 256], F32), Tok())
                    hg = (sb(nc, es, "hg", [128, 2, 256], BF16), Tok())
                    KcT2 = [(sb(nc, es, "KcT%d" % i, [128, 256], BF16), Tok()) for i in range(2)]
                    ost = [(sb(nc, es, "ost%d" % i, [128, 512], BF16), Tok()) for i in range(2)]
                    Vc = (sb(nc, es, "Vc", [128, 2, 128], BF16), Tok())
                    penT = (sb(nc, es, "penT", [128, S], BF16), Tok())
                    ocmp = (sb(nc, es, "ocmp", [128, 4, 512], F32), Tok())
                    owin = (sb(nc, es, "owin", [128, 4, 512], F32), Tok())
                    den = [(sb(nc, es, "den%d" % i, [128, 512], F32), Tok()) for i in range(2)]
                    impn = (sb(nc, es, "impn", [128, 512], F32), Tok())
                    impT = (sb(nc, es, "impT", [128, 128], F32), Tok())
                    tk = (sb(nc, es, "tk", [128, 160], F32), Tok())
                    penb = (sb(nc, es, "penb", [128, 64], BF16), Tok())
                    Gb = [(sb(nc, es, "Gb%d" % i, [128, 512], F32), Tok()) for i in range(6)]
                    acc = (sb(nc, es, "acc", [128, 512], F32), Tok())
                    tmpa = (sb(nc, es, "tmpa", [128, 512], F32), Tok())
                    ab = AttnBufs(kb, es)
                    pimp = (ps(nc, es, "pimp", [128, 512]), Tok())
                    pmisc = (ps(nc, es, "pmisc", [128, 512]), Tok())
                    pmb = (ps(nc, es, "pmb", [128, 1024], BF16), Tok())
                    for i_ in range(2):
                        kb.op("pool", lambda g, i_=i_: g.memset(w1p[i_][0][:], 0.0), writes=[w1p[i_][1]])
                    kb.op("pool", lambda g: g.memset(w2d[0][:], 0.0), writes=[w2d[1]])
                    kb.dma("pool", w1p[0][0][0:64, :, :], wk1.rearrange("(l d) j -> d l j", d=64), w1p[0][1], True)
                    kb.dma("pool", w1p[1][0][64:128, :, :], wv1.rearrange("(l d) j -> d l j", d=64), w1p[1][1], True)
                    kb.dma("pool", w2d[0][:, 0, 0:64], wk2, w2d[1], True)
                    kb.dma("pool", w2d[0][:, 1, 64:128], wk2, w2d[1], True)
                    for hf in range(2):
                        kb.dma("pool", w2d[0][:, 2, hf * 64:(hf + 1) * 64], wv2, w2d[1], True)
                    kb.dma("pool", peT[0][0:64, :], pos_k.rearrange("l d -> d l"), peT[1], True, allow_slow_non_contiguous=True)
                    kb.dma("pool", peT[0][64:128, :], pos_v.rearrange("l d -> d l"), peT[1], True, allow_slow_non_contiguous=True)
                    for kv in range(2):
                        for l in range(32):
                            kb.mm(pmisc[1], pmisc[0][:, kv:kv + 1], w1p[kv][0][:, l, :], peT[0][:, l:l + 1], l == 0, l == 31, [w1p[kv][1], peT[1]])
                    kb.op("act", lambda a: a.activation(out=bias2[0][:], in_=pmisc[0][:, 0:2], func=AF.Copy), reads=[pmisc[1]], writes=[bias2[1]])
                    for kv in range(2):
                        for l in range(32):
                            kb.mm(pimp[1], pimp[0][:, kv * 256:kv * 256 + NCMP], w1p[kv][0][:, l, :], KVc[0][:, l:l + 16 * (NCMP - 1) + 1:16],
                                  l == 0, l == 31, [w1p[kv][1], KVc[1]])
                    C0 = 0.7978845608028654
                    for kv in range(2):
                        x_ = hx[0][:, kv, 0:NCMP]
                        y_ = hy[0][:, kv, 0:NCMP]
                        kb.op("act", lambda a, kv=kv, x_=x_: a.activation(out=x_, in_=pimp[0][:, kv * 256:kv * 256 + NCMP], func=AF.Identity,
                                                                       bias=bias2[0][:, kv:kv + 1], scale=1.0), reads=[pimp[1], bias2[1]], writes=[hx[1]])
                        kb.op("dve", lambda v, x_=x_, y_=y_: v.tensor_tensor(out=y_, in0=x_, in1=x_, op=ALU.mult), reads=[hx[1]], writes=[hy[1]])
                        kb.op("dve", lambda v, y_=y_: v.tensor_scalar(out=y_, in0=y_, scalar1=0.044715, scalar2=1.0, op0=ALU.mult, op1=ALU.add),
                              reads=[hy[1]], writes=[hy[1]])
                        kb.op("dve", lambda v, x_=x_, y_=y_: v.tensor_tensor(out=y_, in0=y_, in1=x_, op=ALU.mult), reads=[hx[1], hy[1]], writes=[hy[1]])
                        kb.op("act", lambda a, y_=y_: a.activation(out=y_, in_=y_, func=AF.Tanh, scale=C0), reads=[hy[1]], writes=[hy[1]])
                        kb.op("dve", lambda v, x_=x_, y_=y_: v.scalar_tensor_tensor(out=y_, in0=y_, scalar=1.0, in1=x_, op0=ALU.add, op1=ALU.mult),
                              reads=[hx[1], hy[1]], writes=[hy[1]])
                        kb.op("dve", lambda v, kv=kv, y_=y_: v.tensor_scalar(out=hg[0][:, kv, 0:NCMP], in0=y_, scalar1=0.5, scalar2=None, op0=ALU.mult),
                              reads=[hy[1]], writes=[hg[1]])
                    for i_ in range(2):
                        kb.mm(pmisc[1], pmisc[0][:, 0:NCMP], w2d[0][:, i_, :], hg[0][:, 0, 0:NCMP], True, True, [w2d[1], hg[1]])
                        kb.op("act", lambda a, i_=i_: a.activation(out=KcT2[i_][0][:, 0:NCMP], in_=pmisc[0][:, 0:NCMP], func=AF.Copy),
                              reads=[pmisc[1]], writes=[KcT2[i_][1]])
                    kb.mm(pimp[1], pimp[0][:, 0:128], hg[0][:, 1, 0:128], w2d[0][:, 2, :], True, True, [w2d[1], hg[1]])
                    kb.mm(pimp[1], pimp[0][0:127, 128:256], hg[0][:, 1, 128:NCMP], w2d[0][:, 2, :], True, True, [w2d[1], hg[1]])
                    kb.op("act", lambda a: a.activation(out=Vc[0][:, 0, :], in_=pimp[0][:, 0:128], func=AF.Copy), reads=[pimp[1]], writes=[Vc[1]])
                    kb.op("act", lambda a: a.activation(out=Vc[0][0:127, 1, :], in_=pimp[0][0:127, 128:256], func=AF.Copy), reads=[pimp[1]], writes=[Vc[1]])
                    ngb = 0
                    for qg in range(8):
                        for b4 in range(4):
                            n = 4 * qg + b4
                            qs = slice(n * 128, (n + 1) * 128)
                            c2s = [0] + ([1] if n >= 16 else [])
                            for ci, c2 in enumerate(c2s):
                                nk = 128 if c2 == 0 else 127
                                rows = slice(0, nk)
                                mk = None
                                if c2 == 0 and n < 17:
                                    mk = cm0[rows, n, :]
                                if c2 == 1:
                                    mk = cm1[rows, n - 16, :]
                                sb_, sk = ab.pss[ab.n % 2]
                                pt_, ptk = ab.PT[ab.n % 3]
                                ab.n += 1
                                for g in range(4):
                                    gc = slice(g * 128, (g + 1) * 128)
                                    if mk is not None:
                                        kb.mm(sk, sb_[rows, gc], c.identb[rows, 0:nk], mk, True, False, [c.const_k, ck])
                                    kb.mm(sk, sb_[rows, gc], KcT2[g % 2][0][:, c2 * 128:c2 * 128 + nk], QT[0][:, g // 2, qs], mk is None, True,
                                          [KcT2[g % 2][1], QT[1]])
                                kb.op("act", lambda a, pt_=pt_, sb_=sb_, rows=rows: a.activation(out=pt_[rows, :], in_=sb_[rows, :], func=AF.Exp, scale=0.125),
                                      reads=[sk], writes=[ptk])
                                last = ci == len(c2s) - 1
                                kb.mm(ab.pso[1], ab.pso[0][:], Vc[0][rows, c2, :], pt_[rows, :], ci == 0, last, [Vc[1], ptk])
                                kb.mm(ab.psm[1], ab.psm[0][:], c.onesb[rows, :], pt_[rows, :], ci == 0, last, [c.const_k, ptk])
                                kb.mm(pimp[1], pimp[0][0:64, :], ovl[rows, c2, :], pt_[rows, :], ci == 0, last, [ck, ptk])
                            dn, dnk = den[0]
                            kb.op("dve", lambda v, dn=dn: v.tensor_scalar(out=dn[:], in0=ab.psm[0][:], scalar1=1e-30, scalar2=None, op0=ALU.max),
                                  reads=[ab.psm[1]], writes=[dnk])
                            kb.op("dve", lambda v, dn=dn: v.reciprocal(out=dn[:], in_=dn[:]), reads=[dnk], writes=[dnk])
                            kb.op("dve", lambda v, dn=dn, b4=b4: v.tensor_tensor(out=ocmp[0][:, b4, :], in0=ab.pso[0][:], in1=dn[:], op=ALU.mult),
                                  reads=[ab.pso[1], dnk], writes=[ocmp[1]])
                            kb.op("dve", lambda v, dn=dn: v.tensor_tensor(out=impn[0][0:64, :], in0=pimp[0][0:64, :], in1=dn[0:64, :], op=ALU.mult),
                                  reads=[pimp[1], dnk], writes=[impn[1]])
                            kb.op("dve", lambda v: v.tensor_reduce(out=impT[0][0:64, :], in_=impn[0][0:64, :].rearrange("p (g i) -> p i g", g=4),
                                                                   axis=AX.X, op=ALU.add), reads=[impn[1]], writes=[impT[1]])
                            kb.op("pe", lambda pe: pe.transpose(out=pmisc[0][:, 0:64], in_=impT[0][0:64, :], identity=c.ident[0:64, 0:64]),
                                  reads=[impT[1], c.ident_k], writes=[pmisc[1]])
                            impF, m8a, m8b, wrk = tk[0][:, 0:64], tk[0][:, 64:72], tk[0][:, 72:80], tk[0][:, 80:144]
                            kb.op("dve", lambda v, n=n: v.tensor_tensor(out=impF, in0=pmisc[0][:, 0:64], in1=Fm[:, n, :], op=ALU.add),
                                  reads=[pmisc[1], ck], writes=[tk[1]])
                            kb.op("dve", lambda v: v.max(out=m8a, in_=impF), reads=[tk[1]], writes=[tk[1]])
                            kb.op("dve", lambda v: v.match_replace(out=wrk, in_to_replace=m8a, in_values=impF, imm_value=-3.0e38), reads=[tk[1]], writes=[tk[1]])
                            kb.op("dve", lambda v: v.max(out=m8b, in_=wrk), reads=[tk[1]], writes=[tk[1]])
                            kb.op("dve", lambda v: v.tensor_scalar(out=wrk, in0=impF, scalar1=m8b[:, 7:8], scalar2=None, op0=ALU.is_ge), reads=[tk[1]], writes=[tk[1]])
                            kb.op("dve", lambda v: v.tensor_scalar(out=penb[0][:], in0=wrk, scalar1=-NEG, scalar2=NEG, op0=ALU.mult, op1=ALU.add),
                                  reads=[tk[1]], writes=[penb[1]])
                            kb.op("pe", lambda pe: pe.transpose(out=pmb[0][0:64, 0:128], in_=penb[0][:, :], identity=c.identb[:, :]),
                                  reads=[penb[1], c.const_k], writes=[pmb[1]])
                            kb.op("act", lambda a, qs=qs: a.activation(out=penT[0][0:64, qs], in_=pmb[0][0:64, 0:128], func=AF.Copy), reads=[pmb[1]], writes=[penT[1]])
                            kts = list(range(max(0, n - 4), n + 1))
                            for ki, kt in enumerate(kts):
                                ks = slice(kt * 128, (kt + 1) * 128)
                                mk = c.mb_cur if kt == n else (c.mb_prev if kt == n - 4 else None)
                                sb_, sk = ab.pss[ab.n % 2]
                                pt_, ptk = ab.PT[ab.n % 3]
                                ab.n += 1
                                if mk is not None:
                                    kb.mm(sk, sb_[:], c.identb[:], mk[:], True, False, [c.const_k])
                                for g in range(4):
                                    kb.mm(sk, sb_[:, g * 128:(g + 1) * 128], KwT2[g % 2][0][:, ks], QT[0][:, g // 2, qs], mk is None, mk is None or g == 3,
                                          [KwT2[g % 2][1], QT[1]])
                                kb.op("act", lambda a, pt_=pt_, sb_=sb_: a.activation(out=pt_[:], in_=sb_[:], func=AF.Exp, scale=0.125), reads=[sk], writes=[ptk])
                                kb.mm(ab.pso[1], ab.pso[0][:], Vw[0][:, kt, :], pt_[:], ki == 0, ki == len(kts) - 1, [Vw[1], ptk])
                                kb.mm(ab.psm[1], ab.psm[0][:], c.onesb[:], pt_[:], ki == 0, ki == len(kts) - 1, [c.const_k, ptk])
                            dn, dnk = den[1]
                            kb.op("dve", lambda v, dn=dn: v.reciprocal(out=dn[:], in_=ab.psm[0][:]), reads=[ab.psm[1]], writes=[dnk])
                            kb.op("dve", lambda v, dn=dn, b4=b4: v.tensor_tensor(out=owin[0][:, b4, :], in0=ab.pso[0][:], in1=dn[:], op=ALU.mult),
                                  reads=[ab.pso[1], dnk], writes=[owin[1]])
                        for g in range(4):
                            hs = slice((g % 2) * 64, (g % 2) * 64 + 64)
                            head = 4 * hk + g
                            gbs = []
                            for br in range(3):
                                gt_, gtk = Gb[ngb % 6]
                                ngb += 1
                                row = head * 3 + br
                                kb.dma("sp", gt_[:], gsc[row:row + 1, qg * 512:(qg + 1) * 512].partition_broadcast(128), gtk, True)
                                gbs.append((gt_, gtk))

                            def bias_fn(sk, sb_, kt, qg_, lo, first):
                                kb.mm(sk, sb_[:, lo:512], Em[0:64, kt, :], penT[0][0:64, qg_ * 512 + lo:(qg_ + 1) * 512], first, False, [ck, penT[1]])
                                return False

                            def on_done(qg_, hs=hs, g=g, gbs=gbs):
                                dn, dnk = den[0]
                                qsl = slice(qg_ * 512, (qg_ + 1) * 512)
                                kb.op("dve", lambda v: v.reciprocal(out=dn[hs, :], in_=ab.psm[0][hs, :]), reads=[ab.psm[1]], writes=[dnk])
                                kb.op("dve", lambda v: v.tensor_tensor(out=dn[hs, :], in0=dn[hs, :], in1=gbs[1][0][hs, :], op=ALU.mult),
                                      reads=[dnk, gbs[1][1]], writes=[dnk])
                                kb.op("dve", lambda v: v.tensor_tensor(out=acc[0][hs, :], in0=ab.pso[0][hs, :], in1=dn[hs, :], op=ALU.mult),
                                      reads=[ab.pso[1], dnk], writes=[acc[1]])
                                for (src, gi) in [(ocmp, 0), (owin, 2)]:
                                    kb.op("dve", lambda v, src=src, gi=gi: v.tensor_tensor(
                                        out=tmpa[0][hs, :].rearrange("p (b i) -> p b i", b=4), in0=src[0][hs, :, g * 128:(g + 1) * 128],
                                        in1=gbs[gi][0][hs, :].rearrange("p (b i) -> p b i", b=4), op=ALU.mult),
                                        reads=[src[1], gbs[gi][1]], writes=[tmpa[1]])
                                    kb.op("dve", lambda v: v.tensor_tensor(out=acc[0][hs, :], in0=acc[0][hs, :], in1=tmpa[0][hs, :], op=ALU.add),
                                          reads=[acc[1], tmpa[1]], writes=[acc[1]])
                                ob, obk = ost[g % 2]
                                kb.op("act", lambda a: a.activation(out=ob[hs, :], in_=acc[0][hs, :], func=AF.Copy), reads=[acc[1]], writes=[obk])
                                r0 = (2 * hk + g // 2) * 128 + hs.start
                                kb.dma("sp", ot_dram[r0:r0 + 64, qsl], ob[hs, :], obk, False)

                            causal_attention(kb, c, ab,
                                             lambda kt, g=g: KsT2[g % 2][0][:, kt * 128:(kt + 1) * 128],
                                             lambda a, b_, g=g: QT[0][:, g // 2, a:b_],
                                             lambda kt: Vs[0][:, kt, :],
                                             0.125, [KsT2[g % 2][1], QT[1]], [Vs[1]], on_done, qgroups=[qg], bias_fn=bias_fn)
                    kb.barrier()
        outproj_epilogue(kb, c, None, w_o.rearrange("(c p) n -> p c n", p=128), src_h, dst_h, dst_hT, ln_g, ln_b, router, rw_dst,
                         ot_dram=ot_dram)


W_SHAPES = {
    "a_w_in": (1024, 1536), "a_sinks": (1, 16), "a_w_o": (1024, 1024),
    "b_w_down": (1024, 672), "b_q_norm": (1, 384), "b_kv_norm": (1, 256), "b_w_uq": (384, 1536),
    "b_w_ukv": (256, 2048), "b_w_o": (1024, 1024),
    "c_w_in": (1024, 2608), "c_pos_k": (32, 64), "c_pos_v": (32, 64), "c_wk1": (2048, 128), "c_wk2": (128, 64),
    "c_wv1": (2048, 128), "c_wv2": (128, 64), "c_w_o": (1024, 1024),
    "d_w_in": (1024, 3072), "d_lq1": (1, 64), "d_lk1": (1, 64), "d_lq2": (1, 64), "d_lk2": (1, 64),
    "d_subln": (1, 128), "d_w_o": (1024, 1024),
}
LAYER_W = {0: ["a_w_in", "a_sinks", "a_w_o"],
           1: ["b_w_down", "b_q_norm", "b_kv_norm", "b_w_uq", "b_w_ukv", "b_w_o"],
           2: ["c_w_in", "c_pos_k", "c_pos_v", "c_wk1", "c_wk2", "c_wv1", "c_wv2", "c_w_o"],
           3: ["d_w_in", "d_lq1", "d_lk1", "d_lq2", "d_lk2", "d_subln", "d_w_o"]}


def host_constants():
    cst = {}
    cst["ident"] = np.eye(128, dtype=np.float32)
    t = np.arange(S, dtype=np.float32)

    def rope_tab(rot_dim, base_part, period):
        half = rot_dim // 2
        inv = (1.0 / (np.float32(500000.0) ** (np.arange(half, dtype=np.float32) * np.float32(2.0 / rot_dim)))).astype(np.float32)
        ang = t[None, :] * inv[:, None]
        cos = np.ones((128, S), np.float32)
        sin = np.zeros((128, S), np.float32)
        for p in range(128):
            d = (p - base_part) % period
            if p < base_part:
                continue
            if d < half:
                cos[p] = np.cos(ang[d]); sin[p] = -np.sin(ang[d])
            elif d < rot_dim:
                cos[p] = np.cos(ang[d - half]); sin[p] = np.sin(ang[d - half])
        return np.stack([cos, sin]).astype(np.float32)

    cst["rope16"] = rope_tab(16, 0, 64)
    cst["rope32"] = rope_tab(32, 64, 64)
    jj = np.arange(128)[:, None]
    ii = np.arange(128)[None, :]
    cur = np.where(jj <= ii, 0.0, NEG).astype(np.float32)
    prev = np.where(jj > ii, 0.0, NEG).astype(np.float32)
    diag = np.concatenate([cur, np.zeros((128, 384), np.float32)], axis=1)
    nn = np.arange(256)
    cend = 16 * nn + 31
    tq = np.arange(S).reshape(32, 128)
    vis = (cend[:, None, None] <= tq[None, :, :]) & (nn[:, None, None] < 255)
    cmk = np.where(vis, 0.0, NEG).astype(np.float32)
    cst["ncm0"] = np.ascontiguousarray(cmk[0:128, 0:17, :])
    cst["ncm1"] = np.ascontiguousarray(cmk[128:256, 16:32, :])
    cstart = 16 * nn
    sstart = 64 * np.arange(64)
    ov = ((cstart[:, None] <= sstart[None, :] + 63) & (sstart[None, :] <= cend[:, None]) & (nn[:, None] < 255)).astype(np.float32)
    cst["novl"] = np.ascontiguousarray(ov.reshape(2, 128, 64).transpose(1, 0, 2))
    curb = (tq // 64)[:, :, None]
    jb = np.arange(64)[None, None, :]
    Fm = np.where((jb == 0) | (jb == curb) | (jb == curb - 1), 1e30, 0.0)
    Fm = np.where(jb <= curb, Fm, -1e30).astype(np.float32)
    cst["nF"] = np.ascontiguousarray(Fm.transpose(1, 0, 2))
    Em = np.zeros((64, 32, 128), np.float32)
    for kt in range(32):
        for r in range(128):
            Em[2 * kt + r // 64, kt, r] = 1.0
    cst["nE"] = Em
    cst["maskb"] = np.stack([np.tile(cur, (1, 4)), np.tile(prev, (1, 4)), diag]).astype(np.float32)
    return cst


def build_program(n_layers=DEPTH, debug=False, layer_kinds=None, upto=99):
    nc = bass.Bass("TRN2", target_bir_lowering=False)
    kinds = layer_kinds if layer_kinds is not None else [i % 4 for i in range(n_layers)]
    c = Ctx()
    dk = "ExternalOutput" if debug else "Internal"

    def din(name, shape, dt=F32):
        return nc.dram_tensor(name, list(shape), dt, kind="ExternalInput").ap()

    x = din("x", [S, D])
    W = {}
    for kind in sorted(set(kinds)):
        for nm in LAYER_W[kind]:
            W[nm] = din(nm, W_SHAPES[nm])
    moe = []
    for i in range(n_layers):
        moe.append(dict(wg=din("moe_w_group_%d" % i, [D, 4]), we=din("moe_w_expert_%d" % i, [D, 32]),
                        gate=din("moe_w_gate_%d" % i, [32, D, 512]), up=din("moe_w_up_%d" % i, [32, D, 512]),
                        down=din("moe_w_down_%d" % i, [32, 512, D])))
    ln_g = din("ln_g", [n_layers * 2, D])
    ln_b = din("ln_b", [n_layers * 2, D])
    c_ident = din("c_ident", [128, 128])
    c.rope16 = din("c_rope16", [2, 128, S])
    c.rope32 = din("c_rope32", [2, 128, S])
    c_maskb = din("c_maskb", [3, 128, 512])
    if 2 in kinds:
        c.n_cm0 = din("c_ncm0", [128, 17, 128])
        c.n_cm1 = din("c_ncm1", [128, 16, 128])
        c.n_ovl = din("c_novl", [128, 2, 64])
        c.n_F = din("c_nF", [128, 32, 64])
        c.n_E = din("c_nE", [64, 32, 128])
    gsc = nc.dram_tensor("gsc", [48, S], F32, kind="Internal").ap()
    ot_dram = nc.dram_tensor("ot_dram", [D, S], BF16, kind="Internal").ap()
    out = nc.dram_tensor("out", [S, D], F32, kind="ExternalOutput").ap()
    hmid = nc.dram_tensor("hmid", [S, D], F32, kind=dk).ap()
    hcur = nc.dram_tensor("hcur", [S, D], F32, kind=dk).ap()
    hT_a = nc.dram_tensor("hT_a", [D, S], BF16, kind=dk).ap()
    hT_b = nc.dram_tensor("hT_b", [D, S], BF16, kind=dk).ap()
    rw = nc.dram_tensor("rw", [S, 32], F32, kind=dk).ap()

    with ExitStack() as es:
        kb = KB(nc, es)
        c.ident = sb(nc, es, "ident", [128, 128], F32)
        c.ident_k = Tok()
        c.identb = sb(nc, es, "identb", [128, 128], BF16)
        c.onesb = sb(nc, es, "onesb", [128, 128], BF16)
        c.mb_cur = sb(nc, es, "mbcur", [128, 512], BF16)
        c.mb_prev = sb(nc, es, "mbprev", [128, 512], BF16)
        c.mb_diag = sb(nc, es, "mbdiag", [128, 512], BF16)
        c.const_k = Tok()
        kb.dma("sp", c.ident[:], c_ident, c.ident_k, True)
        kb.dma("pool", c.identb[:], c_ident, c.const_k, True)
        kb.dma("pool", c.mb_cur[:], c_maskb[0], c.const_k, True)
        kb.dma("pool", c.mb_prev[:], c_maskb[1], c.const_k, True)
        kb.dma("pool", c.mb_diag[:], c_maskb[2], c.const_k, True)
        kb.op("pool", lambda g: g.memset(c.onesb[:], 1.0), writes=[c.const_k])
        phase_transpose_in(kb, c, x, hT_a)
        src = x
        for i in range(n_layers):
            if upto < 1:
                break
            kind = kinds[i]
            router = (moe[i]["wg"], moe[i]["we"])
            g0, b0 = ln_g[2 * i:2 * i + 1, :], ln_b[2 * i:2 * i + 1, :]
            g1, b1 = ln_g[2 * i + 1:2 * i + 2, :], ln_b[2 * i + 1:2 * i + 2, :]
            if kind == 0:
                phase_swa(kb, c, src, hT_a, W["a_w_in"], W["a_sinks"], W["a_w_o"], g0, b0, router, hmid, hT_b, rw)
            elif kind == 1:
                phase_mla(kb, c, src, hT_a, W["b_w_down"], W["b_q_norm"], W["b_kv_norm"], W["b_w_uq"], W["b_w_ukv"], W["b_w_o"],
                          g0, b0, router, hmid, hT_b, rw)
            elif kind == 3:
                phase_diff(kb, c, 3, src, hT_a, W["d_w_in"], W["d_lq1"], W["d_lk1"], W["d_lq2"], W["d_lk2"], W["d_subln"], W["d_w_o"],
                           g0, b0, router, hmid, hT_b, rw)
            elif kind == 2:
                phase_nsa(kb, c, src, hT_a, W["c_w_in"], W["c_pos_k"], W["c_pos_v"], W["c_wk1"], W["c_wk2"], W["c_wv1"], W["c_wv2"],
                          W["c_w_o"], g0, b0, router, hmid, hT_b, rw, gsc, ot_dram)
            else:
                raise NotImplementedError
            if upto < 2:
                break
            last = i == n_layers - 1
            dst = out if last else hcur
            phase_moe(kb, c, hmid, hT_b, rw, moe[i]["gate"], moe[i]["up"], moe[i]["down"], g1, b1, dst,
                      None if last else hT_a)
            src = hcur
        kb.final_wait()
        c.n_inst = kb.n_inst
        c.log = kb.log
    return nc, c


def make_inputs(inputs, n_layers=DEPTH, layer_kinds=None):
    kinds = layer_kinds if layer_kinds is not None else [i % 4 for i in range(n_layers)]
    cst = host_constants()
    shared = {}
    for kind in sorted(set(kinds)):
        for nm in LAYER_W[kind]:
            shared[nm] = np.ascontiguousarray(np.asarray(inputs[nm], np.float32).reshape(W_SHAPES[nm]))
    for i in range(n_layers):
        shared["moe_w_group_%d" % i] = np.ascontiguousarray(inputs["moe_w_group"][i])
        shared["moe_w_expert_%d" % i] = np.ascontiguousarray(inputs["moe_w_expert"][i])
        shared["moe_w_gate_%d" % i] = np.ascontiguousarray(inputs["moe_w_gate"][i])
        shared["moe_w_up_%d" % i] = np.ascontiguousarray(inputs["moe_w_up"][i])
        shared["moe_w_down_%d" % i] = np.ascontiguousarray(inputs["moe_w_down"][i])
    shared["ln_g"] = np.ascontiguousarray(np.asarray(inputs["ln_g"], np.float32)[:n_layers].reshape(n_layers * 2, D))
    shared["ln_b"] = np.ascontiguousarray(np.asarray(inputs["ln_b"], np.float32)[:n_layers].reshape(n_layers * 2, D))
    shared["c_ident"] = cst["ident"]
    shared["c_rope16"] = cst["rope16"]
    shared["c_rope32"] = cst["rope32"]
    shared["c_maskb"] = cst["maskb"]
    if 2 in kinds:
        for nm in ["ncm0", "ncm1", "novl", "nF", "nE"]:
            shared["c_" + nm] = cst[nm]
    return shared


def kernel(**inputs):
    x = np.asarray(inputs["x"], np.float32)
    nb = x.shape[0]
    nc, _ = build_program()
    shared = make_inputs(inputs)
    in_maps = []
    for b in range(nb):
        m = dict(shared)
        m["x"] = np.ascontiguousarray(x[b])
        in_maps.append(m)
    res = run_bass_kernel_spmd(nc, in_maps, core_ids=list(range(nb)))
    return np.stack([np.asarray(r["out"], np.float32) for r in res.results], axis=0)
```
